# Optimizing a Trainium2 kernel written in Bass

```python
import math
import jax
import jax.numpy as jnp
from jax import lax
import numpy as np

D_MODEL = 1024
BATCH = 1
SEQ = 16384
DEPTH = 1

CTX_LEN = 256
GRID_W = 64
HEAD_DIM = 64
ATTN_HEADS = 8
ATTN_KV_HEADS = 2
GQA_GROUP = ATTN_HEADS // ATTN_KV_HEADS
RET_HEADS = 4
RET_QK_DIM = 64
RET_V_DIM = 128
ATTN_WIDTH = ATTN_HEADS * HEAD_DIM
RET_WIDTH = RET_HEADS * RET_V_DIM
MIX_WIDTH = ATTN_WIDTH + RET_WIDTH
PROJ_SIZES = (ATTN_WIDTH, ATTN_KV_HEADS * HEAD_DIM, ATTN_KV_HEADS * HEAD_DIM,
              RET_HEADS * RET_QK_DIM, RET_HEADS * RET_QK_DIM, RET_WIDTH, RET_WIDTH)
PROJ_DIM = sum(PROJ_SIZES)
PROJ_SPLITS = tuple(int(v) for v in np.cumsum(PROJ_SIZES)[:-1])
Q_BLOCK = 128
RET_CHUNK = 128
ROPE_THETA = 10000.0
ROPE_AXIS_DIM = HEAD_DIM // 2
N_GROUPS = 4
EXPERTS_PER_GROUP = 8
N_EXPERTS = N_GROUPS * EXPERTS_PER_GROUP
TOP_K_IN_GROUP = 2
D_EXPERT = 512
MOE_BLOCK = 128
EPS = 1e-6

kernel_name = "hybrid_attn_retention_hmoe_dit"


def _rmsnorm(x, g):
    xf = x.astype(jnp.float32)
    xf = xf * lax.rsqrt(jnp.mean(xf * xf, axis=-1, keepdims=True) + EPS)
    return xf.astype(x.dtype) * g


def _modulate(xn, shift, scale):
    return xn * (1.0 + scale) + shift


def _axial_rope_tables(rows):
    row = jnp.repeat(jnp.arange(rows), GRID_W).astype(jnp.float32)
    col = jnp.tile(jnp.arange(GRID_W), rows).astype(jnp.float32)
    freqs = ROPE_THETA ** (-jnp.arange(0, ROPE_AXIS_DIM, 2, dtype=jnp.float32) / ROPE_AXIS_DIM)
    ang = jnp.concatenate([row[:, None] * freqs, col[:, None] * freqs], axis=-1)
    return jnp.cos(ang), jnp.sin(ang)


def _rope(x, cos, sin):
    B, L, H, d = x.shape
    xp = x.astype(jnp.float32).reshape(B, L, H, d // 2, 2)
    x0, x1 = xp[..., 0], xp[..., 1]
    c = cos[None, :, None, :]
    s = sin[None, :, None, :]
    out = jnp.stack([x0 * c - x1 * s, x0 * s + x1 * c], axis=-1).reshape(B, L, H, d)
    return out.astype(x.dtype)


def _attend(q, k, v):
    B, L, _, _ = q.shape
    nb = L // Q_BLOCK
    qb = jnp.moveaxis(q.reshape(B, nb, Q_BLOCK, ATTN_KV_HEADS, GQA_GROUP, HEAD_DIM), 1, 0)
    scale = HEAD_DIM ** -0.5

    def one_block(q_blk):
        s = jnp.einsum('bqhgd,bkhd->bhgqk', q_blk, k).astype(jnp.float32) * scale
        p = jax.nn.softmax(s, axis=-1)
        return jnp.einsum('bhgqk,bkhd->bqhgd', p.astype(v.dtype), v)

    o = lax.map(one_block, qb)
    return jnp.moveaxis(o, 0, 1).reshape(B, L, ATTN_WIDTH)


def _head_rms(x, g):
    xf = x.astype(jnp.float32)
    xf = xf * lax.rsqrt(jnp.mean(xf * xf, axis=-1, keepdims=True) + EPS)
    return xf.astype(x.dtype) * g


def _ret_heads(q, k, v, cos, sin):
    B, L, _ = q.shape
    q = q.reshape(B, L, RET_HEADS, RET_QK_DIM)
    k = k.reshape(B, L, RET_HEADS, RET_QK_DIM)
    if cos is not None:
        q = _rope(q, cos, sin)
        k = _rope(k, cos, sin)
    q = jnp.transpose(q, (0, 2, 1, 3)).astype(jnp.float32)
    k = jnp.transpose(k, (0, 2, 1, 3)).astype(jnp.float32) * (RET_QK_DIM ** -0.5)
    v = jnp.transpose(v.reshape(B, L, RET_HEADS, RET_V_DIM), (0, 2, 1, 3)).astype(jnp.float32)
    return q, k, v


def _retention_state(k, v, log_g, reverse):
    Lc = k.shape[2]
    m = jnp.arange(Lc, dtype=jnp.float32)
    dist = m if reverse else (Lc - 1 - m)
    w = jnp.exp(log_g[:, None] * dist[None, :])
    return jnp.einsum('bhmk,bhmv,hm->bhkv', k, v, w)


def _retention_chunked(q, k, v, log_g, s0, include_diag):
    B, H, L, dk = q.shape
    dv = v.shape[-1]
    C = RET_CHUNK
    n = L // C
    qc = q.reshape(B, H, n, C, dk)
    kc = k.reshape(B, H, n, C, dk)
    vc = v.reshape(B, H, n, C, dv)
    pos = jnp.arange(C, dtype=jnp.float32)
    diff = pos[:, None] - pos[None, :]
    mask = (diff >= 0) if include_diag else (diff > 0)
    dmat = jnp.where(mask, jnp.exp(log_g[:, None, None] * jnp.where(mask, diff, 0.0)[None]), 0.0)
    scores = jnp.einsum('bhnik,bhnjk->bhnij', qc, kc) * dmat[None, :, None]
    intra = jnp.einsum('bhnij,bhnjv->bhniv', scores, vc)
    zeta = jnp.exp(log_g[:, None] * (C - 1 - pos)[None, :])
    u = jnp.einsum('bhnjk,bhnjv,hj->bhnkv', kc, vc, zeta)
    g_chunk = jnp.exp(log_g * C)[None, :, None, None]

    def step(s, u_n):
        return g_chunk * s + u_n, s

    s_final, s_prev = lax.scan(step, s0, jnp.moveaxis(u, 2, 0))
    xi = jnp.exp(log_g[:, None] * (pos + 1.0)[None, :])
    cross = jnp.einsum('bhnik,nbhkv,hi->bhniv', qc, s_prev, xi)
    return (intra + cross).reshape(B, H, L, dv), s_final


def _retention_latent(q, k, v, k_c, v_c, log_gf, log_gb):
    s_f = _retention_state(k_c, v_c, log_gf, reverse=False)
    s_b = _retention_state(k_c, v_c, log_gb, reverse=True)
    o_f, _ = _retention_chunked(q, k, v, log_gf, s_f, True)
    o_b, _ = _retention_chunked(jnp.flip(q, 2), jnp.flip(k, 2), jnp.flip(v, 2), log_gb, s_b, False)
    return o_f + jnp.flip(o_b, 2)


def _retention_context(q, k, v, log_gf, log_gb):
    B, H, _, dk = q.shape
    s0 = jnp.zeros((B, H, dk, v.shape[-1]), jnp.float32)
    o_f, _ = _retention_chunked(q, k, v, log_gf, s0, True)
    o_b, _ = _retention_chunked(jnp.flip(q, 2), jnp.flip(k, 2), jnp.flip(v, 2), log_gb, s0, False)
    return o_f + jnp.flip(o_b, 2)


def _ret_output(o, gn_g, gn_b, gate, dtype):
    B, H, L, dv = o.shape
    mu = jnp.mean(o, axis=-1, keepdims=True)
    var = jnp.mean(jnp.square(o - mu), axis=-1, keepdims=True)
    on = (o - mu) * lax.rsqrt(var + EPS)
    on = jnp.transpose(on, (0, 2, 1, 3)).reshape(B, L, H * dv).astype(dtype)
    return (on * gn_g + gn_b) * jax.nn.silu(gate)


def _hier_moe(h, w_grp, b_grp, w_exp, b_exp, w_gate, w_up, w_down):
    T, D = h.shape
    grp_p = jax.nn.softmax((h @ w_grp).astype(jnp.float32) + b_grp, axis=-1)
    p_g, g_idx = lax.top_k(grp_p, 1)
    exp_logits = ((h @ w_exp).astype(jnp.float32) + b_exp).reshape(T, N_GROUPS, EXPERTS_PER_GROUP)
    sel = jnp.take_along_axis(exp_logits, jnp.broadcast_to(g_idx[:, :, None], (T, 1, EXPERTS_PER_GROUP)), axis=1)[:, 0]
    p_e, j_idx = lax.top_k(jax.nn.softmax(sel, axis=-1), TOP_K_IN_GROUP)
    weights = p_g * (p_e / jnp.sum(p_e, axis=-1, keepdims=True))
    eid = (g_idx * EXPERTS_PER_GROUP + j_idx).astype(jnp.int32)

    A = T * TOP_K_IN_GROUP
    eid_f = eid.reshape(A)
    tok_f = jnp.repeat(jnp.arange(T, dtype=jnp.int32), TOP_K_IN_GROUP)
    w_f = weights.reshape(A)
    order = jnp.argsort(eid_f)
    e_s, tok_s, w_s = eid_f[order], tok_f[order], w_f[order]
    counts = jnp.zeros((N_EXPERTS,), jnp.int32).at[eid_f].add(1)
    starts = jnp.cumsum(counts) - counts
    pcounts = (counts + MOE_BLOCK - 1) // MOE_BLOCK * MOE_BLOCK
    pends = jnp.cumsum(pcounts)
    pstarts = pends - pcounts
    dest = pstarts[e_s] + (jnp.arange(A, dtype=jnp.int32) - starts[e_s])
    P = A + N_EXPERTS * MOE_BLOCK
    nblk = P // MOE_BLOCK
    buf_tok = jnp.full((P,), T, jnp.int32).at[dest].set(tok_s)
    buf_w = jnp.zeros((P,), jnp.float32).at[dest].set(w_s).astype(h.dtype)
    blk_start = jnp.arange(nblk, dtype=jnp.int32) * MOE_BLOCK
    blk_e = jnp.minimum(jnp.sum(blk_start[:, None] >= pends[None, :], axis=1), N_EXPERTS - 1)
    h_pad = jnp.concatenate([h, jnp.zeros((1, D), h.dtype)], axis=0)
    xb = h_pad[buf_tok].reshape(nblk, MOE_BLOCK, D)

    def expert_block(args):
        x_blk, e = args
        return (jax.nn.silu(x_blk @ w_gate[e]) * (x_blk @ w_up[e])) @ w_down[e]

    yb = lax.map(expert_block, (xb, blk_e)).reshape(P, D)
    out = jnp.zeros((T + 1, D), h.dtype).at[buf_tok].add(yb * buf_w[:, None])
    return out[:T]


def setup_inputs(seed: int = 0) -> dict:
    key = jax.random.key(seed)
    ks = jax.random.split(key, 32)
    f32 = jnp.float32

    def nrm(k, shape, scale):
        return jax.random.normal(k, shape, f32) * scale

    base_logit = jnp.asarray(np.log(2.0 ** (5 + np.arange(RET_HEADS)) - 1.0).astype(np.float32))
    return {
        "x": nrm(ks[0], (BATCH, SEQ, D_MODEL), 1.0),
        "c": nrm(ks[1], (BATCH, D_MODEL), 1.0),
        "ctx": nrm(ks[2], (BATCH, CTX_LEN, D_MODEL), 1.0),
        "c_ctx": nrm(ks[3], (D_MODEL,), 1.0),
        "w_ada": nrm(ks[4], (DEPTH, D_MODEL, 6 * D_MODEL), 0.5 * D_MODEL ** -0.5),
        "b_ada": nrm(ks[5], (DEPTH, 6 * D_MODEL), 0.01),
        "norm1_g": 1.0 + nrm(ks[6], (DEPTH, D_MODEL), 0.05),
        "w_in": nrm(ks[7], (DEPTH, D_MODEL, PROJ_DIM), D_MODEL ** -0.5),
        "attn_q_norm": 1.0 + nrm(ks[8], (DEPTH, HEAD_DIM), 0.05),
        "attn_k_norm": 1.0 + nrm(ks[9], (DEPTH, HEAD_DIM), 0.05),
        "ret_decay_fwd": base_logit + nrm(ks[10], (DEPTH, RET_HEADS), 0.1),
        "ret_decay_bwd": base_logit + nrm(ks[11], (DEPTH, RET_HEADS), 0.1),
        "ret_gn_g": 1.0 + nrm(ks[12], (DEPTH, RET_WIDTH), 0.05),
        "ret_gn_b": nrm(ks[13], (DEPTH, RET_WIDTH), 0.01),
        "w_out": nrm(ks[14], (DEPTH, MIX_WIDTH, D_MODEL), MIX_WIDTH ** -0.5),
        "norm2_g": 1.0 + nrm(ks[15], (DEPTH, D_MODEL), 0.05),
        "moe_w_grp": nrm(ks[16], (DEPTH, D_MODEL, N_GROUPS), D_MODEL ** -0.5),
        "moe_b_grp": nrm(ks[17], (DEPTH, N_GROUPS), 0.01),
        "moe_w_exp": nrm(ks[18], (DEPTH, D_MODEL, N_EXPERTS), D_MODEL ** -0.5),
        "moe_b_exp": nrm(ks[19], (DEPTH, N_EXPERTS), 0.01),
        "moe_w_gate": nrm(ks[20], (DEPTH, N_EXPERTS, D_MODEL, D_EXPERT), D_MODEL ** -0.5),
        "moe_w_up": nrm(ks[21], (DEPTH, N_EXPERTS, D_MODEL, D_EXPERT), D_MODEL ** -0.5),
        "moe_w_down": nrm(ks[22], (DEPTH, N_EXPERTS, D_EXPERT, D_MODEL), D_EXPERT ** -0.5),
        "final_norm_g": 1.0 + nrm(ks[23], (D_MODEL,), 0.05),
    }


def reference(x, c, ctx, c_ctx, w_ada, b_ada, norm1_g, w_in, attn_q_norm, attn_k_norm,
              ret_decay_fwd, ret_decay_bwd, ret_gn_g, ret_gn_b, w_out, norm2_g,
              moe_w_grp, moe_b_grp, moe_w_exp, moe_b_exp, moe_w_gate, moe_w_up, moe_w_down,
              final_norm_g):
    B, L, D = x.shape
    CL = ctx.shape[1]
    ROWS = L // GRID_W
    cos, sin = _axial_rope_tables(ROWS)

    for l in range(DEPTH):
        mod_x = (jax.nn.silu(c) @ w_ada[l] + b_ada[l])[:, None, :]
        mod_c = (jax.nn.silu(c_ctx) @ w_ada[l] + b_ada[l])[None, None, :]
        sh1, sc1, gt1, sh2, sc2, gt2 = jnp.split(mod_x, 6, axis=-1)
        csh1, csc1, cgt1, csh2, csc2, cgt2 = jnp.split(mod_c, 6, axis=-1)
        log_gf = jax.nn.log_sigmoid(ret_decay_fwd[l].astype(jnp.float32))
        log_gb = jax.nn.log_sigmoid(ret_decay_bwd[l].astype(jnp.float32))

        hx = _modulate(_rmsnorm(x, norm1_g[l]), sh1, sc1)
        hc = _modulate(_rmsnorm(ctx, norm1_g[l]), csh1, csc1)
        qa, ka, va, qr, kr, vr, gr = jnp.split(hx @ w_in[l], PROJ_SPLITS, axis=-1)
        qa_c, ka_c, va_c, qr_c, kr_c, vr_c, gr_c = jnp.split(hc @ w_in[l], PROJ_SPLITS, axis=-1)

        qa = _rope(_head_rms(qa.reshape(B, L, ATTN_HEADS, HEAD_DIM), attn_q_norm[l]), cos, sin)
        ka = _rope(_head_rms(ka.reshape(B, L, ATTN_KV_HEADS, HEAD_DIM), attn_k_norm[l]), cos, sin)
        va = va.reshape(B, L, ATTN_KV_HEADS, HEAD_DIM)
        ka_c = _head_rms(ka_c.reshape(B, CL, ATTN_KV_HEADS, HEAD_DIM), attn_k_norm[l])
        va_c = va_c.reshape(B, CL, ATTN_KV_HEADS, HEAD_DIM)
        attn_x = _attend(qa, jnp.concatenate([ka_c, ka], axis=1), jnp.concatenate([va_c, va], axis=1))

        q_r, k_r, v_r = _ret_heads(qr, kr, vr, cos, sin)
        q_rc, k_rc, v_rc = _ret_heads(qr_c, kr_c, vr_c, None, None)
        ret_o = _retention_latent(q_r, k_r, v_r, k_rc, v_rc, log_gf, log_gb)
        ret_x = _ret_output(ret_o, ret_gn_g[l], ret_gn_b[l], gr, x.dtype)

        x_new = x + gt1 * (jnp.concatenate([attn_x, ret_x], axis=-1) @ w_out[l])

        h2 = _modulate(_rmsnorm(x_new, norm2_g[l]), sh2, sc2)
        y = _hier_moe(h2.reshape(B * L, D), moe_w_grp[l], moe_b_grp[l], moe_w_exp[l], moe_b_exp[l],
                      moe_w_gate[l], moe_w_up[l], moe_w_down[l]).reshape(B, L, D)
        x_new = x_new + gt2 * y

        if l + 1 < DEPTH:
            qa_c = _head_rms(qa_c.reshape(B, CL, ATTN_HEADS, HEAD_DIM), attn_q_norm[l])
            attn_c = _attend(qa_c, ka_c, va_c)
            ret_c = _ret_output(_retention_context(q_rc, k_rc, v_rc, log_gf, log_gb),
                                ret_gn_g[l], ret_gn_b[l], gr_c, ctx.dtype)
            ctx_new = ctx + cgt1 * (jnp.concatenate([attn_c, ret_c], axis=-1) @ w_out[l])
            h2c = _modulate(_rmsnorm(ctx_new, norm2_g[l]), csh2, csc2)
            yc = _hier_moe(h2c.reshape(B * CL, D), moe_w_grp[l], moe_b_grp[l], moe_w_exp[l], moe_b_exp[l],
                           moe_w_gate[l], moe_w_up[l], moe_w_down[l]).reshape(B, CL, D)
            ctx = ctx_new + cgt2 * yc
        x = x_new

    return _rmsnorm(x, final_norm_g)
```

```python
import numpy as np
from contextlib import ExitStack
import concourse.bass as bass
import concourse.mybir as mybir
from concourse.bass_utils import run_bass_kernel_spmd

F32 = mybir.dt.float32
BF16 = mybir.dt.bfloat16
I32 = mybir.dt.int32
U32 = mybir.dt.uint32
AF = mybir.ActivationFunctionType
ALU = mybir.AluOpType
AX = mybir.AxisListType

ENGS = ("pe", "act", "dve", "pool", "sp")
NCORES = 8
D = 1024
EPS = 1e-6


import types


def _freeze(fn):
    if fn is None or fn.__closure__ is None:
        return fn
    cells = []
    for c in fn.__closure__:
        try:
            cells.append(types.CellType(c.cell_contents))
        except ValueError:
            cells.append(c)
    return types.FunctionType(fn.__code__, fn.__globals__, fn.__name__, fn.__defaults__, tuple(cells))


class Res:
    __slots__ = ("name", "w", "rs", "dsem", "dcount", "excl")

    def __init__(self, name, excl=False):
        self.name = name
        self.excl = excl
        self.w = {}
        self.rs = {}
        self.dsem = None
        self.dcount = 0


class Prog:
    def __init__(self, nc):
        self.nc = nc
        self.ops = {e: [] for e in ENGS}
        self.cnt = {e: 0 for e in ENGS}
        self.known = {e: {} for e in ENGS}
        self.semnames = []
        self.dres = []

    def _deps(self, eng, reads, writes):
        best = {}

        def add(d):
            for s, v in d.items():
                if v > best.get(s, 0):
                    best[s] = v
        for r in reads:
            add(r.w)
        for w in writes:
            add(w.w)
            add(w.rs)
        waits = []
        kn = self.known[eng]
        for s, v in best.items():
            if kn.get(s, 0) < v:
                kn[s] = v
                waits.append((s, v))
        return waits

    def op(self, eng, fn, reads=(), writes=(), inc=True, pe_acc=False):
        fn = _freeze(fn)
        xr = [r for r in reads if r.excl]
        if xr:
            writes = list(writes) + xr
            reads = [r for r in reads if not r.excl]
        waits = self._deps(eng, reads, writes)
        if pe_acc:
            waits = [(s, v) for (s, v) in waits if s != "E_pe"]
        sem = "E_" + eng
        val = self.cnt[eng] + 1
        if inc:
            self.cnt[eng] = val
        self.ops[eng].append(("op", fn, waits, sem if inc else None))
        for r in reads:
            if r.rs.get(sem, 0) < val:
                r.rs[sem] = val
        for w in writes:
            w.w = {sem: val}
            w.rs = {}
        return (sem, val)

    def dma(self, eng, fn, dst, reads=(), no_waw=False, semres=None):
        fn = _freeze(fn)
        owner = semres if semres is not None else dst
        wl = [] if no_waw else [dst]
        if semres is not None:
            wl.append(semres)
        rl = list(reads)
        waits = self._deps(eng, rl, wl)
        if owner.dsem is None:
            owner.dsem = "D%d_%s" % (len(self.semnames), owner.name)
            self.semnames.append(owner.dsem)
            self.dres.append(owner)
        owner.dcount += 16
        sem, val = owner.dsem, owner.dcount
        self.ops[eng].append(("dma", fn, waits, sem))
        for r in rl:
            if r.rs.get(sem, 0) < val:
                r.rs[sem] = val
        if semres is not None:
            semres.rs[sem] = val
        if no_waw:
            dst.w[sem] = val
        else:
            dst.w = {sem: val}
            dst.rs = {}
        return (sem, val)

    def wait_all(self, eng, ress):
        waits = self._deps(eng, ress, [])
        self.ops[eng].append(("wait", None, waits, None))

    def barrier(self):
        evs = [("E_" + e, self.cnt[e]) for e in ENGS if self.cnt[e] > 0]
        evs += [(r.dsem, r.dcount) for r in self.dres]
        for e in ENGS:
            kn = self.known[e]
            waits = []
            for (s, v) in evs:
                if kn.get(s, 0) < v:
                    kn[s] = v
                    waits.append((s, v))
            if waits:
                self.ops[e].append(("wait", None, waits, None))

    def emit(self, stack):
        nc = self.nc
        sems = {}
        for e in ENGS:
            sems["E_" + e] = stack.enter_context(nc.semaphore("E_" + e))
        for n in self.semnames:
            sems[n] = stack.enter_context(nc.semaphore(n))
        block = stack.enter_context(nc.Block())
        engmap = {"pe": block.tensor, "act": block.scalar, "dve": block.vector,
                  "pool": block.gpsimd, "sp": block.sync}
        for e in ENGS:
            ops = self.ops[e]
            if not ops:
                continue

            def body(engine, ops=ops):
                for kind, fn, waits, sem in ops:
                    for (s, v) in waits:
                        engine.wait_ge(sems[s], v)
                    if kind == "op":
                        ins = fn(engine)
                        if sem is not None:
                            ins.then_inc(sems[sem], 1)
                    elif kind == "dma":
                        ins = fn(engine)
                        ins.then_inc(sems[sem], 16)
            engmap[e](body)


class Arena:
    def __init__(self, t, nwords, start=0):
        self.t = t
        self.n = nwords
        self.top = start

    def alloc(self, shape, dt):
        esz = 2 if dt == BF16 else 4
        nel = int(np.prod(shape))
        words = (nel * esz + 3) // 4
        words = (words + 7) // 8 * 8
        assert self.top + words <= self.n, ("arena overflow", self.top, words, self.n)
        ap = self.t[:, self.top:self.top + words]
        self.top += words
        if dt != F32:
            ap = ap.bitcast(dt)
        ap = ap[:, 0:nel]
        if len(shape) == 2:
            ap = ap.rearrange("p (a b) -> p a b", a=shape[0])
        elif len(shape) == 3:
            ap = ap.rearrange("p (a b c) -> p a b c", a=shape[0], b=shape[1])
        return ap

    def mark(self):
        return self.top

    def release(self, m):
        self.top = m


class _Stop(Exception):
    pass


def build_program(NT, debug=False, stop=99):
    import os
    SUB = int(os.environ.get('KSUB', '0'))
    TPC = NT // NCORES
    NS = NT + 2
    nc = bass.Bass("TRN2", target_bir_lowering=False)

    def din(name, shape, dt=F32):
        return nc.dram_tensor(name, shape, dt, kind="ExternalInput").ap()

    xp = din("xp", [NS * 128, D])
    cvecT = din("cvecT", [128, 16])
    w_ada = din("w_ada", [D, 6 * D])
    b_ada = din("b_ada", [1, 6 * D])
    norm1_g = din("norm1_g", [1, D])
    norm2_g = din("norm2_g", [1, D])
    final_g = din("final_g", [1, D])
    w_in = din("w_in", [D, 2304])
    qn = din("qn", [1, 64])
    kn = din("kn", [1, 64])
    dec = din("dec", [1, 8])
    gn_g = din("gn_g", [1, 512])
    gn_b = din("gn_b", [1, 512])
    w_out = din("w_out", [D, D])
    w_rt = din("w_rt", [D, 36])
    b_rt = din("b_rt", [1, 36])
    NEXP = 32 if stop >= 6 else 1
    wg = din("wg", [NEXP, D, 512])
    wu = din("wu", [NEXP, D, 512])
    wd = din("wd", [NEXP, 512, D])
    cos_t = din("cos_t", [NS * 128, 32])
    sin_t = din("sin_t", [NS * 128, 32])
    dist_t = din("dist_t", [128, 4 * NS])
    dmat_t = din("dmat_t", [128, 256])
    pcol_t = din("pcol_t", [128, 2])
    out = nc.dram_tensor("out", [TPC * 128, D], F32, kind="ExternalOutput").ap()
    dbg = {}
    if debug:
        for nm, shp in [("d_hx", [128, D]), ("d_attn", [TPC * 128, 512]), ("d_ret", [TPC * 128, 512]),
                        ("d_xnew", [TPC * 128, D]), ("d_wtok", [TPC * 128, 32]), ("d_g1", [128, D]),
                        ("d_kt", [128, NS * 128]), ("d_v1", [128, NS * 130]), ("d_qt", [128, TPC * 512])]:
            dbg[nm] = nc.dram_tensor(nm, shp, F32, kind="ExternalOutput").ap()

    KT_d = nc.dram_tensor("KT_d", [128, NS * 128], BF16).ap()
    V_d = nc.dram_tensor("V_d", [NS, 128, 130], BF16).ap()
    XN_d = nc.dram_tensor("XN_d", [TPC * 128, D], F32).ap()
    r_KTd = Res("KTd"); r_Vd = Res("Vd"); r_XNd = Res("XNd"); r_out = Res("out"); r_dbg = Res("dbg")

    P = Prog(nc)
    st = ExitStack()
    with st:
        ARW = 51 * 1024
        arena_t = st.enter_context(nc.sbuf_tensor("arena", [128, ARW], F32))
        A = Arena(arena_t, ARW)
        banks = [st.enter_context(nc.psum_tensor("bank%d" % i, [128, 512], F32)) for i in range(8)]
        rb = [Res("bank%d" % i, excl=True) for i in range(8)]

        def bank_bf(i):
            return banks[i][:].bitcast(BF16)

        def V(eng, fn, reads, writes, **kw):
            return P.op(eng, fn, reads, writes, **kw)

        def mm_group(ps_ap, ps_res, pairs, reads):
            n = len(pairs)
            for i, (l, r) in enumerate(pairs):
                P.op("pe", lambda e, l=l, r=r, i=i: e.matmul(ps_ap, l, r, start=(i == 0), stop=(i == n - 1)),
                     reads, [ps_res], inc=(i == n - 1), pe_acc=(i > 0))

        def body():
            nonlocal A
            ident_f = A.alloc([128], F32); r_idf = Res("idf")
            ident_b = A.alloc([128], BF16); r_idb = Res("idb")
            V("pool", lambda e: e.memset(ident_f, 0.0), [], [r_idf])
            V("pool", lambda e: e.affine_select(out=ident_f, in_=ident_f, pattern=[[-1, 128]], compare_op=ALU.not_equal,
                                                fill=1.0, base=0, channel_multiplier=1), [r_idf], [r_idf])
            V("dve", lambda e: e.tensor_copy(out=ident_b, in_=ident_f), [r_idf], [r_idb])

            def bload(dram_row, n, name):
                t = A.alloc([n], F32); r = Res(name)
                P.dma("sp", lambda e: e.dma_start(out=t, in_=dram_row.partition_broadcast(128)), r)
                return t, r

            GT1 = A.alloc([D], F32); r_GT1 = Res("GT1")
            G2 = A.alloc([D], F32); r_G2 = Res("G2")
            SH2 = A.alloc([D], F32); r_SH2 = Res("SH2")
            GT2 = A.alloc([D], F32); r_GT2 = Res("GT2")
            dec_bc, r_dec = bload(dec, 8, "dec")
            qn_bc, r_qn = bload(qn, 64, "qn")
            kn_bc, r_kn = bload(kn, 64, "kn")
            lg = A.alloc([8], F32); r_lg = Res("lg")
            V("act", lambda e: e.activation(out=lg, in_=dec_bc, func=AF.Exp, scale=-1.0), [r_dec], [r_lg])
            V("act", lambda e: e.activation(out=lg, in_=lg, func=AF.Ln, bias=1.0, scale=1.0), [r_lg], [r_lg])
            V("dve", lambda e: e.tensor_scalar(out=lg, in0=lg, scalar1=-1.0, scalar2=None, op0=ALU.mult), [r_lg], [r_lg])
            negC = A.alloc([1], F32); r_negC = Res("negC")
            mk_ = A.alloc([1], F32); r_mk = Res("mk")
            V("dve", lambda e: e.tensor_reduce(out=negC, in_=qn_bc, axis=AX.X, op=ALU.max, apply_absolute_value=True), [r_qn], [r_negC])
            V("dve", lambda e: e.tensor_reduce(out=mk_, in_=kn_bc, axis=AX.X, op=ALU.max, apply_absolute_value=True), [r_kn], [r_mk])
            V("dve", lambda e: e.tensor_tensor(out=negC, in0=negC, in1=mk_, op=ALU.mult), [r_negC, r_mk], [r_negC])
            V("dve", lambda e: e.tensor_scalar(out=negC, in0=negC, scalar1=-8.0, scalar2=None, op0=ALU.mult), [r_negC], [r_negC])
            Wtok = A.alloc([TPC, 32], F32); r_Wtok = [Res("Wtok%d" % i) for i in range(TPC)]
            m_scrA0 = A.mark()
            actT = A.alloc([TPC * 1024], BF16)
            hxT_own = actT.rearrange("p (c x) -> p c x", c=TPC); r_hxT_own = [Res("hxTo%d" % i) for i in range(TPC)]
            h2T = actT.rearrange("p (j t) -> p j t", j=8); r_h2T = [Res("h2T%d" % i) for i in range(TPC)]
            m_moe = A.mark()
            retx = A.alloc([TPC, 512], BF16); r_retx = [Res("retx%d" % i) for i in range(TPC)]
            QT = A.alloc([TPC, 512], BF16); r_QT = Res("QT")
            m_persist = A.mark()
            m_scrA1 = A.mark()
            w_in_sb = A.alloc([8, 2304], BF16); r_win = Res("win")
            w_in_v = w_in.rearrange("(j p) n -> p j n", p=128)
            for c0 in range(0, 2304, 256):
                P.dma("pool", lambda e, c0=c0: e.dma_start(out=w_in_sb[:, :, c0:c0 + 256], in_=w_in_v[:, :, c0:c0 + 256]), r_win, no_waw=True)
            Ust = A.alloc([TPC, 512], BF16); r_Ust = [Res("Ust%d" % i) for i in range(TPC)]
            Sacc = A.alloc([512], F32); r_Sacc = Res("Sacc")
            V("pool", lambda e: e.memset(Sacc, 0.0), [], [r_Sacc])
            ri = A.alloc([14, 64], F32); r_ri = Res("ri")
            ro = A.alloc([14, 64], F32); r_ro = Res("ro")
            rt = [A.alloc([14, 32], F32) for _ in range(2)]; r_rt = [Res("rt%d" % i) for i in range(2)]
            m_sw = A.mark()

            G1 = A.alloc([D], F32); r_G1 = Res("G1")
            SH1 = A.alloc([D], F32); r_SH1 = Res("SH1")
            G1c = A.alloc([D], F32); r_G1c = Res("G1c")
            SH1c = A.alloc([D], F32); r_SH1c = Res("SH1c")
            A_main = A
            if m_scrA1 - m_scrA0 >= 15000:
                A = Arena(arena_t, m_scrA1, start=m_scrA0)
            else:
                A = Arena(arena_t, ARW, start=A_main.top)
            cv = A.alloc([16], F32); r_cv = Res("cv")
            P.dma("sp", lambda e: e.dma_start(out=cv, in_=cvecT[:, :]), r_cv)
            V("act", lambda e: e.activation(out=cv, in_=cv, func=AF.Silu), [r_cv], [r_cv])
            rep = A.alloc([16, 128], F32); r_rep = Res("rep")
            V("dve", lambda e: e.tensor_copy(out=rep, in_=cv.unsqueeze(2).to_broadcast([128, 16, 128])), [r_cv], [r_rep])
            bada2 = [A.alloc([512], F32) for _ in range(2)]; r_bada2 = [Res("bada0"), Res("bada1")]
            n1g, r_n1g = bload(norm1_g, D, "n1g")
            n2g, r_n2g = bload(norm2_g, D, "n2g")
            wst = [A.alloc([8, 512], F32) for _ in range(2)]; r_wst = [Res("wst0"), Res("wst1")]
            tmpA = A.alloc([512], F32); r_tmpA = Res("tmpA")
            w_ada_v = w_ada.rearrange("(j p) n -> p j n", p=128)
            plan = {0: ("sh", SH1, r_SH1, SH1c, r_SH1c, None), 1: ("sh", SH1, r_SH1, SH1c, r_SH1c, None),
                    2: ("sc", G1, r_G1, G1c, r_G1c, (n1g, r_n1g)), 3: ("sc", G1, r_G1, G1c, r_G1c, (n1g, r_n1g)),
                    4: ("sh", GT1, r_GT1, None, None, None), 5: ("sh", GT1, r_GT1, None, None, None),
                    6: ("sh", SH2, r_SH2, None, None, None), 7: ("sh", SH2, r_SH2, None, None, None),
                    8: ("sc", G2, r_G2, None, None, (n2g, r_n2g)), 9: ("sc", G2, r_G2, None, None, (n2g, r_n2g)),
                    10: ("sh", GT2, r_GT2, None, None, None), 11: ("sh", GT2, r_GT2, None, None, None)}
            for cb in range(12):
                kind, dst, r_dst, dstc, r_dstc, gg = plan[cb]
                wb = wst[cb % 2]; r_wb = r_wst[cb % 2]
                P.dma("sp" if cb % 2 == 0 else "act", lambda e, wb=wb, cb=cb: e.dma_start(out=wb, in_=w_ada_v[:, :, cb * 512:(cb + 1) * 512]), r_wb)
                bada = bada2[cb % 2]; r_bada = r_bada2[cb % 2]
                P.dma("sp", lambda e, bada=bada, cb=cb: e.dma_start(out=bada, in_=b_ada[:, cb * 512:(cb + 1) * 512].partition_broadcast(128)), r_bada)
                half = (cb % 2) * 512
                for which in range(2):
                    if which == 1 and dstc is None:
                        continue
                    bk = (cb * 2 + which) % 4
                    mm_group(banks[bk][:], rb[bk], [(rep[:, which * 8 + j, :], wb[:, j, :]) for j in range(8)], [r_rep, r_wb])
                    d_ap = (dst if which == 0 else dstc)[:, half:half + 512]
                    r_d = r_dst if which == 0 else r_dstc
                    bslice = bada
                    if kind == "sh":
                        V("dve", lambda e, bk=bk, d_ap=d_ap, bslice=bslice: e.tensor_tensor(out=d_ap, in0=banks[bk][:], in1=bslice, op=ALU.add),
                          [rb[bk], r_bada], [r_d])
                    else:
                        g_ap = gg[0][:, half:half + 512]
                        V("dve", lambda e, bk=bk, bslice=bslice: e.tensor_tensor(out=tmpA, in0=banks[bk][:], in1=bslice, op=ALU.add),
                          [rb[bk], r_bada], [r_tmpA])
                        V("dve", lambda e, d_ap=d_ap, g_ap=g_ap: e.scalar_tensor_tensor(out=d_ap, in0=tmpA, scalar=1.0, in1=g_ap, op0=ALU.add, op1=ALU.mult),
                          [r_tmpA, gg[1]], [r_d])
            if debug:
                P.dma("sp", lambda e: e.dma_start(out=dbg["d_g1"], in_=G1), r_dbg, [r_G1], no_waw=True, semres=r_G1)
            P.barrier()
            if stop == 1: raise _Stop()
            A = A_main

            distt = A.alloc([4 * NS], F32); r_distt = Res("distt")
            P.dma("sp", lambda e: e.dma_start(out=distt, in_=dist_t[:, :]), r_distt)
            Wf = A.alloc([NS, 4], F32); r_Wf = Res("Wf")
            Wb = A.alloc([NS, 4], F32); r_Wb = Res("Wb")
            for h in range(4):
                V("act", lambda e, h=h: e.activation(out=Wf[:, :, h], in_=distt[:, 0:NS], func=AF.Exp, scale=lg[:, h:h + 1]), [r_distt, r_lg], [r_Wf])
                V("act", lambda e, h=h: e.activation(out=Wb[:, :, h], in_=distt[:, NS:2 * NS], func=AF.Exp, scale=lg[:, 4 + h:5 + h]), [r_distt, r_lg], [r_Wb])
            V("dve", lambda e: e.tensor_tensor(out=Wf, in0=Wf, in1=distt[:, 2 * NS:3 * NS].unsqueeze(2).to_broadcast([128, NS, 4]), op=ALU.mult), [r_Wf, r_distt], [r_Wf])
            V("dve", lambda e: e.tensor_tensor(out=Wb, in0=Wb, in1=distt[:, 3 * NS:4 * NS].unsqueeze(2).to_broadcast([128, NS, 4]), op=ALU.mult), [r_Wb, r_distt], [r_Wb])

            NB = 2
            xt = [A.alloc([D], F32) for _ in range(NB)]; r_xt = [Res("xt%d" % i) for i in range(NB)]
            cst = [A.alloc([64], F32) for _ in range(NB)]; r_cst = [Res("cst%d" % i) for i in range(NB)]
            tmpx = A.alloc([D], F32); r_tmpx = Res("tmpx")
            junk = tmpx; r_junk = r_tmpx
            hx1 = A.alloc([D], BF16); hx = [hx1, hx1]; r_hx1 = Res("hx"); r_hx = [r_hx1, r_hx1]
            hxTw1 = A.alloc([8 * 128], BF16); hxTw = [hxTw1, hxTw1]; r_hxTw1 = Res("hxTw"); r_hxTw = [r_hxTw1, r_hxTw1]
            sm = [A.alloc([32], F32) for _ in range(NB)]; r_sm = [Res("sm%d" % i) for i in range(NB)]
            sqh = A.alloc([640], F32); r_sqh = Res("sqh")
            kb = A.alloc([128], BF16); r_kb = Res("kb")
            qb = A.alloc([8, 64], BF16); r_qb = Res("qb")
            ktb = [A.alloc([128], BF16) for _ in range(NB)]; r_ktb = [Res("ktb%d" % i) for i in range(NB)]
            v1 = [A.alloc([2, 65], BF16) for _ in range(NB)]; r_v1 = [Res("v1%d" % i) for i in range(NB)]
            for i in range(NB):
                V("pool", lambda e, i=i: e.memset(v1[i], 1.0), [], [r_v1[i]])
            kaug = A.alloc([4, 128], BF16); r_kaug = Res("kaug")
            vb = A.alloc([512], BF16); r_vb = Res("vb")

            def rope(H0, H1, cs, r_cs):
                H = H1 - H0
                riv = ri[:, H0:H1, :].rearrange("p h (i t) -> p h i t", t=2)
                rov = ro[:, H0:H1, :].rearrange("p h (i t) -> p h i t", t=2)
                cb_ = cs[:, 0:32].unsqueeze(1).to_broadcast([128, H, 32])
                sb_ = cs[:, 32:64].unsqueeze(1).to_broadcast([128, H, 32])
                t = [rt[i][:, 0:H, :] for i in range(2)]
                V("dve", lambda e: e.tensor_tensor(out=t[0], in0=riv[:, :, :, 0], in1=cb_, op=ALU.mult), [r_ri, r_cs], [r_rt[0]])
                V("dve", lambda e: e.tensor_tensor(out=t[1], in0=riv[:, :, :, 1], in1=sb_, op=ALU.mult), [r_ri, r_cs], [r_rt[1]])
                V("dve", lambda e: e.tensor_tensor(out=rov[:, :, :, 0], in0=t[0], in1=t[1], op=ALU.subtract), [r_rt[0], r_rt[1]], [r_ro])
                V("dve", lambda e: e.tensor_tensor(out=t[0], in0=riv[:, :, :, 0], in1=sb_, op=ALU.mult), [r_ri, r_cs], [r_rt[0]])
                V("dve", lambda e: e.tensor_tensor(out=t[1], in0=riv[:, :, :, 1], in1=cb_, op=ALU.mult), [r_ri, r_cs], [r_rt[1]])
                V("dve", lambda e: e.tensor_tensor(out=rov[:, :, :, 1], in0=t[0], in1=t[1], op=ALU.add), [r_rt[0], r_rt[1]], [r_ro])

            def rsqrt_mean(dst, src, r_dst, r_src, n):
                V("act", lambda e: e.activation(out=dst, in_=src, func=AF.Ln, scale=1.0 / n, bias=EPS), [r_src], [r_dst])
                V("act", lambda e: e.activation(out=dst, in_=dst, func=AF.Exp, scale=-0.5), [r_dst], [r_dst])

            def norm_mod_transpose(b, x_ap, r_x, Gm, r_Gm, SHm, r_SHm, dstT, r_dstT, tb):
                s_ = sm[b]; r_s = r_sm[b]
                V("act", lambda e: e.activation(out=junk, in_=x_ap, func=AF.Square, accum_out=s_[:, 0:1]), [r_x], [r_junk, r_s])
                rsqrt_mean(s_[:, 1:2], s_[:, 0:1], r_s, r_s, D)
                V("dve", lambda e: e.scalar_tensor_tensor(out=tmpx, in0=x_ap, scalar=s_[:, 1:2], in1=Gm, op0=ALU.mult, op1=ALU.mult),
                  [r_x, r_s, r_Gm], [r_tmpx])
                V("pool", lambda e: e.tensor_tensor(out=hx[b], in0=tmpx, in1=SHm, op=ALU.add), [r_tmpx, r_SHm], [r_hx[b]])
                pt = bank_bf(tb)
                for j in range(8):
                    V("pe", lambda e, j=j: e.transpose(out=pt[:, j * 128:(j + 1) * 128], in_=hx[b][:, j * 128:(j + 1) * 128], identity=ident_b),
                      [r_hx[b], r_idb], [rb[tb]], inc=(j == 7), pe_acc=(j > 0))
                V("act", lambda e: e.activation(out=dstT, in_=pt, func=AF.Copy), [rb[tb]], [r_dstT])

            xpv = xp.rearrange("(s p) n -> s p n", p=128)
            cosv = cos_t.rearrange("(s p) n -> s p n", p=128)
            sinv = sin_t.rearrange("(s p) n -> s p n", p=128)

            for s in range(NS):
                b = s % NB
                own = s < TPC
                isctx = s >= NT
                P.dma("sp", lambda e, s=s, b=b: e.dma_start(out=xt[b], in_=xpv[s]), r_xt[b])
                P.dma("sp", lambda e, s=s, b=b: e.dma_start(out=cst[b][:, 0:32], in_=cosv[s]), r_cst[b])
                P.dma("sp", lambda e, s=s, b=b: e.dma_start(out=cst[b][:, 32:64], in_=sinv[s]), r_cst[b])
                if own:
                    dstT = hxT_own[:, s, :]; r_dT = r_hxT_own[s]
                else:
                    dstT = hxTw[b]; r_dT = r_hxTw[b]
                norm_mod_transpose(b, xt[b], r_xt[b], G1c if isctx else G1, r_G1c if isctx else r_G1,
                                   SH1c if isctx else SH1, r_SH1c if isctx else r_SH1, dstT, r_dT, tb=b)
                if debug and s == 0:
                    V("dve", lambda e: e.tensor_copy(out=tmpx, in_=hx[0]), [r_hx[0]], [r_tmpx])
                    P.dma("sp", lambda e: e.dma_start(out=dbg["d_hx"], in_=tmpx), r_dbg, [r_tmpx], no_waw=True, semres=r_tmpx)
                hT = dstT.rearrange("p (j t) -> p j t", j=8)
                mm_group(banks[2][:, 0:256], rb[2], [(hT[:, j, :], w_in_sb[:, j, 512:768]) for j in range(8)], [r_dT, r_win])
                mm_group(banks[2][:, 256:512], rb[2], [(hT[:, j, :], w_in_sb[:, j, 1536:1792]) for j in range(8)], [r_dT, r_win])
                mm_group(banks[3][:], rb[3], [(hT[:, j, :], w_in_sb[:, j, 1024:1536]) for j in range(8)], [r_dT, r_win])
                if own:
                    mm_group(banks[4][:], rb[4], [(hT[:, j, :], w_in_sb[:, j, 0:512]) for j in range(8)], [r_dT, r_win])
                s_ = sm[b]; r_s = r_sm[b]
                H0 = 0 if own else 8
                V("act", lambda e: e.activation(out=sqh[:, 512:640], in_=banks[2][:, 0:128], func=AF.Square), [rb[2]], [r_sqh])
                if own:
                    V("act", lambda e: e.activation(out=sqh[:, 0:512], in_=banks[4][:], func=AF.Square), [rb[4]], [r_sqh])
                nh = 10 - H0
                hs = s_[:, 8 + H0:18]
                V("dve", lambda e: e.tensor_reduce(out=hs, in_=sqh[:, H0 * 64:640].rearrange("p (h d) -> p h d", d=64), axis=AX.X, op=ALU.add),
                  [r_sqh], [r_s])
                rsqrt_mean(hs, hs, r_s, r_s, 64)
                V("dve", lambda e: e.tensor_tensor(out=ri[:, 8:10, :], in0=banks[2][:, 0:128].rearrange("p (h d) -> p h d", d=64),
                                                   in1=s_[:, 16:18].unsqueeze(2).to_broadcast([128, 2, 64]), op=ALU.mult), [rb[2], r_s], [r_ri])
                V("dve", lambda e: e.tensor_tensor(out=ri[:, 8:10, :], in0=ri[:, 8:10, :], in1=kn_bc.unsqueeze(1).to_broadcast([128, 2, 64]), op=ALU.mult),
                  [r_ri, r_kn], [r_ri])
                if own:
                    V("dve", lambda e: e.tensor_tensor(out=ri[:, 0:8, :], in0=banks[4][:].rearrange("p (h d) -> p h d", d=64),
                                                       in1=s_[:, 8:16].unsqueeze(2).to_broadcast([128, 8, 64]), op=ALU.mult), [rb[4], r_s], [r_ri])
                    V("dve", lambda e: e.tensor_tensor(out=ri[:, 0:8, :], in0=ri[:, 0:8, :], in1=qn_bc.unsqueeze(1).to_broadcast([128, 8, 64]), op=ALU.mult),
                      [r_ri, r_qn], [r_ri])
                V("act", lambda e: e.activation(out=ri[:, 10:14, :], in_=banks[3][:, 0:256].rearrange("p (h d) -> p h d", d=64), func=AF.Copy, scale=0.125),
                  [rb[3]], [r_ri])
                rope(H0, 14, cst[b], r_cst[b])
                V("dve", lambda e: e.tensor_copy(out=kb, in_=ro[:, 8:10, :]), [r_ro], [r_kb])
                ptk = bank_bf(5)
                V("pe", lambda e: e.transpose(out=ptk[:, 0:128], in_=kb, identity=ident_b), [r_kb, r_idb], [rb[5]])
                V("act", lambda e, b=b: e.activation(out=ktb[b], in_=ptk[:, 0:128], func=AF.Copy), [rb[5]], [r_ktb[b]])
                P.dma("pool", lambda e, s=s, b=b: e.dma_start(out=KT_d[:, s * 128:(s + 1) * 128], in_=ktb[b]), r_KTd, [r_ktb[b]], no_waw=True, semres=r_ktb[b])
                V("act", lambda e, b=b: e.activation(out=v1[b][:, :, 0:64], in_=banks[2][:, 128:256].rearrange("p (h d) -> p h d", d=64), func=AF.Copy),
                  [rb[2]], [r_v1[b]])
                P.dma("pool", lambda e, s=s, b=b: e.dma_start(out=V_d[s], in_=v1[b]), r_Vd, [r_v1[b]], no_waw=True, semres=r_v1[b])
                if own:
                    ptq = bank_bf(6)
                    V("dve", lambda e: e.tensor_copy(out=qb, in_=ro[:, 0:8, :]), [r_ro], [r_qb])
                    for g in range(4):
                        V("pe", lambda e, g=g: e.transpose(out=ptq[0:64, g * 128:(g + 1) * 128], in_=qb[:, g, :], identity=ident_b),
                          [r_qb, r_idb], [rb[6]], inc=False, pe_acc=(g > 0))
                    for g in range(4):
                        V("pe", lambda e, g=g: e.transpose(out=ptq[64:128, g * 128:(g + 1) * 128], in_=qb[:, 4 + g, :], identity=ident_b),
                          [r_qb, r_idb], [rb[6]], inc=(g == 3), pe_acc=True)
                    V("act", lambda e, s=s: e.activation(out=QT[:, s, :], in_=ptq[:, 0:512], func=AF.Copy), [rb[6]], [r_QT])
                V("dve", lambda e, s=s: e.tensor_tensor(out=kaug[:, :, 0:64], in0=ro[:, 10:14, :], in1=Wf[:, s, :].unsqueeze(2).to_broadcast([128, 4, 64]), op=ALU.mult),
                  [r_ro, r_Wf], [r_kaug])
                V("dve", lambda e, s=s: e.tensor_tensor(out=kaug[:, :, 64:128], in0=ro[:, 10:14, :], in1=Wb[:, s, :].unsqueeze(2).to_broadcast([128, 4, 64]), op=ALU.mult),
                  [r_ro, r_Wb], [r_kaug])
                V("act", lambda e: e.activation(out=vb[:, 0:256], in_=banks[3][:, 256:512], func=AF.Copy), [rb[3]], [r_vb])
                V("act", lambda e: e.activation(out=vb[:, 256:512], in_=banks[2][:, 256:512], func=AF.Copy), [rb[2]], [r_vb])
                for h in range(4):
                    P.op("pe", lambda e, h=h: e.matmul(banks[7][:, h * 128:(h + 1) * 128], kaug[:, h, :], vb[:, h * 128:(h + 1) * 128], start=True, stop=True),
                         [r_kaug, r_vb], [rb[7]], inc=(h == 3), pe_acc=(h > 0))
                if own:
                    V("dve", lambda e, s=s: e.tensor_copy(out=Ust[:, s, :], in_=banks[7][:]), [rb[7]], [r_Ust[s]])
                else:
                    V("dve", lambda e: e.tensor_tensor(out=Sacc, in0=Sacc, in1=banks[7][:], op=ALU.add), [r_Sacc, rb[7]], [r_Sacc])

            P.barrier()
            if stop == 2: raise _Stop()
            A.release(m_sw)

            dmt = A.alloc([256], F32); r_dmt = Res("dmt")
            P.dma("sp", lambda e: e.dma_start(out=dmt, in_=dmat_t[:, :]), r_dmt)
            pct = A.alloc([2], F32); r_pct = Res("pct")
            P.dma("sp", lambda e: e.dma_start(out=pct, in_=pcol_t[:, :]), r_pct)
            DT = A.alloc([4, 128], F32); r_DT = Res("DT")
            tmpd = A.alloc([128], F32); r_tmpd = Res("tmpd")
            for h in range(4):
                V("dve", lambda e, h=h: e.tensor_scalar(out=tmpd, in0=dmt[:, 0:128], scalar1=lg[:, h:h + 1], scalar2=None, op0=ALU.mult), [r_dmt, r_lg], [r_tmpd])
                V("dve", lambda e, h=h: e.scalar_tensor_tensor(out=tmpd, in0=dmt[:, 128:256], scalar=lg[:, 4 + h:5 + h], in1=tmpd, op0=ALU.mult, op1=ALU.add),
                  [r_dmt, r_lg, r_tmpd], [r_tmpd])
                V("act", lambda e, h=h: e.activation(out=DT[:, h, :], in_=tmpd, func=AF.Exp), [r_tmpd], [r_DT])
            xif = A.alloc([4], F32); xib = A.alloc([4], F32); r_xi = Res("xi")
            g128 = A.alloc([4], F32); r_g128 = Res("g128")
            for h in range(4):
                V("act", lambda e, h=h: e.activation(out=xif[:, h:h + 1], in_=pct[:, 0:1], func=AF.Exp, scale=lg[:, h:h + 1]), [r_pct, r_lg], [r_xi])
                V("act", lambda e, h=h: e.activation(out=xib[:, h:h + 1], in_=pct[:, 1:2], func=AF.Exp, scale=lg[:, 4 + h:5 + h]), [r_pct, r_lg], [r_xi])
            V("act", lambda e: e.activation(out=g128[0:64, :], in_=lg[0:64, 0:4], func=AF.Exp, scale=128.0), [r_lg], [r_g128])
            V("act", lambda e: e.activation(out=g128[64:128, :], in_=lg[64:128, 4:8], func=AF.Exp, scale=128.0), [r_lg], [r_g128])
            if SUB == 1: raise _Stop()
            Sst = A.alloc([TPC, 512], BF16); r_Sst = [Res("Sst%d" % i) for i in range(TPC)]
            cur = A.alloc([512], F32); r_cur = Res("cur")
            V("dve", lambda e: e.tensor_copy(out=cur, in_=Sacc), [r_Sacc], [r_cur])
            for c in range(TPC):
                V("dve", lambda e, c=c: e.tensor_copy(out=Sst[0:64, c, :], in_=cur[0:64, :]), [r_cur], [r_Sst[c]])
                for h in range(4):
                    V("dve", lambda e, c=c, h=h: e.scalar_tensor_tensor(out=cur[0:64, h * 128:(h + 1) * 128], in0=cur[0:64, h * 128:(h + 1) * 128],
                                                                       scalar=g128[0:64, h:h + 1], in1=Ust[0:64, c, h * 128:(h + 1) * 128],
                                                                       op0=ALU.mult, op1=ALU.add), [r_cur, r_g128, r_Ust[c]], [r_cur])
            for c in range(TPC - 1, -1, -1):
                V("dve", lambda e, c=c: e.tensor_copy(out=Sst[64:128, c, :], in_=cur[64:128, :]), [r_cur], [r_Sst[c]])
                for h in range(4):
                    V("dve", lambda e, c=c, h=h: e.scalar_tensor_tensor(out=cur[64:128, h * 128:(h + 1) * 128], in0=cur[64:128, h * 128:(h + 1) * 128],
                                                                       scalar=g128[64:128, h:h + 1], in1=Ust[64:128, c, h * 128:(h + 1) * 128],
                                                                       op0=ALU.mult, op1=ALU.add), [r_cur, r_g128, r_Ust[c]], [r_cur])
            if SUB == 2: raise _Stop()
            gng, r_gng = bload(gn_g, 512, "gng")
            gnb, r_gnb = bload(gn_b, 512, "gnb")
            cs2 = [A.alloc([64], F32) for _ in range(2)]; r_cs2 = [Res("cs2_0"), Res("cs2_1")]
            qk = A.alloc([8, 64], BF16); r_qk = Res("qk")
            qaug = A.alloc([4, 128], BF16); r_qaug = Res("qaug")
            qkT = A.alloc([4, 128], BF16); r_qkT = Res("qkT")
            qaT = A.alloc([4, 128], BF16); r_qaT = Res("qaT")
            qpad = A.alloc([4, 128], BF16); r_qpad = Res("qpad")
            V("pool", lambda e: e.memset(qpad, 0.0), [], [r_qpad])
            vb2 = A.alloc([512], BF16); r_vb2 = Res("vb2")
            scm = A.alloc([512], BF16); r_scm = Res("scm")
            gate = A.alloc([512], F32); r_gate = Res("gate")
            osb = A.alloc([512], F32); r_osb = Res("osb")
            osq = A.alloc([512], F32); r_osq = Res("osq")
            st2 = A.alloc([16], F32); r_st2 = Res("st2")
            for c in range(TPC):
                b = c % 2
                P.dma("sp", lambda e, c=c, b=b: e.dma_start(out=cs2[b][:, 0:32], in_=cosv[c]), r_cs2[b])
                P.dma("sp", lambda e, c=c, b=b: e.dma_start(out=cs2[b][:, 32:64], in_=sinv[c]), r_cs2[b])
                hT = hxT_own[:, c, :].rearrange("p (j t) -> p j t", j=8)
                r_dT = r_hxT_own[c]
                mm_group(banks[0][:, 0:256], rb[0], [(hT[:, j, :], w_in_sb[:, j, 768:1024]) for j in range(8)], [r_dT, r_win])
                mm_group(banks[0][:, 256:512], rb[0], [(hT[:, j, :], w_in_sb[:, j, 1024:1280]) for j in range(8)], [r_dT, r_win])
                mm_group(banks[1][:], rb[1], [(hT[:, j, :], w_in_sb[:, j, 1280:1792]) for j in range(8)], [r_dT, r_win])
                mm_group(banks[2][:], rb[2], [(hT[:, j, :], w_in_sb[:, j, 1792:2304]) for j in range(8)], [r_dT, r_win])
                V("act", lambda e: e.activation(out=ri[:, 0:4, :], in_=banks[0][:, 0:256].rearrange("p (h d) -> p h d", d=64), func=AF.Copy), [rb[0]], [r_ri])
                V("act", lambda e: e.activation(out=ri[:, 4:8, :], in_=banks[0][:, 256:512].rearrange("p (h d) -> p h d", d=64), func=AF.Copy, scale=0.125), [rb[0]], [r_ri])
                if SUB == 3: raise _Stop()
                rope(0, 8, cs2[b], r_cs2[b])
                if SUB == 4: raise _Stop()
                V("dve", lambda e: e.tensor_copy(out=qk, in_=ro[:, 0:8, :]), [r_ro], [r_qk])
                if SUB == 41: raise _Stop()
                V("dve", lambda e: e.tensor_tensor(out=qaug[:, :, 0:64], in0=ro[:, 0:4, :], in1=xif.unsqueeze(2).to_broadcast([128, 4, 64]), op=ALU.mult), [r_ro, r_xi], [r_qaug])
                V("dve", lambda e: e.tensor_tensor(out=qaug[:, :, 64:128], in0=ro[:, 0:4, :], in1=xib.unsqueeze(2).to_broadcast([128, 4, 64]), op=ALU.mult), [r_ro, r_xi], [r_qaug])
                if SUB == 42: raise _Stop()
                V("act", lambda e: e.activation(out=vb2, in_=banks[1][:], func=AF.Copy), [rb[1]], [r_vb2])
                if SUB == 43: raise _Stop()
                V("act", lambda e: e.activation(out=gate, in_=banks[2][:], func=AF.Silu), [rb[2]], [r_gate])
                if SUB == 5: raise _Stop()
                pt3 = bank_bf(3)
                qkf = qk.rearrange("p h d -> p (h d)")
                for i in range(4):
                    V("pe", lambda e, i=i: e.transpose(out=pt3[:, i * 128:(i + 1) * 128], in_=qkf[:, i * 128:(i + 1) * 128], identity=ident_b),
                      [r_qk, r_idb], [rb[3]], inc=False, pe_acc=(i > 0))
                for h in range(4):
                    V("pe", lambda e, h=h: e.transpose(out=pt3[:, 512 + h * 128:512 + (h + 1) * 128], in_=qaug[:, h, :], identity=ident_b),
                      [r_qaug, r_idb], [rb[3]], inc=(h == 3), pe_acc=True)
                if SUB == 51: raise _Stop()
                V("dve", lambda e: e.tensor_copy(out=qkT.rearrange("p a b -> p (a b)"), in_=pt3[:, 0:512]), [rb[3]], [r_qkT])
                if SUB == 52: raise _Stop()
                for h in range(4):
                    pr = (h % 2) * 64
                    V("dve", lambda e, h=h, pr=pr: e.tensor_copy(out=qpad[pr:pr + 64, h, :], in_=pt3[pr:pr + 64, (h // 2) * 128:(h // 2 + 1) * 128]), [rb[3]], [r_qpad])
                if SUB == 53: raise _Stop()
                V("act", lambda e: e.activation(out=qaT.rearrange("p a b -> p (a b)"), in_=pt3[:, 512:1024], func=AF.Copy), [rb[3]], [r_qaT])
                if SUB == 6: raise _Stop()
                for h in range(4):
                    P.op("pe", lambda e, h=h: e.matmul(banks[4][:, h * 128:(h + 1) * 128], qkT[:, 2 + h // 2, :], qpad[:, h, :], start=True, stop=True),
                         [r_qkT, r_qpad], [rb[4]], inc=(h == 3), pe_acc=(h > 0))
                V("dve", lambda e: e.tensor_tensor(out=scm, in0=banks[4][:], in1=DT.rearrange("p a b -> p (a b)"), op=ALU.mult), [rb[4], r_DT], [r_scm])
                for h in range(4):
                    P.op("pe", lambda e, h=h: e.matmul(banks[5][:, h * 128:(h + 1) * 128], scm[:, h * 128:(h + 1) * 128], vb2[:, h * 128:(h + 1) * 128], start=True, stop=False),
                         [r_scm, r_vb2], [rb[5]], inc=False, pe_acc=(h > 0))
                    P.op("pe", lambda e, h=h, c=c: e.matmul(banks[5][:, h * 128:(h + 1) * 128], qaT[:, h, :], Sst[:, c, h * 128:(h + 1) * 128], start=False, stop=True),
                         [r_qaT, r_Sst[c]], [rb[5]], inc=(h == 3), pe_acc=True)
                if SUB == 7: raise _Stop()
                V("act", lambda e: e.activation(out=osb, in_=banks[5][:], func=AF.Copy), [rb[5]], [r_osb])
                V("dve", lambda e: e.tensor_reduce(out=st2[:, 0:4], in_=osb.rearrange("p (h d) -> p h d", d=128), axis=AX.X, op=ALU.add), [r_osb], [r_st2])
                V("dve", lambda e: e.tensor_tensor(out=osq, in0=osb, in1=osb, op=ALU.mult), [r_osb], [r_osq])
                V("dve", lambda e: e.tensor_reduce(out=st2[:, 4:8], in_=osq.rearrange("p (h d) -> p h d", d=128), axis=AX.X, op=ALU.add), [r_osq], [r_st2])
                V("dve", lambda e: e.tensor_scalar(out=st2[:, 0:4], in0=st2[:, 0:4], scalar1=1.0 / 128, scalar2=None, op0=ALU.mult), [r_st2], [r_st2])
                V("dve", lambda e: e.tensor_tensor(out=st2[:, 8:12], in0=st2[:, 0:4], in1=st2[:, 0:4], op=ALU.mult), [r_st2], [r_st2])
                V("dve", lambda e: e.scalar_tensor_tensor(out=st2[:, 4:8], in0=st2[:, 4:8], scalar=1.0 / 128, in1=st2[:, 8:12], op0=ALU.mult, op1=ALU.subtract), [r_st2], [r_st2])
                V("act", lambda e: e.activation(out=st2[:, 4:8], in_=st2[:, 4:8], func=AF.Ln, bias=EPS, scale=1.0), [r_st2], [r_st2])
                V("act", lambda e: e.activation(out=st2[:, 4:8], in_=st2[:, 4:8], func=AF.Exp, scale=-0.5), [r_st2], [r_st2])
                for h in range(4):
                    V("dve", lambda e, h=h: e.tensor_scalar(out=osb[:, h * 128:(h + 1) * 128], in0=osb[:, h * 128:(h + 1) * 128], scalar1=st2[:, h:h + 1],
                                                           scalar2=st2[:, 4 + h:5 + h], op0=ALU.subtract, op1=ALU.mult), [r_osb, r_st2], [r_osb])
                V("dve", lambda e: e.tensor_tensor(out=osb, in0=osb, in1=gng, op=ALU.mult), [r_osb, r_gng], [r_osb])
                V("dve", lambda e: e.tensor_tensor(out=osb, in0=osb, in1=gnb, op=ALU.add), [r_osb, r_gnb], [r_osb])
                V("dve", lambda e, c=c: e.tensor_tensor(out=retx[:, c, :], in0=osb, in1=gate, op=ALU.mult), [r_osb, r_gate], [r_retx[c]])
                if debug:
                    V("dve", lambda e: e.tensor_tensor(out=osq, in0=osb, in1=gate, op=ALU.mult), [r_osb, r_gate], [r_osq])
                    P.dma("sp", lambda e, c=c: e.dma_start(out=dbg["d_ret"][c * 128:(c + 1) * 128, :], in_=osq), r_dbg, [r_osq], no_waw=True, semres=r_osq)

            P.barrier()
            if stop == 3: raise _Stop()
            A.release(m_persist)

            attnx = A.alloc([TPC, 512], BF16); r_attnx = [Res("attnx%d" % i) for i in range(TPC)]
            m_att = A.mark()
            KT = A.alloc([NS * 128], BF16); r_KT = Res("KT")
            V1 = A.alloc([NS, 130], BF16); r_V1 = Res("V1")
            P.dma("sp", lambda e: e.dma_start(out=KT, in_=KT_d[:, :]), r_KT, [r_KTd])
            SCH = 10
            for s0 in range(0, NS, SCH):
                s1 = min(NS, s0 + SCH)
                P.dma("act", lambda e, s0=s0, s1=s1: e.dma_start(out=V1[:, s0:s1, :], in_=V_d[s0:s1].rearrange("s p c -> p s c")), r_V1, [r_Vd], no_waw=True)
            if debug:
                dtmp = A.alloc([NS * 130], F32); r_dtmp = Res("dtmp")
                V("dve", lambda e: e.tensor_copy(out=dtmp[:, 0:NS * 128], in_=KT), [r_KT], [r_dtmp])
                P.dma("sp", lambda e: e.dma_start(out=dbg["d_kt"], in_=dtmp[:, 0:NS * 128]), r_dbg, [r_dtmp], no_waw=True, semres=r_dtmp)
                V("dve", lambda e: e.tensor_copy(out=dtmp, in_=V1.rearrange("p a b -> p (a b)")), [r_V1], [r_dtmp])
                P.dma("sp", lambda e: e.dma_start(out=dbg["d_v1"], in_=dtmp), r_dbg, [r_dtmp], no_waw=True, semres=r_dtmp)
                V("dve", lambda e: e.tensor_copy(out=dtmp[:, 0:TPC * 512], in_=QT.rearrange("p a b -> p (a b)")), [r_QT], [r_dtmp])
                P.dma("sp", lambda e: e.dma_start(out=dbg["d_qt"], in_=dtmp[:, 0:TPC * 512]), r_dbg, [r_dtmp], no_waw=True, semres=r_dtmp)
            NPT = 3
            PT = [A.alloc([512], BF16) for _ in range(NPT)]; r_PT = [Res("PT%d" % i) for i in range(NPT)]
            OT = A.alloc([512], F32); r_OT = Res("OT")
            rcp = A.alloc([8], F32); r_rcp = Res("rcp")
            QP = [[A.alloc([512], BF16) for _ in range(2)] for _ in range(2)]
            r_QP = [[Res("QP%d%d" % (i, j)) for j in range(2)] for i in range(2)]
            for i in range(2):
                for j in range(2):
                    V("pool", lambda e, i=i, j=j: e.memset(QP[i][j], 0.0), [], [r_QP[i][j]])
            it = 0
            for qt in range(TPC):
                for kvh in range(2):
                    pr = kvh * 64
                    qp = QP[kvh][qt % 2]; r_qp = r_QP[kvh][qt % 2]
                    V("pool", lambda e, qp=qp, pr=pr, qt=qt: e.tensor_copy(out=qp[pr:pr + 64, :], in_=QT[pr:pr + 64, qt, :]), [r_QT], [r_qp])
                    ob = 4 + (qt * 2 + kvh) % 2
                    for kt in range(NS):
                        sb_ = it % 2
                        pb = it % NPT
                        it += 1
                        P.op("pe", lambda e, sb_=sb_, kt=kt, qp=qp: e.matmul(banks[sb_][:], KT[:, kt * 128:(kt + 1) * 128], qp, start=True, stop=True),
                             [r_KT, r_qp], [rb[sb_]])
                        V("act", lambda e, sb_=sb_, pb=pb: e.activation(out=PT[pb], in_=banks[sb_][:], func=AF.Exp, scale=0.125, bias=negC), [rb[sb_], r_negC], [r_PT[pb]])
                        P.op("pe", lambda e, ob=ob, kt=kt, kvh=kvh, pb=pb: e.matmul(banks[ob][0:65, :], V1[:, kt, kvh * 65:(kvh + 1) * 65], PT[pb], start=(kt == 0), stop=(kt == NS - 1)),
                             [r_V1, r_PT[pb]], [rb[ob]], inc=(kt == NS - 1), pe_acc=(kt > 0))
                    V("dve", lambda e, ob=ob: e.tensor_copy(out=OT[0:65, :], in_=banks[ob][0:65, :]), [rb[ob]], [r_OT])
                    for g in range(4):
                        V("pe", lambda e, g=g: e.transpose(out=banks[6][:, g * 66:g * 66 + 65], in_=OT[0:65, g * 128:(g + 1) * 128], identity=ident_f[0:65, 0:65]),
                          [r_OT, r_idf], [rb[6]], inc=(g == 3), pe_acc=(g > 0))
                    o4 = banks[6][:, 0:264].rearrange("p (g c) -> p g c", c=66)
                    V("dve", lambda e, o4=o4: e.reciprocal(out=rcp[:, 0:4], in_=o4[:, :, 64]), [rb[6]], [r_rcp])
                    V("dve", lambda e, o4=o4, qt=qt, kvh=kvh: e.tensor_tensor(out=attnx[:, qt, kvh * 256:(kvh + 1) * 256].rearrange("p (g d) -> p g d", d=64), in0=o4[:, :, 0:64],
                                                                        in1=rcp[:, 0:4].unsqueeze(2).to_broadcast([128, 4, 64]), op=ALU.mult), [rb[6], r_rcp], [r_attnx[qt]])
            if debug:
                for qt in range(TPC):
                    V("dve", lambda e, qt=qt: e.tensor_copy(out=OT, in_=attnx[:, qt, :]), [r_attnx[qt]], [r_OT])
                    P.dma("sp", lambda e, qt=qt: e.dma_start(out=dbg["d_attn"][qt * 128:(qt + 1) * 128, :], in_=OT), r_dbg, [r_OT], no_waw=True, semres=r_OT)
            P.barrier()
            if stop == 4: raise _Stop()
            A.release(m_att)

            w_out_sb = A.alloc([8, D], BF16); r_wout = Res("wout")
            w_out_v = w_out.rearrange("(j p) n -> p j n", p=128)
            for c0 in range(0, D, 256):
                P.dma("pool", lambda e, c0=c0: e.dma_start(out=w_out_sb[:, :, c0:c0 + 256], in_=w_out_v[:, :, c0:c0 + 256]), r_wout, no_waw=True)
            wrt = A.alloc([8, 36], F32); r_wrt = Res("wrt")
            P.dma("sp", lambda e: e.dma_start(out=wrt, in_=w_rt.rearrange("(j p) n -> p j n", p=128)), r_wrt)
            brt, r_brt = bload(b_rt, 36, "brt")
            mixT = A.alloc([8, 128], BF16); r_mixT = Res("mixT")
            xr = [A.alloc([D], F32) for _ in range(2)]; r_xr = [Res("xr0"), Res("xr1")]
            xn = A.alloc([D], F32); r_xn = Res("xn")
            h2 = A.alloc([D], F32); r_h2 = Res("h2")
            h2b = A.alloc([D], BF16); r_h2b = Res("h2b")
            h2Tf = A.alloc([8, 128], F32); r_h2Tf = Res("h2Tf")
            junk2 = A.alloc([D], BF16); r_junk2 = Res("junk2")
            lgt = A.alloc([36], F32); r_lgt = Res("lgt")
            msk = A.alloc([32], F32); r_msk = Res("msk")
            m8 = A.alloc([8], F32); r_m8 = Res("m8")
            sm3 = A.alloc([16], F32); r_sm3 = Res("sm3")
            oh = A.alloc([64], F32); r_oh = Res("oh")
            xown = xp.rearrange("(s p) n -> s p n", p=128)
            for c in range(TPC):
                b = c % 2
                P.dma("sp", lambda e, c=c, b=b: e.dma_start(out=xr[b], in_=xown[c]), r_xr[b])
                pt0 = bank_bf(0)
                for j in range(8):
                    src = attnx[:, c, j * 128:(j + 1) * 128] if j < 4 else retx[:, c, (j - 4) * 128:(j - 3) * 128]
                    rs_ = r_attnx[c] if j < 4 else r_retx[c]
                    V("pe", lambda e, j=j, src=src: e.transpose(out=pt0[:, j * 128:(j + 1) * 128], in_=src, identity=ident_b), [rs_, r_idb], [rb[0]], inc=(j == 7), pe_acc=(j > 0))
                V("act", lambda e: e.activation(out=mixT.rearrange("p a b -> p (a b)"), in_=pt0, func=AF.Copy), [rb[0]], [r_mixT])
                for hf in range(2):
                    mm_group(banks[1 + hf][:], rb[1 + hf], [(mixT[:, j, :], w_out_sb[:, j, hf * 512:(hf + 1) * 512]) for j in range(8)], [r_mixT, r_wout])
                for hf in range(2):
                    sl = slice(hf * 512, (hf + 1) * 512)
                    V("dve", lambda e, hf=hf, sl=sl: e.tensor_tensor(out=xn[:, sl], in0=banks[1 + hf][:], in1=GT1[:, sl], op=ALU.mult), [rb[1 + hf], r_GT1], [r_xn])
                V("dve", lambda e, b=b: e.tensor_tensor(out=xn, in0=xn, in1=xr[b], op=ALU.add), [r_xn, r_xr[b]], [r_xn])
                P.dma("sp", lambda e, c=c: e.dma_start(out=XN_d[c * 128:(c + 1) * 128, :], in_=xn), r_XNd, [r_xn], no_waw=True, semres=r_xn)
                if debug:
                    P.dma("sp", lambda e, c=c: e.dma_start(out=dbg["d_xnew"][c * 128:(c + 1) * 128, :], in_=xn), r_dbg, [r_xn], no_waw=True, semres=r_xn)
                V("act", lambda e: e.activation(out=junk2, in_=xn, func=AF.Square, accum_out=sm3[:, 0:1]), [r_xn], [r_junk2, r_sm3])
                V("act", lambda e: e.activation(out=sm3[:, 1:2], in_=sm3[:, 0:1], func=AF.Ln, scale=1.0 / D, bias=EPS), [r_sm3], [r_sm3])
                V("act", lambda e: e.activation(out=sm3[:, 1:2], in_=sm3[:, 1:2], func=AF.Exp, scale=-0.5), [r_sm3], [r_sm3])
                V("dve", lambda e: e.scalar_tensor_tensor(out=h2, in0=xn, scalar=sm3[:, 1:2], in1=G2, op0=ALU.mult, op1=ALU.mult), [r_xn, r_sm3, r_G2], [r_h2])
                V("dve", lambda e: e.tensor_tensor(out=h2, in0=h2, in1=SH2, op=ALU.add), [r_h2, r_SH2], [r_h2])
                V("pool", lambda e: e.tensor_copy(out=h2b, in_=h2), [r_h2], [r_h2b])
                pt3 = bank_bf(3)
                for j in range(8):
                    V("pe", lambda e, j=j: e.transpose(out=pt3[:, j * 128:(j + 1) * 128], in_=h2b[:, j * 128:(j + 1) * 128], identity=ident_b), [r_h2b, r_idb], [rb[3]], inc=(j == 7), pe_acc=(j > 0))
                V("act", lambda e, c=c: e.activation(out=h2T[:, :, c * 128:(c + 1) * 128], in_=pt3.rearrange("p (j t) -> p j t", j=8), func=AF.Copy), [rb[3]], [r_h2T[c]])
                for j in range(8):
                    bk = 4 + j // 4
                    V("pe", lambda e, j=j, bk=bk: e.transpose(out=banks[bk][:, (j % 4) * 128:(j % 4 + 1) * 128], in_=h2[:, j * 128:(j + 1) * 128], identity=ident_f), [r_h2, r_idf], [rb[bk]],
                      inc=(j % 4 == 3), pe_acc=(j % 4 > 0))
                V("dve", lambda e: e.tensor_copy(out=h2Tf[:, 0:4, :].rearrange("p a b -> p (a b)"), in_=banks[4][:]), [rb[4]], [r_h2Tf])
                V("dve", lambda e: e.tensor_copy(out=h2Tf[:, 4:8, :].rearrange("p a b -> p (a b)"), in_=banks[5][:]), [rb[5]], [r_h2Tf])
                mm_group(banks[6][:, 0:36], rb[6], [(h2Tf[:, j, :], wrt[:, j, :]) for j in range(8)], [r_h2Tf, r_wrt])
                V("dve", lambda e: e.tensor_tensor(out=lgt, in0=banks[6][:, 0:36], in1=brt, op=ALU.add), [rb[6], r_brt], [r_lgt])
                V("dve", lambda e: e.tensor_reduce(out=sm3[:, 2:3], in_=lgt[:, 0:4], axis=AX.X, op=ALU.max), [r_lgt], [r_sm3])
                V("dve", lambda e: e.tensor_scalar(out=oh[:, 0:4], in0=lgt[:, 0:4], scalar1=sm3[:, 2:3], scalar2=None, op0=ALU.is_equal), [r_lgt, r_sm3], [r_oh])
                V("dve", lambda e: e.tensor_scalar(out=sm3[:, 3:4], in0=sm3[:, 2:3], scalar1=-1.0, scalar2=None, op0=ALU.mult), [r_sm3], [r_sm3])
                V("act", lambda e: e.activation(out=oh[:, 8:12], in_=lgt[:, 0:4], func=AF.Exp, bias=sm3[:, 3:4], scale=1.0, accum_out=sm3[:, 4:5]), [r_lgt, r_sm3], [r_oh, r_sm3])
                V("dve", lambda e: e.reciprocal(out=sm3[:, 5:6], in_=sm3[:, 4:5]), [r_sm3], [r_sm3])
                V("dve", lambda e: e.tensor_scalar(out=oh[:, 4:8], in0=oh[:, 0:4], scalar1=-1.0, scalar2=1e30, op0=ALU.add, op1=ALU.mult), [r_oh], [r_oh])
                V("dve", lambda e: e.tensor_tensor(out=msk.rearrange("p (g j) -> p g j", j=8), in0=lgt[:, 4:36].rearrange("p (g j) -> p g j", j=8),
                                                   in1=oh[:, 4:8].unsqueeze(2).to_broadcast([128, 4, 8]), op=ALU.add), [r_lgt, r_oh], [r_msk])
                V("dve", lambda e: e.max(out=m8, in_=msk), [r_msk], [r_m8])
                V("dve", lambda e: e.tensor_tensor(out=sm3[:, 6:7], in0=m8[:, 1:2], in1=m8[:, 0:1], op=ALU.subtract), [r_m8], [r_sm3])
                V("act", lambda e: e.activation(out=sm3[:, 6:7], in_=sm3[:, 6:7], func=AF.Exp), [r_sm3], [r_sm3])
                V("dve", lambda e: e.tensor_scalar(out=sm3[:, 6:7], in0=sm3[:, 6:7], scalar1=1.0, scalar2=None, op0=ALU.add), [r_sm3], [r_sm3])
                V("dve", lambda e: e.reciprocal(out=sm3[:, 7:8], in_=sm3[:, 6:7]), [r_sm3], [r_sm3])
                V("dve", lambda e: e.tensor_tensor(out=sm3[:, 8:9], in0=sm3[:, 7:8], in1=sm3[:, 5:6], op=ALU.mult), [r_sm3], [r_sm3])
                V("dve", lambda e: e.tensor_tensor(out=sm3[:, 9:10], in0=sm3[:, 5:6], in1=sm3[:, 8:9], op=ALU.subtract), [r_sm3], [r_sm3])
                V("dve", lambda e: e.tensor_scalar(out=oh[:, 0:32], in0=msk, scalar1=m8[:, 0:1], scalar2=sm3[:, 8:9], op0=ALU.is_equal, op1=ALU.mult), [r_msk, r_m8, r_sm3], [r_oh])
                V("dve", lambda e: e.tensor_scalar(out=oh[:, 32:64], in0=msk, scalar1=m8[:, 1:2], scalar2=sm3[:, 9:10], op0=ALU.is_equal, op1=ALU.mult), [r_msk, r_m8, r_sm3], [r_oh])
                V("dve", lambda e, c=c: e.tensor_tensor(out=Wtok[:, c, :], in0=oh[:, 0:32], in1=oh[:, 32:64], op=ALU.add), [r_oh], [r_Wtok[c]])
                if debug:
                    P.dma("sp", lambda e, c=c: e.dma_start(out=dbg["d_wtok"][c * 128:(c + 1) * 128, :], in_=Wtok[:, c, :]), r_dbg, [r_Wtok[c]], no_waw=True, semres=r_Wtok[c])
            P.barrier()
            if stop == 5: raise _Stop()
            A.release(m_moe)
            FG, r_FG = bload(final_g, D, "FG")

            yacc = A.alloc([TPC, D], F32); r_yacc = [Res("yacc%d" % i) for i in range(TPC)]
            for c in range(TPC):
                V("pool", lambda e, c=c: e.memset(yacc[:, c, :], 0.0), [], [r_yacc[c]])
            NWB = 2
            wgs = [A.alloc([8, 512], BF16) for _ in range(NWB)]
            wus = [A.alloc([8, 512], BF16) for _ in range(NWB)]
            wds = [A.alloc([4, D], BF16) for _ in range(NWB)]
            r_wgs = [Res("wgs%d" % i) for i in range(NWB)]; r_wus = [Res("wus%d" % i) for i in range(NWB)]; r_wds = [Res("wds%d" % i) for i in range(NWB)]
            sg = [A.alloc([512], F32) for _ in range(2)]; r_sg = [Res("sg0"), Res("sg1")]
            TG = min(4, TPC)
            NG = TPC // TG
            h1T = [A.alloc([4, TG * 128], BF16) for _ in range(2)]; r_h1T = [Res("h1T0"), Res("h1T1")]
            gi = 0
            for ex in range(32):
                wbf = ex % NWB
                for c0 in range(0, 512, 256):
                    P.dma("pool", lambda e, ex=ex, wbf=wbf, c0=c0: e.dma_start(out=wgs[wbf][:, :, c0:c0 + 256], in_=wg[ex].rearrange("(j p) n -> p j n", p=128)[:, :, c0:c0 + 256]), r_wgs[wbf], no_waw=(c0 > 0))
                for c0 in range(0, 512, 256):
                    P.dma("pool", lambda e, ex=ex, wbf=wbf, c0=c0: e.dma_start(out=wus[wbf][:, :, c0:c0 + 256], in_=wu[ex].rearrange("(j p) n -> p j n", p=128)[:, :, c0:c0 + 256]), r_wus[wbf], no_waw=(c0 > 0))
                for c0 in range(0, D, 256):
                    P.dma("pool", lambda e, ex=ex, wbf=wbf, c0=c0: e.dma_start(out=wds[wbf][:, :, c0:c0 + 256], in_=wd[ex].rearrange("(j p) n -> p j n", p=128)[:, :, c0:c0 + 256]), r_wds[wbf], no_waw=(c0 > 0))
                for tg in range(NG):
                    hb = gi % 2
                    gi += 1
                    N = TG * 128
                    tsl = slice(tg * N, (tg + 1) * N)
                    r_h2g = [r_h2T[tg * TG + i] for i in range(TG)]
                    for ec in range(4):
                        gb_ = (ec % 2) * 2
                        mm_group(banks[gb_][:, 0:N], rb[gb_], [(wgs[wbf][:, j, ec * 128:(ec + 1) * 128], h2T[:, j, tsl]) for j in range(8)], r_h2g + [r_wgs[wbf]])
                        mm_group(banks[gb_ + 1][:, 0:N], rb[gb_ + 1], [(wus[wbf][:, j, ec * 128:(ec + 1) * 128], h2T[:, j, tsl]) for j in range(8)], r_h2g + [r_wus[wbf]])
                        sb2 = ec % 2
                        V("act", lambda e, gb_=gb_, sb2=sb2, N=N: e.activation(out=sg[sb2][:, 0:N], in_=banks[gb_][:, 0:N], func=AF.Silu), [rb[gb_]], [r_sg[sb2]])
                        V("dve", lambda e, gb_=gb_, sb2=sb2, hb=hb, ec=ec, N=N: e.tensor_tensor(out=h1T[hb][:, ec, :], in0=banks[gb_ + 1][:, 0:N], in1=sg[sb2][:, 0:N], op=ALU.mult),
                          [rb[gb_ + 1], r_sg[sb2]], [r_h1T[hb]])
                    for i in range(TG):
                        c = tg * TG + i
                        for hf in range(2):
                            ob = 4 + (i * 2 + hf) % 4
                            mm_group(banks[ob][:], rb[ob], [(h1T[hb][:, ec, i * 128:(i + 1) * 128], wds[wbf][:, ec, hf * 512:(hf + 1) * 512]) for ec in range(4)], [r_h1T[hb], r_wds[wbf]])
                            V("dve", lambda e, ob=ob, c=c, hf=hf, ex=ex: e.scalar_tensor_tensor(out=yacc[:, c, hf * 512:(hf + 1) * 512], in0=banks[ob][:], scalar=Wtok[:, c, ex:ex + 1],
                                                                                       in1=yacc[:, c, hf * 512:(hf + 1) * 512], op0=ALU.mult, op1=ALU.add),
                              [rb[ob], r_Wtok[c], r_yacc[c]], [r_yacc[c]])
            xq = [A.alloc([D], F32) for _ in range(2)]; r_xq = [Res("xq0"), Res("xq1")]
            junk3 = A.alloc([D], BF16); r_junk3 = Res("junk3")
            sm4 = A.alloc([4], F32); r_sm4 = Res("sm4")
            for c in range(TPC):
                b = c % 2
                P.dma("sp", lambda e, c=c, b=b: e.dma_start(out=xq[b], in_=XN_d[c * 128:(c + 1) * 128, :]), r_xq[b], [r_XNd])
                V("dve", lambda e, c=c: e.tensor_tensor(out=yacc[:, c, :], in0=yacc[:, c, :], in1=GT2, op=ALU.mult), [r_yacc[c], r_GT2], [r_yacc[c]])
                V("dve", lambda e, c=c, b=b: e.tensor_tensor(out=xq[b], in0=xq[b], in1=yacc[:, c, :], op=ALU.add), [r_xq[b], r_yacc[c]], [r_xq[b]])
                V("act", lambda e, b=b: e.activation(out=junk3, in_=xq[b], func=AF.Square, accum_out=sm4[:, 0:1]), [r_xq[b]], [r_junk3, r_sm4])
                V("act", lambda e: e.activation(out=sm4[:, 1:2], in_=sm4[:, 0:1], func=AF.Ln, scale=1.0 / D, bias=EPS), [r_sm4], [r_sm4])
                V("act", lambda e: e.activation(out=sm4[:, 1:2], in_=sm4[:, 1:2], func=AF.Exp, scale=-0.5), [r_sm4], [r_sm4])
                V("dve", lambda e, b=b: e.scalar_tensor_tensor(out=xq[b], in0=xq[b], scalar=sm4[:, 1:2], in1=FG, op0=ALU.mult, op1=ALU.mult), [r_xq[b], r_sm4, r_FG], [r_xq[b]])
                P.dma("sp", lambda e, c=c, b=b: e.dma_start(out=out[c * 128:(c + 1) * 128, :], in_=xq[b]), r_out, [r_xq[b]], no_waw=True, semres=r_xq[b])
        try:
            body()
        except _Stop:
            P.barrier()
        fin = [r_out] + ([r_dbg] if debug else [])
        P.wait_all("sp", fin)
        P.wait_all("pool", fin)
        P.emit(st)
    return nc


def _rope_tables(L):
    GRID_W = 64
    rows = L // GRID_W
    row = np.repeat(np.arange(rows), GRID_W).astype(np.float32)
    col = np.tile(np.arange(GRID_W), rows).astype(np.float32)
    freqs = (np.float32(10000.0) ** (-np.arange(0, 32, 2, dtype=np.float32) / np.float32(32))).astype(np.float32)
    ang = np.concatenate([row[:, None] * freqs, col[:, None] * freqs], axis=-1).astype(np.float32)
    return np.cos(ang).astype(np.float32), np.sin(ang).astype(np.float32)


def make_in_maps(inputs, NT):
    TPC = NT // NCORES
    NS = NT + 2
    L = NT * 128
    f = lambda a: np.ascontiguousarray(np.asarray(a, dtype=np.float32))
    x = f(inputs["x"]).reshape(L, D)
    ctx = f(inputs["ctx"]).reshape(256, D)
    cos, sin = _rope_tables(L)
    cvecT = np.zeros((128, 16), np.float32)
    cvecT[:, 0:8] = f(inputs["c"]).reshape(8, 128).T
    cvecT[:, 8:16] = f(inputs["c_ctx"]).reshape(8, 128).T
    p = np.arange(128, dtype=np.float32)
    dpos = np.maximum(p[None, :] - p[:, None], 0.0)
    dneg = np.maximum(p[:, None] - p[None, :], 0.0)
    dmat = np.concatenate([dpos, dneg], axis=1).astype(np.float32)
    pcol = np.stack([p + 1.0, 128.0 - p], axis=1).astype(np.float32)
    shared = {
        "cvecT": cvecT,
        "w_ada": f(inputs["w_ada"]).reshape(D, 6 * D), "b_ada": f(inputs["b_ada"]).reshape(1, 6 * D),
        "norm1_g": f(inputs["norm1_g"]).reshape(1, D), "norm2_g": f(inputs["norm2_g"]).reshape(1, D),
        "final_g": f(inputs["final_norm_g"]).reshape(1, D),
        "w_in": f(inputs["w_in"]).reshape(D, 2304),
        "qn": f(inputs["attn_q_norm"]).reshape(1, 64), "kn": f(inputs["attn_k_norm"]).reshape(1, 64),
        "dec": np.concatenate([f(inputs["ret_decay_fwd"]).reshape(1, 4), f(inputs["ret_decay_bwd"]).reshape(1, 4)], axis=1),
        "gn_g": f(inputs["ret_gn_g"]).reshape(1, 512), "gn_b": f(inputs["ret_gn_b"]).reshape(1, 512),
        "w_out": f(inputs["w_out"]).reshape(D, D),
        "w_rt": np.ascontiguousarray(np.concatenate([f(inputs["moe_w_grp"]).reshape(D, 4), f(inputs["moe_w_exp"]).reshape(D, 32)], axis=1)),
        "b_rt": np.concatenate([f(inputs["moe_b_grp"]).reshape(1, 4), f(inputs["moe_b_exp"]).reshape(1, 32)], axis=1),
        "wg": f(inputs["moe_w_gate"]).reshape(32, D, 512), "wu": f(inputs["moe_w_up"]).reshape(32, D, 512),
        "wd": f(inputs["moe_w_down"]).reshape(32, 512, D),
        "dmat_t": dmat, "pcol_t": pcol,
    }
    maps = []
    for r in range(NCORES):
        own = list(range(r * TPC, (r + 1) * TPC))
        others = [t for t in range(NT) if t not in own]
        order = own + others
        rows = np.concatenate([np.arange(t * 128, (t + 1) * 128) for t in order])
        xpm = np.concatenate([x[rows], ctx], axis=0)
        cosp = np.concatenate([cos[rows], np.ones((256, 32), np.float32)], axis=0)
        sinp = np.concatenate([sin[rows], np.zeros((256, 32), np.float32)], axis=0)
        s0 = r * TPC * 128
        e0 = s0 + TPC * 128
        distf = np.zeros((128, NS), np.float32); distb = np.zeros((128, NS), np.float32)
        maskf = np.zeros((128, NS), np.float32); maskb = np.zeros((128, NS), np.float32)
        for s, t in enumerate(order):
            m = t * 128 + p
            if s < TPC:
                distf[:, s] = 127.0 - p; distb[:, s] = p; maskf[:, s] = 1.0; maskb[:, s] = 1.0
            elif t * 128 < s0:
                distf[:, s] = s0 - 1 - m; maskf[:, s] = 1.0
            else:
                distb[:, s] = m - e0; maskb[:, s] = 1.0
        for j in range(2):
            mc = j * 128 + p
            distf[:, NT + j] = s0 + 255 - mc; maskf[:, NT + j] = 1.0
            distb[:, NT + j] = L + mc - e0; maskb[:, NT + j] = 1.0
        d = dict(shared)
        d["xp"] = np.ascontiguousarray(xpm)
        d["cos_t"] = np.ascontiguousarray(cosp)
        d["sin_t"] = np.ascontiguousarray(sinp)
        d["dist_t"] = np.ascontiguousarray(np.concatenate([distf, distb, maskf, maskb], axis=1))
        maps.append(d)
    return maps


_NC_CACHE = {}


def run(inputs, NT, debug=False, stop=99, cores=None):
    key = (NT, debug, stop)
    if key not in _NC_CACHE:
        _NC_CACHE[key] = build_program(NT, debug, stop)
    nc = _NC_CACHE[key]
    maps = make_in_maps(inputs, NT)
    if stop < 6:
        for m in maps:
            for k in ("wg", "wu", "wd"):
                m[k] = m[k][0:1]
    if cores is not None:
        maps = [maps[c] for c in cores]
        return run_bass_kernel_spmd(nc, maps, core_ids=list(range(len(cores))))
    res = run_bass_kernel_spmd(nc, maps, core_ids=list(range(NCORES)))
    return res


def kernel(**inputs):
    NT = 128
    res = run(inputs, NT)
    outp = np.concatenate([np.asarray(res.results[r]["out"]) for r in range(NCORES)], axis=0)
    return outp.reshape(1, NT * 128, D).astype(np.float32)
```

```python
import numpy as np
from contextlib import ExitStack
import concourse.bass as bass
import concourse.mybir as mybir
from concourse.bass_utils import run_bass_kernel_spmd

F32 = mybir.dt.float32
BF16 = mybir.dt.bfloat16
I32 = mybir.dt.int32
U32 = mybir.dt.uint32
AF = mybir.ActivationFunctionType
ALU = mybir.AluOpType
AX = mybir.AxisListType

ENGS = ("pe", "act", "dve", "pool", "sp")
NCORES = 8
D = 1024
EPS = 1e-6


import types


def _freeze(fn):
    if fn is None or fn.__closure__ is None:
        return fn
    cells = []
    for c in fn.__closure__:
        try:
            cells.append(types.CellType(c.cell_contents))
        except ValueError:
            cells.append(c)
    return types.FunctionType(fn.__code__, fn.__globals__, fn.__name__, fn.__defaults__, tuple(cells))


class Res:
    __slots__ = ("name", "w", "rs", "dsem", "dcount", "excl")

    def __init__(self, name, excl=False):
        self.name = name
        self.excl = excl
        self.w = {}
        self.rs = {}
        self.dsem = None
        self.dcount = 0


class Prog:
    def __init__(self, nc):
        self.nc = nc
        self.ops = {e: [] for e in ENGS}
        self.cnt = {e: 0 for e in ENGS}
        self.known = {e: {} for e in ENGS}
        self.semnames = []
        self.dres = []

    def _deps(self, eng, reads, writes):
        best = {}

        def add(d):
            for s, v in d.items():
                if v > best.get(s, 0):
                    best[s] = v
        for r in reads:
            add(r.w)
        for w in writes:
            add(w.w)
            add(w.rs)
        waits = []
        kn = self.known[eng]
        for s, v in best.items():
            if kn.get(s, 0) < v:
                kn[s] = v
                waits.append((s, v))
        return waits

    def op(self, eng, fn, reads=(), writes=(), inc=True, pe_acc=False):
        fn = _freeze(fn)
        xr = [r for r in reads if r.excl]
        if xr:
            writes = list(writes) + xr
            reads = [r for r in reads if not r.excl]
        waits = self._deps(eng, reads, writes)
        if pe_acc:
            waits = [(s, v) for (s, v) in waits if s != "E_pe"]
        sem = "E_" + eng
        val = self.cnt[eng] + 1
        if inc:
            self.cnt[eng] = val
        self.ops[eng].append(("op", fn, waits, sem if inc else None))
        for r in reads:
            if r.rs.get(sem, 0) < val:
                r.rs[sem] = val
        for w in writes:
            w.w = {sem: val}
            w.rs = {}
        return (sem, val)

    def dma(self, eng, fn, dst, reads=(), no_waw=False, semres=None):
        fn = _freeze(fn)
        owner = semres if semres is not None else dst
        wl = [] if no_waw else [dst]
        if semres is not None:
            wl.append(semres)
        rl = list(reads)
        waits = self._deps(eng, rl, wl)
        if owner.dsem is None:
            owner.dsem = "D%d_%s" % (len(self.semnames), owner.name)
            self.semnames.append(owner.dsem)
            self.dres.append(owner)
        owner.dcount += 16
        sem, val = owner.dsem, owner.dcount
        self.ops[eng].append(("dma", fn, waits, sem))
        for r in rl:
            if r.rs.get(sem, 0) < val:
                r.rs[sem] = val
        if semres is not None:
            semres.rs[sem] = val
        if no_waw:
            dst.w[sem] = val
        else:
            dst.w = {sem: val}
            dst.rs = {}
        return (sem, val)

    def wait_all(self, eng, ress):
        waits = self._deps(eng, ress, [])
        self.ops[eng].append(("wait", None, waits, None))

    def barrier(self):
        evs = [("E_" + e, self.cnt[e]) for e in ENGS if self.cnt[e] > 0]
        evs += [(r.dsem, r.dcount) for r in self.dres]
        for e in ENGS:
            kn = self.known[e]
            waits = []
            for (s, v) in evs:
                if kn.get(s, 0) < v:
                    kn[s] = v
                    waits.append((s, v))
            if waits:
                self.ops[e].append(("wait", None, waits, None))

    def emit(self, stack):
        nc = self.nc
        sems = {}
        for e in ENGS:
            sems["E_" + e] = stack.enter_context(nc.semaphore("E_" + e))
        for n in self.semnames:
            sems[n] = stack.enter_context(nc.semaphore(n))
        block = stack.enter_context(nc.Block())
        engmap = {"pe": block.tensor, "act": block.scalar, "dve": block.vector,
                  "pool": block.gpsimd, "sp": block.sync}
        for e in ENGS:
            ops = self.ops[e]
            if not ops:
                continue

            def body(engine, ops=ops):
                for kind, fn, waits, sem in ops:
                    for (s, v) in waits:
                        engine.wait_ge(sems[s], v)
                    if kind == "op":
                        ins = fn(engine)
                        if sem is not None:
                            ins.then_inc(sems[sem], 1)
                    elif kind == "dma":
                        ins = fn(engine)
                        ins.then_inc(sems[sem], 16)
            engmap[e](body)


class Arena:
    def __init__(self, t, nwords, start=0):
        self.t = t
        self.n = nwords
        self.top = start

    def alloc(self, shape, dt):
        esz = 2 if dt == BF16 else 4
        nel = int(np.prod(shape))
        words = (nel * esz + 3) // 4
        words = (words + 7) // 8 * 8
        assert self.top + words <= self.n, ("arena overflow", self.top, words, self.n)
        ap = self.t[:, self.top:self.top + words]
        self.top += words
        if dt != F32:
            ap = ap.bitcast(dt)
        ap = ap[:, 0:nel]
        if len(shape) == 2:
            ap = ap.rearrange("p (a b) -> p a b", a=shape[0])
        elif len(shape) == 3:
            ap = ap.rearrange("p (a b c) -> p a b c", a=shape[0], b=shape[1])
        return ap

    def mark(self):
        return self.top

    def release(self, m):
        self.top = m


class _Stop(Exception):
    pass


def build_program(NT, debug=False, stop=99):
    import os
    SUB = int(os.environ.get('KSUB', '0'))
    TPC = NT // NCORES
    NS = NT + 2
    nc = bass.Bass("TRN2", target_bir_lowering=False)

    def din(name, shape, dt=F32):
        return nc.dram_tensor(name, shape, dt, kind="ExternalInput").ap()

    xp = din("xp", [NS * 128, D])
    cvecT = din("cvecT", [128, 16])
    w_ada = din("w_ada", [D, 6 * D])
    b_ada = din("b_ada", [1, 6 * D])
    norm1_g = din("norm1_g", [1, D])
    norm2_g = din("norm2_g", [1, D])
    final_g = din("final_g", [1, D])
    w_in = din("w_in", [D, 2304])
    qn = din("qn", [1, 64])
    kn = din("kn", [1, 64])
    dec = din("dec", [1, 8])
    gn_g = din("gn_g", [1, 512])
    gn_b = din("gn_b", [1, 512])
    w_out = din("w_out", [D, D])
    w_rt = din("w_rt", [D, 36])
    b_rt = din("b_rt", [1, 36])
    NEXP = 32 if stop >= 6 else 1
    wg = din("wg", [NEXP, D, 512])
    wu = din("wu", [NEXP, D, 512])
    wd = din("wd", [NEXP, 512, D])
    cos_t = din("cos_t", [NS * 128, 32])
    sin_t = din("sin_t", [NS * 128, 32])
    dist_t = din("dist_t", [128, 4 * NS])
    dmat_t = din("dmat_t", [128, 256])
    pcol_t = din("pcol_t", [128, 2])
    out = nc.dram_tensor("out", [TPC * 128, D], F32, kind="ExternalOutput").ap()
    dbg = {}
    if debug:
        for nm, shp in [("d_hx", [128, D]), ("d_attn", [TPC * 128, 512]), ("d_ret", [TPC * 128, 512]),
                        ("d_xnew", [TPC * 128, D]), ("d_wtok", [TPC * 128, 32]), ("d_g1", [128, D]),
                        ("d_kt", [128, NS * 128]), ("d_v1", [128, NS * 130]), ("d_qt", [128, TPC * 512])]:
            dbg[nm] = nc.dram_tensor(nm, shp, F32, kind="ExternalOutput").ap()

    KT_d = nc.dram_tensor("KT_d", [128, NS * 128], BF16).ap()
    V_d = nc.dram_tensor("V_d", [NS, 128, 130], BF16).ap()
    XN_d = nc.dram_tensor("XN_d", [TPC * 128, D], F32).ap()
    r_KTd = Res("KTd"); r_Vd = Res("Vd"); r_XNd = Res("XNd"); r_out = Res("out"); r_dbg = Res("dbg")

    P = Prog(nc)
    st = ExitStack()
    with st:
        ARW = 51 * 1024
        arena_t = st.enter_context(nc.sbuf_tensor("arena", [128, ARW], F32))
        A = Arena(arena_t, ARW)
        banks = [st.enter_context(nc.psum_tensor("bank%d" % i, [128, 512], F32)) for i in range(8)]
        rb = [Res("bank%d" % i, excl=True) for i in range(8)]

        def bank_bf(i):
            return banks[i][:].bitcast(BF16)

        def V(eng, fn, reads, writes, **kw):
            return P.op(eng, fn, reads, writes, **kw)

        def mm_group(ps_ap, ps_res, pairs, reads):
            n = len(pairs)
            for i, (l, r) in enumerate(pairs):
                P.op("pe", lambda e, l=l, r=r, i=i: e.matmul(ps_ap, l, r, start=(i == 0), stop=(i == n - 1)),
                     reads, [ps_res], inc=(i == n - 1), pe_acc=(i > 0))

        def body():
            nonlocal A
            ident_f = A.alloc([128], F32); r_idf = Res("idf")
            ident_b = A.alloc([128], BF16); r_idb = Res("idb")
            V("pool", lambda e: e.memset(ident_f, 0.0), [], [r_idf])
            V("pool", lambda e: e.affine_select(out=ident_f, in_=ident_f, pattern=[[-1, 128]], compare_op=ALU.not_equal,
                                                fill=1.0, base=0, channel_multiplier=1), [r_idf], [r_idf])
            V("dve", lambda e: e.tensor_copy(out=ident_b, in_=ident_f), [r_idf], [r_idb])

            def bload(dram_row, n, name):
                t = A.alloc([n], F32); r = Res(name)
                P.dma("sp", lambda e: e.dma_start(out=t, in_=dram_row.partition_broadcast(128)), r)
                return t, r

            GT1 = A.alloc([D], F32); r_GT1 = Res("GT1")
            G2 = A.alloc([D], F32); r_G2 = Res("G2")
            SH2 = A.alloc([D], F32); r_SH2 = Res("SH2")
            GT2 = A.alloc([D], F32); r_GT2 = Res("GT2")
            dec_bc, r_dec = bload(dec, 8, "dec")
            qn_bc, r_qn = bload(qn, 64, "qn")
            kn_bc, r_kn = bload(kn, 64, "kn")
            lg = A.alloc([8], F32); r_lg = Res("lg")
            V("act", lambda e: e.activation(out=lg, in_=dec_bc, func=AF.Exp, scale=-1.0), [r_dec], [r_lg])
            V("act", lambda e: e.activation(out=lg, in_=lg, func=AF.Ln, bias=1.0, scale=1.0), [r_lg], [r_lg])
            V("dve", lambda e: e.tensor_scalar(out=lg, in0=lg, scalar1=-1.0, scalar2=None, op0=ALU.mult), [r_lg], [r_lg])
            negC = A.alloc([1], F32); r_negC = Res("negC")
            mk_ = A.alloc([1], F32); r_mk = Res("mk")
            V("dve", lambda e: e.tensor_reduce(out=negC, in_=qn_bc, axis=AX.X, op=ALU.max, apply_absolute_value=True), [r_qn], [r_negC])
            V("dve", lambda e: e.tensor_reduce(out=mk_, in_=kn_bc, axis=AX.X, op=ALU.max, apply_absolute_value=True), [r_kn], [r_mk])
            V("dve", lambda e: e.tensor_tensor(out=negC, in0=negC, in1=mk_, op=ALU.mult), [r_negC, r_mk], [r_negC])
            V("dve", lambda e: e.tensor_scalar(out=negC, in0=negC, scalar1=-8.0, scalar2=None, op0=ALU.mult), [r_negC], [r_negC])
            Wtok = A.alloc([TPC, 32], F32); r_Wtok = [Res("Wtok%d" % i) for i in range(TPC)]
            m_scrA0 = A.mark()
            actT = A.alloc([TPC * 1024], BF16)
            hxT_own = actT.rearrange("p (c x) -> p c x", c=TPC); r_hxT_own = [Res("hxTo%d" % i) for i in range(TPC)]
            h2T = actT.rearrange("p (j t) -> p j t", j=8); r_h2T = [Res("h2T%d" % i) for i in range(TPC)]
            m_moe = A.mark()
            retx = A.alloc([TPC, 512], BF16); r_retx = [Res("retx%d" % i) for i in range(TPC)]
            QT = A.alloc([TPC, 512], BF16); r_QT = Res("QT")
            m_persist = A.mark()
            m_scrA1 = A.mark()
            w_in_sb = A.alloc([8, 2304], BF16); r_win = Res("win")
            w_in_v = w_in.rearrange("(j p) n -> p j n", p=128)
            for c0 in range(0, 2304, 256):
                P.dma("pool", lambda e, c0=c0: e.dma_start(out=w_in_sb[:, :, c0:c0 + 256], in_=w_in_v[:, :, c0:c0 + 256]), r_win, no_waw=True)
            Ust = A.alloc([TPC, 512], BF16); r_Ust = [Res("Ust%d" % i) for i in range(TPC)]
            Sacc = A.alloc([512], F32); r_Sacc = Res("Sacc")
            V("pool", lambda e: e.memset(Sacc, 0.0), [], [r_Sacc])
            ri = A.alloc([14, 64], F32); r_ri = Res("ri")
            ro = A.alloc([14, 64], F32); r_ro = Res("ro")
            rt = [A.alloc([14, 32], F32) for _ in range(2)]; r_rt = [Res("rt%d" % i) for i in range(2)]
            m_sw = A.mark()

            G1 = A.alloc([D], F32); r_G1 = Res("G1")
            SH1 = A.alloc([D], F32); r_SH1 = Res("SH1")
            G1c = A.alloc([D], F32); r_G1c = Res("G1c")
            SH1c = A.alloc([D], F32); r_SH1c = Res("SH1c")
            A_main = A
            if m_scrA1 - m_scrA0 >= 15000:
                A = Arena(arena_t, m_scrA1, start=m_scrA0)
            else:
                A = Arena(arena_t, ARW, start=A_main.top)
            cv = A.alloc([16], F32); r_cv = Res("cv")
            P.dma("sp", lambda e: e.dma_start(out=cv, in_=cvecT[:, :]), r_cv)
            V("act", lambda e: e.activation(out=cv, in_=cv, func=AF.Silu), [r_cv], [r_cv])
            rep = A.alloc([16, 128], F32); r_rep = Res("rep")
            V("dve", lambda e: e.tensor_copy(out=rep, in_=cv.unsqueeze(2).to_broadcast([128, 16, 128])), [r_cv], [r_rep])
            bada2 = [A.alloc([512], F32) for _ in range(2)]; r_bada2 = [Res("bada0"), Res("bada1")]
            n1g, r_n1g = bload(norm1_g, D, "n1g")
            n2g, r_n2g = bload(norm2_g, D, "n2g")
            wst = [A.alloc([8, 512], F32) for _ in range(2)]; r_wst = [Res("wst0"), Res("wst1")]
            tmpA = A.alloc([512], F32); r_tmpA = Res("tmpA")
            w_ada_v = w_ada.rearrange("(j p) n -> p j n", p=128)
            plan = {0: ("sh", SH1, r_SH1, SH1c, r_SH1c, None), 1: ("sh", SH1, r_SH1, SH1c, r_SH1c, None),
                    2: ("sc", G1, r_G1, G1c, r_G1c, (n1g, r_n1g)), 3: ("sc", G1, r_G1, G1c, r_G1c, (n1g, r_n1g)),
                    4: ("sh", GT1, r_GT1, None, None, None), 5: ("sh", GT1, r_GT1, None, None, None),
                    6: ("sh", SH2, r_SH2, None, None, None), 7: ("sh", SH2, r_SH2, None, None, None),
                    8: ("sc", G2, r_G2, None, None, (n2g, r_n2g)), 9: ("sc", G2, r_G2, None, None, (n2g, r_n2g)),
                    10: ("sh", GT2, r_GT2, None, None, None), 11: ("sh", GT2, r_GT2, None, None, None)}
            for cb in range(12):
                kind, dst, r_dst, dstc, r_dstc, gg = plan[cb]
                wb = wst[cb % 2]; r_wb = r_wst[cb % 2]
                P.dma("sp" if cb % 2 == 0 else "act", lambda e, wb=wb, cb=cb: e.dma_start(out=wb, in_=w_ada_v[:, :, cb * 512:(cb + 1) * 512]), r_wb)
                bada = bada2[cb % 2]; r_bada = r_bada2[cb % 2]
                P.dma("sp", lambda e, bada=bada, cb=cb: e.dma_start(out=bada, in_=b_ada[:, cb * 512:(cb + 1) * 512].partition_broadcast(128)), r_bada)
                half = (cb % 2) * 512
                for which in range(2):
                    if which == 1 and dstc is None:
                        continue
                    bk = (cb * 2 + which) % 4
                    mm_group(banks[bk][:], rb[bk], [(rep[:, which * 8 + j, :], wb[:, j, :]) for j in range(8)], [r_rep, r_wb])
                    d_ap = (dst if which == 0 else dstc)[:, half:half + 512]
                    r_d = r_dst if which == 0 else r_dstc
                    bslice = bada
                    if kind == "sh":
                        V("dve", lambda e, bk=bk, d_ap=d_ap, bslice=bslice: e.tensor_tensor(out=d_ap, in0=banks[bk][:], in1=bslice, op=ALU.add),
                          [rb[bk], r_bada], [r_d])
                    else:
                        g_ap = gg[0][:, half:half + 512]
                        V("dve", lambda e, bk=bk, bslice=bslice: e.tensor_tensor(out=tmpA, in0=banks[bk][:], in1=bslice, op=ALU.add),
                          [rb[bk], r_bada], [r_tmpA])
                        V("dve", lambda e, d_ap=d_ap, g_ap=g_ap: e.scalar_tensor_tensor(out=d_ap, in0=tmpA, scalar=1.0, in1=g_ap, op0=ALU.add, op1=ALU.mult),
                          [r_tmpA, gg[1]], [r_d])
            if debug:
                P.dma("sp", lambda e: e.dma_start(out=dbg["d_g1"], in_=G1), r_dbg, [r_G1], no_waw=True, semres=r_G1)
            P.barrier()
            if stop == 1: raise _Stop()
            A = A_main

            distt = A.alloc([4 * NS], F32); r_distt = Res("distt")
            P.dma("sp", lambda e: e.dma_start(out=distt, in_=dist_t[:, :]), r_distt)
            Wf = A.alloc([NS, 4], F32); r_Wf = Res("Wf")
            Wb = A.alloc([NS, 4], F32); r_Wb = Res("Wb")
            for h in range(4):
                V("act", lambda e, h=h: e.activation(out=Wf[:, :, h], in_=distt[:, 0:NS], func=AF.Exp, scale=lg[:, h:h + 1]), [r_distt, r_lg], [r_Wf])
                V("act", lambda e, h=h: e.activation(out=Wb[:, :, h], in_=distt[:, NS:2 * NS], func=AF.Exp, scale=lg[:, 4 + h:5 + h]), [r_distt, r_lg], [r_Wb])
            V("dve", lambda e: e.tensor_tensor(out=Wf, in0=Wf, in1=distt[:, 2 * NS:3 * NS].unsqueeze(2).to_broadcast([128, NS, 4]), op=ALU.mult), [r_Wf, r_distt], [r_Wf])
            V("dve", lambda e: e.tensor_tensor(out=Wb, in0=Wb, in1=distt[:, 3 * NS:4 * NS].unsqueeze(2).to_broadcast([128, NS, 4]), op=ALU.mult), [r_Wb, r_distt], [r_Wb])

            NB = 2
            xt = [A.alloc([D], F32) for _ in range(NB)]; r_xt = [Res("xt%d" % i) for i in range(NB)]
            cst = [A.alloc([64], F32) for _ in range(NB)]; r_cst = [Res("cst%d" % i) for i in range(NB)]
            tmpx = A.alloc([D], F32); r_tmpx = Res("tmpx")
            junk = tmpx; r_junk = r_tmpx
            hx1 = A.alloc([D], BF16); hx = [hx1, hx1]; r_hx1 = Res("hx"); r_hx = [r_hx1, r_hx1]
            hxTw1 = A.alloc([8 * 128], BF16); hxTw = [hxTw1, hxTw1]; r_hxTw1 = Res("hxTw"); r_hxTw = [r_hxTw1, r_hxTw1]
            sm = [A.alloc([32], F32) for _ in range(NB)]; r_sm = [Res("sm%d" % i) for i in range(NB)]
            sqh = A.alloc([640], F32); r_sqh = Res("sqh")
            kb = A.alloc([128], BF16); r_kb = Res("kb")
            qb = A.alloc([8, 64], BF16); r_qb = Res("qb")
            ktb = [A.alloc([128], BF16) for _ in range(NB)]; r_ktb = [Res("ktb%d" % i) for i in range(NB)]
            v1 = [A.alloc([2, 65], BF16) for _ in range(NB)]; r_v1 = [Res("v1%d" % i) for i in range(NB)]
            for i in range(NB):
                V("pool", lambda e, i=i: e.memset(v1[i], 1.0), [], [r_v1[i]])
            kaug = A.alloc([4, 128], BF16); r_kaug = Res("kaug")
            vb = A.alloc([512], BF16); r_vb = Res("vb")

            def rope(H0, H1, cs, r_cs):
                H = H1 - H0
                riv = ri[:, H0:H1, :].rearrange("p h (i t) -> p h i t", t=2)
                rov = ro[:, H0:H1, :].rearrange("p h (i t) -> p h i t", t=2)
                cb_ = cs[:, 0:32].unsqueeze(1).to_broadcast([128, H, 32])
                sb_ = cs[:, 32:64].unsqueeze(1).to_broadcast([128, H, 32])
                t = [rt[i][:, 0:H, :] for i in range(2)]
                V("dve", lambda e: e.tensor_tensor(out=t[0], in0=riv[:, :, :, 0], in1=cb_, op=ALU.mult), [r_ri, r_cs], [r_rt[0]])
                V("dve", lambda e: e.tensor_tensor(out=t[1], in0=riv[:, :, :, 1], in1=sb_, op=ALU.mult), [r_ri, r_cs], [r_rt[1]])
                V("dve", lambda e: e.tensor_tensor(out=rov[:, :, :, 0], in0=t[0], in1=t[1], op=ALU.subtract), [r_rt[0], r_rt[1]], [r_ro])
                V("dve", lambda e: e.tensor_tensor(out=t[0], in0=riv[:, :, :, 0], in1=sb_, op=ALU.mult), [r_ri, r_cs], [r_rt[0]])
                V("dve", lambda e: e.tensor_tensor(out=t[1], in0=riv[:, :, :, 1], in1=cb_, op=ALU.mult), [r_ri, r_cs], [r_rt[1]])
                V("dve", lambda e: e.tensor_tensor(out=rov[:, :, :, 1], in0=t[0], in1=t[1], op=ALU.add), [r_rt[0], r_rt[1]], [r_ro])

            def rsqrt_mean(dst, src, r_dst, r_src, n):
                V("act", lambda e: e.activation(out=dst, in_=src, func=AF.Ln, scale=1.0 / n, bias=EPS), [r_src], [r_dst])
                V("act", lambda e: e.activation(out=dst, in_=dst, func=AF.Exp, scale=-0.5), [r_dst], [r_dst])

            def norm_mod_transpose(b, x_ap, r_x, Gm, r_Gm, SHm, r_SHm, dstT, r_dstT, tb):
                s_ = sm[b]; r_s = r_sm[b]
                V("act", lambda e: e.activation(out=junk, in_=x_ap, func=AF.Square, accum_out=s_[:, 0:1]), [r_x], [r_junk, r_s])
                rsqrt_mean(s_[:, 1:2], s_[:, 0:1], r_s, r_s, D)
                V("dve", lambda e: e.scalar_tensor_tensor(out=tmpx, in0=x_ap, scalar=s_[:, 1:2], in1=Gm, op0=ALU.mult, op1=ALU.mult),
                  [r_x, r_s, r_Gm], [r_tmpx])
                V("pool", lambda e: e.tensor_tensor(out=hx[b], in0=tmpx, in1=SHm, op=ALU.add), [r_tmpx, r_SHm], [r_hx[b]])
                pt = bank_bf(tb)
                for j in range(8):
                    V("pe", lambda e, j=j: e.transpose(out=pt[:, j * 128:(j + 1) * 128], in_=hx[b][:, j * 128:(j + 1) * 128], identity=ident_b),
                      [r_hx[b], r_idb], [rb[tb]], inc=(j == 7), pe_acc=(j > 0))
                V("act", lambda e: e.activation(out=dstT, in_=pt, func=AF.Copy), [rb[tb]], [r_dstT])

            xpv = xp.rearrange("(s p) n -> s p n", p=128)
            cosv = cos_t.rearrange("(s p) n -> s p n", p=128)
            sinv = sin_t.rearrange("(s p) n -> s p n", p=128)

            for s in range(NS):
                b = s % NB
                own = s < TPC
                isctx = s >= NT
                P.dma("sp", lambda e, s=s, b=b: e.dma_start(out=xt[b], in_=xpv[s]), r_xt[b])
                P.dma("sp", lambda e, s=s, b=b: e.dma_start(out=cst[b][:, 0:32], in_=cosv[s]), r_cst[b])
                P.dma("sp", lambda e, s=s, b=b: e.dma_start(out=cst[b][:, 32:64], in_=sinv[s]), r_cst[b])
                if own:
                    dstT = hxT_own[:, s, :]; r_dT = r_hxT_own[s]
                else:
                    dstT = hxTw[b]; r_dT = r_hxTw[b]
                norm_mod_transpose(b, xt[b], r_xt[b], G1c if isctx else G1, r_G1c if isctx else r_G1,
                                   SH1c if isctx else SH1, r_SH1c if isctx else r_SH1, dstT, r_dT, tb=b)
                if debug and s == 0:
                    V("dve", lambda e: e.tensor_copy(out=tmpx, in_=hx[0]), [r_hx[0]], [r_tmpx])
                    P.dma("sp", lambda e: e.dma_start(out=dbg["d_hx"], in_=tmpx), r_dbg, [r_tmpx], no_waw=True, semres=r_tmpx)
                hT = dstT.rearrange("p (j t) -> p j t", j=8)
                mm_group(banks[2][:, 0:256], rb[2], [(hT[:, j, :], w_in_sb[:, j, 512:768]) for j in range(8)], [r_dT, r_win])
                mm_group(banks[2][:, 256:512], rb[2], [(hT[:, j, :], w_in_sb[:, j, 1536:1792]) for j in range(8)], [r_dT, r_win])
                mm_group(banks[3][:], rb[3], [(hT[:, j, :], w_in_sb[:, j, 1024:1536]) for j in range(8)], [r_dT, r_win])
                if own:
                    mm_group(banks[4][:], rb[4], [(hT[:, j, :], w_in_sb[:, j, 0:512]) for j in range(8)], [r_dT, r_win])
                s_ = sm[b]; r_s = r_sm[b]
                H0 = 0 if own else 8
                V("act", lambda e: e.activation(out=sqh[:, 512:640], in_=banks[2][:, 0:128], func=AF.Square), [rb[2]], [r_sqh])
                if own:
                    V("act", lambda e: e.activation(out=sqh[:, 0:512], in_=banks[4][:], func=AF.Square), [rb[4]], [r_sqh])
                nh = 10 - H0
                hs = s_[:, 8 + H0:18]
                V("dve", lambda e: e.tensor_reduce(out=hs, in_=sqh[:, H0 * 64:640].rearrange("p (h d) -> p h d", d=64), axis=AX.X, op=ALU.add),
                  [r_sqh], [r_s])
                rsqrt_mean(hs, hs, r_s, r_s, 64)
                V("dve", lambda e: e.tensor_tensor(out=ri[:, 8:10, :], in0=banks[2][:, 0:128].rearrange("p (h d) -> p h d", d=64),
                                                   in1=s_[:, 16:18].unsqueeze(2).to_broadcast([128, 2, 64]), op=ALU.mult), [rb[2], r_s], [r_ri])
                V("dve", lambda e: e.tensor_tensor(out=ri[:, 8:10, :], in0=ri[:, 8:10, :], in1=kn_bc.unsqueeze(1).to_broadcast([128, 2, 64]), op=ALU.mult),
                  [r_ri, r_kn], [r_ri])
                if own:
                    V("dve", lambda e: e.tensor_tensor(out=ri[:, 0:8, :], in0=banks[4][:].rearrange("p (h d) -> p h d", d=64),
                                                       in1=s_[:, 8:16].unsqueeze(2).to_broadcast([128, 8, 64]), op=ALU.mult), [rb[4], r_s], [r_ri])
                    V("dve", lambda e: e.tensor_tensor(out=ri[:, 0:8, :], in0=ri[:, 0:8, :], in1=qn_bc.unsqueeze(1).to_broadcast([128, 8, 64]), op=ALU.mult),
                      [r_ri, r_qn], [r_ri])
                V("act", lambda e: e.activation(out=ri[:, 10:14, :], in_=banks[3][:, 0:256].rearrange("p (h d) -> p h d", d=64), func=AF.Copy, scale=0.125),
                  [rb[3]], [r_ri])
                rope(H0, 14, cst[b], r_cst[b])
                V("dve", lambda e: e.tensor_copy(out=kb, in_=ro[:, 8:10, :]), [r_ro], [r_kb])
                ptk = bank_bf(5)
                V("pe", lambda e: e.transpose(out=ptk[:, 0:128], in_=kb, identity=ident_b), [r_kb, r_idb], [rb[5]])
                V("act", lambda e, b=b: e.activation(out=ktb[b], in_=ptk[:, 0:128], func=AF.Copy), [rb[5]], [r_ktb[b]])
                P.dma("pool", lambda e, s=s, b=b: e.dma_start(out=KT_d[:, s * 128:(s + 1) * 128], in_=ktb[b]), r_KTd, [r_ktb[b]], no_waw=True, semres=r_ktb[b])
                V("act", lambda e, b=b: e.activation(out=v1[b][:, :, 0:64], in_=banks[2][:, 128:256].rearrange("p (h d) -> p h d", d=64), func=AF.Copy),
                  [rb[2]], [r_v1[b]])
                P.dma("pool", lambda e, s=s, b=b: e.dma_start(out=V_d[s], in_=v1[b]), r_Vd, [r_v1[b]], no_waw=True, semres=r_v1[b])
                if own:
                    ptq = bank_bf(6)
                    V("dve", lambda e: e.tensor_copy(out=qb, in_=ro[:, 0:8, :]), [r_ro], [r_qb])
                    for g in range(4):
                        V("pe", lambda e, g=g: e.transpose(out=ptq[0:64, g * 128:(g + 1) * 128], in_=qb[:, g, :], identity=ident_b),
                          [r_qb, r_idb], [rb[6]], inc=False, pe_acc=(g > 0))
                    for g in range(4):
                        V("pe", lambda e, g=g: e.transpose(out=ptq[64:128, g * 128:(g + 1) * 128], in_=qb[:, 4 + g, :], identity=ident_b),
                          [r_qb, r_idb], [rb[6]], inc=(g == 3), pe_acc=True)
                    V("act", lambda e, s=s: e.activation(out=QT[:, s, :], in_=ptq[:, 0:512], func=AF.Copy), [rb[6]], [r_QT])
                V("dve", lambda e, s=s: e.tensor_tensor(out=kaug[:, :, 0:64], in0=ro[:, 10:14, :], in1=Wf[:, s, :].unsqueeze(2).to_broadcast([128, 4, 64]), op=ALU.mult),
                  [r_ro, r_Wf], [r_kaug])
                V("dve", lambda e, s=s: e.tensor_tensor(out=kaug[:, :, 64:128], in0=ro[:, 10:14, :], in1=Wb[:, s, :].unsqueeze(2).to_broadcast([128, 4, 64]), op=ALU.mult),
                  [r_ro, r_Wb], [r_kaug])
                V("act", lambda e: e.activation(out=vb[:, 0:256], in_=banks[3][:, 256:512], func=AF.Copy), [rb[3]], [r_vb])
                V("act", lambda e: e.activation(out=vb[:, 256:512], in_=banks[2][:, 256:512], func=AF.Copy), [rb[2]], [r_vb])
                for h in range(4):
                    P.op("pe", lambda e, h=h: e.matmul(banks[7][:, h * 128:(h + 1) * 128], kaug[:, h, :], vb[:, h * 128:(h + 1) * 128], start=True, stop=True),
                         [r_kaug, r_vb], [rb[7]], inc=(h == 3), pe_acc=(h > 0))
                if own:
                    V("dve", lambda e, s=s: e.tensor_copy(out=Ust[:, s, :], in_=banks[7][:]), [rb[7]], [r_Ust[s]])
                else:
                    V("dve", lambda e: e.tensor_tensor(out=Sacc, in0=Sacc, in1=banks[7][:], op=ALU.add), [r_Sacc, rb[7]], [r_Sacc])

            P.barrier()
            if stop == 2: raise _Stop()
            A.release(m_sw)

            dmt = A.alloc([256], F32); r_dmt = Res("dmt")
            P.dma("sp", lambda e: e.dma_start(out=dmt, in_=dmat_t[:, :]), r_dmt)
            pct = A.alloc([2], F32); r_pct = Res("pct")
            P.dma("sp", lambda e: e.dma_start(out=pct, in_=pcol_t[:, :]), r_pct)
            DT = A.alloc([4, 128], F32); r_DT = Res("DT")
            tmpd = A.alloc([128], F32); r_tmpd = Res("tmpd")
            for h in range(4):
                V("dve", lambda e, h=h: e.tensor_scalar(out=tmpd, in0=dmt[:, 0:128], scalar1=lg[:, h:h + 1], scalar2=None, op0=ALU.mult), [r_dmt, r_lg], [r_tmpd])
                V("dve", lambda e, h=h: e.scalar_tensor_tensor(out=tmpd, in0=dmt[:, 128:256], scalar=lg[:, 4 + h:5 + h], in1=tmpd, op0=ALU.mult, op1=ALU.add),
                  [r_dmt, r_lg, r_tmpd], [r_tmpd])
                V("act", lambda e, h=h: e.activation(out=DT[:, h, :], in_=tmpd, func=AF.Exp), [r_tmpd], [r_DT])
            xif = A.alloc([4], F32); xib = A.alloc([4], F32); r_xi = Res("xi")
            g128 = A.alloc([4], F32); r_g128 = Res("g128")
            for h in range(4):
                V("act", lambda e, h=h: e.activation(out=xif[:, h:h + 1], in_=pct[:, 0:1], func=AF.Exp, scale=lg[:, h:h + 1]), [r_pct, r_lg], [r_xi])
                V("act", lambda e, h=h: e.activation(out=xib[:, h:h + 1], in_=pct[:, 1:2], func=AF.Exp, scale=lg[:, 4 + h:5 + h]), [r_pct, r_lg], [r_xi])
            V("act", lambda e: e.activation(out=g128[0:64, :], in_=lg[0:64, 0:4], func=AF.Exp, scale=128.0), [r_lg], [r_g128])
            V("act", lambda e: e.activation(out=g128[64:128, :], in_=lg[64:128, 4:8], func=AF.Exp, scale=128.0), [r_lg], [r_g128])
            if SUB == 1: raise _Stop()
            Sst = A.alloc([TPC, 512], BF16); r_Sst = [Res("Sst%d" % i) for i in range(TPC)]
            cur = A.alloc([512], F32); r_cur = Res("cur")
            V("dve", lambda e: e.tensor_copy(out=cur, in_=Sacc), [r_Sacc], [r_cur])
            for c in range(TPC):
                V("dve", lambda e, c=c: e.tensor_copy(out=Sst[0:64, c, :], in_=cur[0:64, :]), [r_cur], [r_Sst[c]])
                for h in range(4):
                    V("dve", lambda e, c=c, h=h: e.scalar_tensor_tensor(out=cur[0:64, h * 128:(h + 1) * 128], in0=cur[0:64, h * 128:(h + 1) * 128],
                                                                       scalar=g128[0:64, h:h + 1], in1=Ust[0:64, c, h * 128:(h + 1) * 128],
                                                                       op0=ALU.mult, op1=ALU.add), [r_cur, r_g128, r_Ust[c]], [r_cur])
            for c in range(TPC - 1, -1, -1):
                V("dve", lambda e, c=c: e.tensor_copy(out=Sst[64:128, c, :], in_=cur[64:128, :]), [r_cur], [r_Sst[c]])
                for h in range(4):
                    V("dve", lambda e, c=c, h=h: e.scalar_tensor_tensor(out=cur[64:128, h * 128:(h + 1) * 128], in0=cur[64:128, h * 128:(h + 1) * 128],
                                                                       scalar=g128[64:128, h:h + 1], in1=Ust[64:128, c, h * 128:(h + 1) * 128],
                                                                       op0=ALU.mult, op1=ALU.add), [r_cur, r_g128, r_Ust[c]], [r_cur])
            if SUB == 2: raise _Stop()
            gng, r_gng = bload(gn_g, 512, "gng")
            gnb, r_gnb = bload(gn_b, 512, "gnb")
            cs2 = [A.alloc([64], F32) for _ in range(2)]; r_cs2 = [Res("cs2_0"), Res("cs2_1")]
            qk = A.alloc([8, 64], BF16); r_qk = Res("qk")
            qaug = A.alloc([4, 128], BF16); r_qaug = Res("qaug")
            qkT = A.alloc([4, 128], BF16); r_qkT = Res("qkT")
            qaT = A.alloc([4, 128], BF16); r_qaT = Res("qaT")
            qpad = A.alloc([4, 128], BF16); r_qpad = Res("qpad")
            V("pool", lambda e: e.memset(qpad, 0.0), [], [r_qpad])
            vb2 = A.alloc([512], BF16); r_vb2 = Res("vb2")
            scm = A.alloc([512], BF16); r_scm = Res("scm")
            gate = A.alloc([512], F32); r_gate = Res("gate")
            osb = A.alloc([512], F32); r_osb = Res("osb")
            osq = A.alloc([512], F32); r_osq = Res("osq")
            st2 = A.alloc([16], F32); r_st2 = Res("st2")
            for c in range(TPC):
                b = c % 2
                P.dma("sp", lambda e, c=c, b=b: e.dma_start(out=cs2[b][:, 0:32], in_=cosv[c]), r_cs2[b])
                P.dma("sp", lambda e, c=c, b=b: e.dma_start(out=cs2[b][:, 32:64], in_=sinv[c]), r_cs2[b])
                hT = hxT_own[:, c, :].rearrange("p (j t) -> p j t", j=8)
                r_dT = r_hxT_own[c]
                mm_group(banks[0][:, 0:256], rb[0], [(hT[:, j, :], w_in_sb[:, j, 768:1024]) for j in range(8)], [r_dT, r_win])
                mm_group(banks[0][:, 256:512], rb[0], [(hT[:, j, :], w_in_sb[:, j, 1024:1280]) for j in range(8)], [r_dT, r_win])
                mm_group(banks[1][:], rb[1], [(hT[:, j, :], w_in_sb[:, j, 1280:1792]) for j in range(8)], [r_dT, r_win])
                mm_group(banks[2][:], rb[2], [(hT[:, j, :], w_in_sb[:, j, 1792:2304]) for j in range(8)], [r_dT, r_win])
                V("act", lambda e: e.activation(out=ri[:, 0:4, :], in_=banks[0][:, 0:256].rearrange("p (h d) -> p h d", d=64), func=AF.Copy), [rb[0]], [r_ri])
                V("act", lambda e: e.activation(out=ri[:, 4:8, :], in_=banks[0][:, 256:512].rearrange("p (h d) -> p h d", d=64), func=AF.Copy, scale=0.125), [rb[0]], [r_ri])
                if SUB == 3: raise _Stop()
                rope(0, 8, cs2[b], r_cs2[b])
                if SUB == 4: raise _Stop()
                V("dve", lambda e: e.tensor_copy(out=qk, in_=ro[:, 0:8, :]), [r_ro], [r_qk])
                if SUB == 41: raise _Stop()
                V("dve", lambda e: e.tensor_tensor(out=qaug[:, :, 0:64], in0=ro[:, 0:4, :], in1=xif.unsqueeze(2).to_broadcast([128, 4, 64]), op=ALU.mult), [r_ro, r_xi], [r_qaug])
                V("dve", lambda e: e.tensor_tensor(out=qaug[:, :, 64:128], in0=ro[:, 0:4, :], in1=xib.unsqueeze(2).to_broadcast([128, 4, 64]), op=ALU.mult), [r_ro, r_xi], [r_qaug])
                if SUB == 42: raise _Stop()
                V("act", lambda e: e.activation(out=vb2, in_=banks[1][:], func=AF.Copy), [rb[1]], [r_vb2])
                if SUB == 43: raise _Stop()
                V("act", lambda e: e.activation(out=gate, in_=banks[2][:], func=AF.Silu), [rb[2]], [r_gate])
                if SUB == 5: raise _Stop()
                pt3 = bank_bf(3)
                qkf = qk.rearrange("p h d -> p (h d)")
                for i in range(4):
                    V("pe", lambda e, i=i: e.transpose(out=pt3[:, i * 128:(i + 1) * 128], in_=qkf[:, i * 128:(i + 1) * 128], identity=ident_b),
                      [r_qk, r_idb], [rb[3]], inc=False, pe_acc=(i > 0))
                for h in range(4):
                    V("pe", lambda e, h=h: e.transpose(out=pt3[:, 512 + h * 128:512 + (h + 1) * 128], in_=qaug[:, h, :], identity=ident_b),
                      [r_qaug, r_idb], [rb[3]], inc=(h == 3), pe_acc=True)
                if SUB == 51: raise _Stop()
                V("dve", lambda e: e.tensor_copy(out=qkT.rearrange("p a b -> p (a b)"), in_=pt3[:, 0:512]), [rb[3]], [r_qkT])
                if SUB == 52: raise _Stop()
                for h in range(4):
                    pr = (h % 2) * 64
                    V("dve", lambda e, h=h, pr=pr: e.tensor_copy(out=qpad[pr:pr + 64, h, :], in_=pt3[pr:pr + 64, (h // 2) * 128:(h // 2 + 1) * 128]), [rb[3]], [r_qpad])
                if SUB == 53: raise _Stop()
                V("act", lambda e: e.activation(out=qaT.rearrange("p a b -> p (a b)"), in_=pt3[:, 512:1024], func=AF.Copy), [rb[3]], [r_qaT])
                if SUB == 6: raise _Stop()
                for h in range(4):
                    P.op("pe", lambda e, h=h: e.matmul(banks[4][:, h * 128:(h + 1) * 128], qkT[:, 2 + h // 2, :], qpad[:, h, :], start=True, stop=True),
                         [r_qkT, r_qpad], [rb[4]], inc=(h == 3), pe_acc=(h > 0))
                V("dve", lambda e: e.tensor_tensor(out=scm, in0=banks[4][:], in1=DT.rearrange("p a b -> p (a b)"), op=ALU.mult), [rb[4], r_DT], [r_scm])
                for h in range(4):
                    P.op("pe", lambda e, h=h: e.matmul(banks[5][:, h * 128:(h + 1) * 128], scm[:, h * 128:(h + 1) * 128], vb2[:, h * 128:(h + 1) * 128], start=True, stop=False),
                         [r_scm, r_vb2], [rb[5]], inc=False, pe_acc=(h > 0))
                    P.op("pe", lambda e, h=h, c=c: e.matmul(banks[5][:, h * 128:(h + 1) * 128], qaT[:, h, :], Sst[:, c, h * 128:(h + 1) * 128], start=False, stop=True),
                         [r_qaT, r_Sst[c]], [rb[5]], inc=(h == 3), pe_acc=True)
                if SUB == 7: raise _Stop()
                V("act", lambda e: e.activation(out=osb, in_=banks[5][:], func=AF.Copy), [rb[5]], [r_osb])
                V("dve", lambda e: e.tensor_reduce(out=st2[:, 0:4], in_=osb.rearrange("p (h d) -> p h d", d=128), axis=AX.X, op=ALU.add), [r_osb], [r_st2])
                V("dve", lambda e: e.tensor_tensor(out=osq, in0=osb, in1=osb, op=ALU.mult), [r_osb], [r_osq])
                V("dve", lambda e: e.tensor_reduce(out=st2[:, 4:8], in_=osq.rearrange("p (h d) -> p h d", d=128), axis=AX.X, op=ALU.add), [r_osq], [r_st2])
                V("dve", lambda e: e.tensor_scalar(out=st2[:, 0:4], in0=st2[:, 0:4], scalar1=1.0 / 128, scalar2=None, op0=ALU.mult), [r_st2], [r_st2])
                V("dve", lambda e: e.tensor_tensor(out=st2[:, 8:12], in0=st2[:, 0:4], in1=st2[:, 0:4], op=ALU.mult), [r_st2], [r_st2])
                V("dve", lambda e: e.scalar_tensor_tensor(out=st2[:, 4:8], in0=st2[:, 4:8], scalar=1.0 / 128, in1=st2[:, 8:12], op0=ALU.mult, op1=ALU.subtract), [r_st2], [r_st2])
                V("act", lambda e: e.activation(out=st2[:, 4:8], in_=st2[:, 4:8], func=AF.Ln, bias=EPS, scale=1.0), [r_st2], [r_st2])
                V("act", lambda e: e.activation(out=st2[:, 4:8], in_=st2[:, 4:8], func=AF.Exp, scale=-0.5), [r_st2], [r_st2])
                for h in range(4):
                    V("dve", lambda e, h=h: e.tensor_scalar(out=osb[:, h * 128:(h + 1) * 128], in0=osb[:, h * 128:(h + 1) * 128], scalar1=st2[:, h:h + 1],
                                                           scalar2=st2[:, 4 + h:5 + h], op0=ALU.subtract, op1=ALU.mult), [r_osb, r_st2], [r_osb])
                V("dve", lambda e: e.tensor_tensor(out=osb, in0=osb, in1=gng, op=ALU.mult), [r_osb, r_gng], [r_osb])
                V("dve", lambda e: e.tensor_tensor(out=osb, in0=osb, in1=gnb, op=ALU.add), [r_osb, r_gnb], [r_osb])
                V("dve", lambda e, c=c: e.tensor_tensor(out=retx[:, c, :], in0=osb, in1=gate, op=ALU.mult), [r_osb, r_gate], [r_retx[c]])
                if debug:
                    V("dve", lambda e: e.tensor_tensor(out=osq, in0=osb, in1=gate, op=ALU.mult), [r_osb, r_gate], [r_osq])
                    P.dma("sp", lambda e, c=c: e.dma_start(out=dbg["d_ret"][c * 128:(c + 1) * 128, :], in_=osq), r_dbg, [r_osq], no_waw=True, semres=r_osq)

            P.barrier()
            if stop == 3: raise _Stop()
            A.release(m_persist)

            attnx = A.alloc([TPC, 512], BF16); r_attnx = [Res("attnx%d" % i) for i in range(TPC)]
            m_att = A.mark()
            KT = A.alloc([NS * 128], BF16); r_KT = Res("KT")
            V1 = A.alloc([NS, 130], BF16); r_V1 = Res("V1")
            P.dma("sp", lambda e: e.dma_start(out=KT, in_=KT_d[:, :]), r_KT, [r_KTd])
            SCH = 10
            for s0 in range(0, NS, SCH):
                s1 = min(NS, s0 + SCH)
                P.dma("act", lambda e, s0=s0, s1=s1: e.dma_start(out=V1[:, s0:s1, :], in_=V_d[s0:s1].rearrange("s p c -> p s c")), r_V1, [r_Vd], no_waw=True)
            if debug:
                dtmp = A.alloc([NS * 130], F32); r_dtmp = Res("dtmp")
                V("dve", lambda e: e.tensor_copy(out=dtmp[:, 0:NS * 128], in_=KT), [r_KT], [r_dtmp])
                P.dma("sp", lambda e: e.dma_start(out=dbg["d_kt"], in_=dtmp[:, 0:NS * 128]), r_dbg, [r_dtmp], no_waw=True, semres=r_dtmp)
                V("dve", lambda e: e.tensor_copy(out=dtmp, in_=V1.rearrange("p a b -> p (a b)")), [r_V1], [r_dtmp])
                P.dma("sp", lambda e: e.dma_start(out=dbg["d_v1"], in_=dtmp), r_dbg, [r_dtmp], no_waw=True, semres=r_dtmp)
                V("dve", lambda e: e.tensor_copy(out=dtmp[:, 0:TPC * 512], in_=QT.rearrange("p a b -> p (a b)")), [r_QT], [r_dtmp])
                P.dma("sp", lambda e: e.dma_start(out=dbg["d_qt"], in_=dtmp[:, 0:TPC * 512]), r_dbg, [r_dtmp], no_waw=True, semres=r_dtmp)
            NPT = 4
            PT = [A.alloc([512], BF16) for _ in range(NPT)]; r_PT = [Res("PT%d" % i) for i in range(NPT)]
            OT = A.alloc([512], F32); r_OT = Res("OT")
            rcp = A.alloc([8], F32); r_rcp = Res("rcp")
            QP = [[A.alloc([512], BF16) for _ in range(2)] for _ in range(2)]
            r_QP = [[Res("QP%d%d" % (i, j)) for j in range(2)] for i in range(2)]
            for i in range(2):
                for j in range(2):
                    V("pool", lambda e, i=i, j=j: e.memset(QP[i][j], 0.0), [], [r_QP[i][j]])
            groups = [(qt, kvh) for qt in range(TPC) for kvh in range(2)]
            iters = [(gi, kt) for gi in range(len(groups)) for kt in range(NS)]
            NSB = 3

            def prep_q(gi):
                qt, kvh = groups[gi]
                pr = kvh * 64
                qp = QP[kvh][qt % 2]; r_qp = r_QP[kvh][qt % 2]
                V("pool", lambda e: e.tensor_copy(out=qp[pr:pr + 64, :], in_=QT[pr:pr + 64, qt, :]), [r_QT], [r_qp])

            def emit_S(i):
                gi, kt = iters[i]
                qt, kvh = groups[gi]
                qp = QP[kvh][qt % 2]; r_qp = r_QP[kvh][qt % 2]
                sb_ = i % NSB
                pb = i % NPT
                P.op("pe", lambda e: e.matmul(banks[sb_][:], KT[:, kt * 128:(kt + 1) * 128], qp, start=True, stop=True), [r_KT, r_qp], [rb[sb_]])
                V("act", lambda e: e.activation(out=PT[pb], in_=banks[sb_][:], func=AF.Exp, scale=0.125, bias=negC), [rb[sb_], r_negC], [r_PT[pb]])

            def emit_PV(i):
                gi, kt = iters[i]
                qt, kvh = groups[gi]
                ob = 4 + gi % 2
                pb = i % NPT
                P.op("pe", lambda e: e.matmul(banks[ob][0:65, :], V1[:, kt, kvh * 65:(kvh + 1) * 65], PT[pb], start=(kt == 0), stop=(kt == NS - 1)),
                     [r_V1, r_PT[pb]], [rb[ob]], inc=(kt == NS - 1), pe_acc=(kt > 0))

            def post_a(gi):
                ob = 4 + gi % 2
                V("dve", lambda e: e.tensor_copy(out=OT[0:65, :], in_=banks[ob][0:65, :]), [rb[ob]], [r_OT])

            def post_b(gi):
                qt, kvh = groups[gi]
                for g in range(4):
                    V("pe", lambda e, g=g: e.transpose(out=banks[6][:, g * 66:g * 66 + 65], in_=OT[0:65, g * 128:(g + 1) * 128], identity=ident_f[0:65, 0:65]),
                      [r_OT, r_idf], [rb[6]], inc=(g == 3), pe_acc=(g > 0))
                o4 = banks[6][:, 0:264].rearrange("p (g c) -> p g c", c=66)
                V("dve", lambda e: e.reciprocal(out=rcp[:, 0:4], in_=o4[:, :, 64]), [rb[6]], [r_rcp])
                V("dve", lambda e: e.tensor_tensor(out=attnx[:, qt, kvh * 256:(kvh + 1) * 256].rearrange("p (g d) -> p g d", d=64), in0=o4[:, :, 0:64],
                                                   in1=rcp[:, 0:4].unsqueeze(2).to_broadcast([128, 4, 64]), op=ALU.mult), [rb[6], r_rcp], [r_attnx[qt]])

            prep_q(0)
            if len(groups) > 1:
                prep_q(1)
            nI = len(iters)
            pending_b = {}
            for i in range(nI + 1):
                if i < nI:
                    gi, kt = iters[i]
                    if kt == 0 and gi + 2 < len(groups) and gi >= 0:
                        pass
                    emit_S(i)
                if i >= 1:
                    emit_PV(i - 1)
                    gj, ktj = iters[i - 1]
                    if ktj == NS - 1:
                        post_a(gj)
                        pending_b[i + 3] = gj
                        if gj + 2 < len(groups):
                            prep_q(gj + 2)
                if i in pending_b:
                    post_b(pending_b.pop(i))
            for k in sorted(pending_b):
                post_b(pending_b[k])
            if debug:
                for qt in range(TPC):
                    V("dve", lambda e, qt=qt: e.tensor_copy(out=OT, in_=attnx[:, qt, :]), [r_attnx[qt]], [r_OT])
                    P.dma("sp", lambda e, qt=qt: e.dma_start(out=dbg["d_attn"][qt * 128:(qt + 1) * 128, :], in_=OT), r_dbg, [r_OT], no_waw=True, semres=r_OT)
            P.barrier()
            if stop == 4: raise _Stop()
            A.release(m_att)

            w_out_sb = A.alloc([8, D], BF16); r_wout = Res("wout")
            w_out_v = w_out.rearrange("(j p) n -> p j n", p=128)
            for c0 in range(0, D, 256):
                P.dma("pool", lambda e, c0=c0: e.dma_start(out=w_out_sb[:, :, c0:c0 + 256], in_=w_out_v[:, :, c0:c0 + 256]), r_wout, no_waw=True)
            wrt = A.alloc([8, 36], F32); r_wrt = Res("wrt")
            P.dma("sp", lambda e: e.dma_start(out=wrt, in_=w_rt.rearrange("(j p) n -> p j n", p=128)), r_wrt)
            brt, r_brt = bload(b_rt, 36, "brt")
            mixT = A.alloc([8, 128], BF16); r_mixT = Res("mixT")
            xr = [A.alloc([D], F32) for _ in range(2)]; r_xr = [Res("xr0"), Res("xr1")]
            xn = A.alloc([D], F32); r_xn = Res("xn")
            h2 = A.alloc([D], F32); r_h2 = Res("h2")
            h2b = A.alloc([D], BF16); r_h2b = Res("h2b")
            h2Tf = A.alloc([8, 128], F32); r_h2Tf = Res("h2Tf")
            junk2 = A.alloc([D], BF16); r_junk2 = Res("junk2")
            lgt = A.alloc([36], F32); r_lgt = Res("lgt")
            msk = A.alloc([32], F32); r_msk = Res("msk")
            m8 = A.alloc([8], F32); r_m8 = Res("m8")
            sm3 = A.alloc([16], F32); r_sm3 = Res("sm3")
            oh = A.alloc([64], F32); r_oh = Res("oh")
            xown = xp.rearrange("(s p) n -> s p n", p=128)
            for c in range(TPC):
                b = c % 2
                P.dma("sp", lambda e, c=c, b=b: e.dma_start(out=xr[b], in_=xown[c]), r_xr[b])
                pt0 = bank_bf(0)
                for j in range(8):
                    src = attnx[:, c, j * 128:(j + 1) * 128] if j < 4 else retx[:, c, (j - 4) * 128:(j - 3) * 128]
                    rs_ = r_attnx[c] if j < 4 else r_retx[c]
                    V("pe", lambda e, j=j, src=src: e.transpose(out=pt0[:, j * 128:(j + 1) * 128], in_=src, identity=ident_b), [rs_, r_idb], [rb[0]], inc=(j == 7), pe_acc=(j > 0))
                V("act", lambda e: e.activation(out=mixT.rearrange("p a b -> p (a b)"), in_=pt0, func=AF.Copy), [rb[0]], [r_mixT])
                for hf in range(2):
                    mm_group(banks[1 + hf][:], rb[1 + hf], [(mixT[:, j, :], w_out_sb[:, j, hf * 512:(hf + 1) * 512]) for j in range(8)], [r_mixT, r_wout])
                for hf in range(2):
                    sl = slice(hf * 512, (hf + 1) * 512)
                    V("dve", lambda e, hf=hf, sl=sl: e.tensor_tensor(out=xn[:, sl], in0=banks[1 + hf][:], in1=GT1[:, sl], op=ALU.mult), [rb[1 + hf], r_GT1], [r_xn])
                V("dve", lambda e, b=b: e.tensor_tensor(out=xn, in0=xn, in1=xr[b], op=ALU.add), [r_xn, r_xr[b]], [r_xn])
                P.dma("sp", lambda e, c=c: e.dma_start(out=XN_d[c * 128:(c + 1) * 128, :], in_=xn), r_XNd, [r_xn], no_waw=True, semres=r_xn)
                if debug:
                    P.dma("sp", lambda e, c=c: e.dma_start(out=dbg["d_xnew"][c * 128:(c + 1) * 128, :], in_=xn), r_dbg, [r_xn], no_waw=True, semres=r_xn)
                V("act", lambda e: e.activation(out=junk2, in_=xn, func=AF.Square, accum_out=sm3[:, 0:1]), [r_xn], [r_junk2, r_sm3])
                V("act", lambda e: e.activation(out=sm3[:, 1:2], in_=sm3[:, 0:1], func=AF.Ln, scale=1.0 / D, bias=EPS), [r_sm3], [r_sm3])
                V("act", lambda e: e.activation(out=sm3[:, 1:2], in_=sm3[:, 1:2], func=AF.Exp, scale=-0.5), [r_sm3], [r_sm3])
                V("dve", lambda e: e.scalar_tensor_tensor(out=h2, in0=xn, scalar=sm3[:, 1:2], in1=G2, op0=ALU.mult, op1=ALU.mult), [r_xn, r_sm3, r_G2], [r_h2])
                V("dve", lambda e: e.tensor_tensor(out=h2, in0=h2, in1=SH2, op=ALU.add), [r_h2, r_SH2], [r_h2])
                V("pool", lambda e: e.tensor_copy(out=h2b, in_=h2), [r_h2], [r_h2b])
                pt3 = bank_bf(3)
                for j in range(8):
                    V("pe", lambda e, j=j: e.transpose(out=pt3[:, j * 128:(j + 1) * 128], in_=h2b[:, j * 128:(j + 1) * 128], identity=ident_b), [r_h2b, r_idb], [rb[3]], inc=(j == 7), pe_acc=(j > 0))
                V("act", lambda e, c=c: e.activation(out=h2T[:, :, c * 128:(c + 1) * 128], in_=pt3.rearrange("p (j t) -> p j t", j=8), func=AF.Copy), [rb[3]], [r_h2T[c]])
                for j in range(8):
                    bk = 4 + j // 4
                    V("pe", lambda e, j=j, bk=bk: e.transpose(out=banks[bk][:, (j % 4) * 128:(j % 4 + 1) * 128], in_=h2[:, j * 128:(j + 1) * 128], identity=ident_f), [r_h2, r_idf], [rb[bk]],
                      inc=(j % 4 == 3), pe_acc=(j % 4 > 0))
                V("dve", lambda e: e.tensor_copy(out=h2Tf[:, 0:4, :].rearrange("p a b -> p (a b)"), in_=banks[4][:]), [rb[4]], [r_h2Tf])
                V("dve", lambda e: e.tensor_copy(out=h2Tf[:, 4:8, :].rearrange("p a b -> p (a b)"), in_=banks[5][:]), [rb[5]], [r_h2Tf])
                mm_group(banks[6][:, 0:36], rb[6], [(h2Tf[:, j, :], wrt[:, j, :]) for j in range(8)], [r_h2Tf, r_wrt])
                V("dve", lambda e: e.tensor_tensor(out=lgt, in0=banks[6][:, 0:36], in1=brt, op=ALU.add), [rb[6], r_brt], [r_lgt])
                V("dve", lambda e: e.tensor_reduce(out=sm3[:, 2:3], in_=lgt[:, 0:4], axis=AX.X, op=ALU.max), [r_lgt], [r_sm3])
                V("dve", lambda e: e.tensor_scalar(out=oh[:, 0:4], in0=lgt[:, 0:4], scalar1=sm3[:, 2:3], scalar2=None, op0=ALU.is_equal), [r_lgt, r_sm3], [r_oh])
                V("dve", lambda e: e.tensor_scalar(out=sm3[:, 3:4], in0=sm3[:, 2:3], scalar1=-1.0, scalar2=None, op0=ALU.mult), [r_sm3], [r_sm3])
                V("act", lambda e: e.activation(out=oh[:, 8:12], in_=lgt[:, 0:4], func=AF.Exp, bias=sm3[:, 3:4], scale=1.0, accum_out=sm3[:, 4:5]), [r_lgt, r_sm3], [r_oh, r_sm3])
                V("dve", lambda e: e.reciprocal(out=sm3[:, 5:6], in_=sm3[:, 4:5]), [r_sm3], [r_sm3])
                V("dve", lambda e: e.tensor_scalar(out=oh[:, 4:8], in0=oh[:, 0:4], scalar1=-1.0, scalar2=1e30, op0=ALU.add, op1=ALU.mult), [r_oh], [r_oh])
                V("dve", lambda e: e.tensor_tensor(out=msk.rearrange("p (g j) -> p g j", j=8), in0=lgt[:, 4:36].rearrange("p (g j) -> p g j", j=8),
                                                   in1=oh[:, 4:8].unsqueeze(2).to_broadcast([128, 4, 8]), op=ALU.add), [r_lgt, r_oh], [r_msk])
                V("dve", lambda e: e.max(out=m8, in_=msk), [r_msk], [r_m8])
                V("dve", lambda e: e.tensor_tensor(out=sm3[:, 6:7], in0=m8[:, 1:2], in1=m8[:, 0:1], op=ALU.subtract), [r_m8], [r_sm3])
                V("act", lambda e: e.activation(out=sm3[:, 6:7], in_=sm3[:, 6:7], func=AF.Exp), [r_sm3], [r_sm3])
                V("dve", lambda e: e.tensor_scalar(out=sm3[:, 6:7], in0=sm3[:, 6:7], scalar1=1.0, scalar2=None, op0=ALU.add), [r_sm3], [r_sm3])
                V("dve", lambda e: e.reciprocal(out=sm3[:, 7:8], in_=sm3[:, 6:7]), [r_sm3], [r_sm3])
                V("dve", lambda e: e.tensor_tensor(out=sm3[:, 8:9], in0=sm3[:, 7:8], in1=sm3[:, 5:6], op=ALU.mult), [r_sm3], [r_sm3])
                V("dve", lambda e: e.tensor_tensor(out=sm3[:, 9:10], in0=sm3[:, 5:6], in1=sm3[:, 8:9], op=ALU.subtract), [r_sm3], [r_sm3])
                V("dve", lambda e: e.tensor_scalar(out=oh[:, 0:32], in0=msk, scalar1=m8[:, 0:1], scalar2=sm3[:, 8:9], op0=ALU.is_equal, op1=ALU.mult), [r_msk, r_m8, r_sm3], [r_oh])
                V("dve", lambda e: e.tensor_scalar(out=oh[:, 32:64], in0=msk, scalar1=m8[:, 1:2], scalar2=sm3[:, 9:10], op0=ALU.is_equal, op1=ALU.mult), [r_msk, r_m8, r_sm3], [r_oh])
                V("dve", lambda e, c=c: e.tensor_tensor(out=Wtok[:, c, :], in0=oh[:, 0:32], in1=oh[:, 32:64], op=ALU.add), [r_oh], [r_Wtok[c]])
                if debug:
                    P.dma("sp", lambda e, c=c: e.dma_start(out=dbg["d_wtok"][c * 128:(c + 1) * 128, :], in_=Wtok[:, c, :]), r_dbg, [r_Wtok[c]], no_waw=True, semres=r_Wtok[c])
            P.barrier()
            if stop == 5: raise _Stop()
            A.release(m_moe)
            FG, r_FG = bload(final_g, D, "FG")

            yacc = A.alloc([TPC, D], F32); r_yacc = [Res("yacc%d" % i) for i in range(TPC)]
            for c in range(TPC):
                V("pool", lambda e, c=c: e.memset(yacc[:, c, :], 0.0), [], [r_yacc[c]])
            NWB = 2
            wgs = [A.alloc([8, 512], BF16) for _ in range(NWB)]
            wus = [A.alloc([8, 512], BF16) for _ in range(NWB)]
            wds = [A.alloc([4, D], BF16) for _ in range(NWB)]
            r_wgs = [Res("wgs%d" % i) for i in range(NWB)]; r_wus = [Res("wus%d" % i) for i in range(NWB)]; r_wds = [Res("wds%d" % i) for i in range(NWB)]
            sg = [A.alloc([512], F32) for _ in range(2)]; r_sg = [Res("sg0"), Res("sg1")]
            TG = min(4, TPC)
            NG = TPC // TG
            h1T = [A.alloc([4, TG * 128], BF16) for _ in range(2)]; r_h1T = [Res("h1T0"), Res("h1T1")]
            gi = 0
            for ex in range(32):
                wbf = ex % NWB
                for c0 in range(0, 512, 256):
                    P.dma("pool", lambda e, ex=ex, wbf=wbf, c0=c0: e.dma_start(out=wgs[wbf][:, :, c0:c0 + 256], in_=wg[ex].rearrange("(j p) n -> p j n", p=128)[:, :, c0:c0 + 256]), r_wgs[wbf], no_waw=(c0 > 0))
                for c0 in range(0, 512, 256):
                    P.dma("pool", lambda e, ex=ex, wbf=wbf, c0=c0: e.dma_start(out=wus[wbf][:, :, c0:c0 + 256], in_=wu[ex].rearrange("(j p) n -> p j n", p=128)[:, :, c0:c0 + 256]), r_wus[wbf], no_waw=(c0 > 0))
                for c0 in range(0, D, 256):
                    P.dma("pool", lambda e, ex=ex, wbf=wbf, c0=c0: e.dma_start(out=wds[wbf][:, :, c0:c0 + 256], in_=wd[ex].rearrange("(j p) n -> p j n", p=128)[:, :, c0:c0 + 256]), r_wds[wbf], no_waw=(c0 > 0))
                for tg in range(NG):
                    hb = gi % 2
                    gi += 1
                    N = TG * 128
                    tsl = slice(tg * N, (tg + 1) * N)
                    r_h2g = [r_h2T[tg * TG + i] for i in range(TG)]
                    for ec in range(4):
                        gb_ = (ec % 2) * 2
                        mm_group(banks[gb_][:, 0:N], rb[gb_], [(wgs[wbf][:, j, ec * 128:(ec + 1) * 128], h2T[:, j, tsl]) for j in range(8)], r_h2g + [r_wgs[wbf]])
                        mm_group(banks[gb_ + 1][:, 0:N], rb[gb_ + 1], [(wus[wbf][:, j, ec * 128:(ec + 1) * 128], h2T[:, j, tsl]) for j in range(8)], r_h2g + [r_wus[wbf]])
                        sb2 = ec % 2
                        V("act", lambda e, gb_=gb_, sb2=sb2, N=N: e.activation(out=sg[sb2][:, 0:N], in_=banks[gb_][:, 0:N], func=AF.Silu), [rb[gb_]], [r_sg[sb2]])
                        V("dve", lambda e, gb_=gb_, sb2=sb2, hb=hb, ec=ec, N=N: e.tensor_tensor(out=h1T[hb][:, ec, :], in0=banks[gb_ + 1][:, 0:N], in1=sg[sb2][:, 0:N], op=ALU.mult),
                          [rb[gb_ + 1], r_sg[sb2]], [r_h1T[hb]])
                    for i in range(TG):
                        c = tg * TG + i
                        for hf in range(2):
                            ob = 4 + (i * 2 + hf) % 4
                            mm_group(banks[ob][:], rb[ob], [(h1T[hb][:, ec, i * 128:(i + 1) * 128], wds[wbf][:, ec, hf * 512:(hf + 1) * 512]) for ec in range(4)], [r_h1T[hb], r_wds[wbf]])
                            V("dve", lambda e, ob=ob, c=c, hf=hf, ex=ex: e.scalar_tensor_tensor(out=yacc[:, c, hf * 512:(hf + 1) * 512], in0=banks[ob][:], scalar=Wtok[:, c, ex:ex + 1],
                                                                                       in1=yacc[:, c, hf * 512:(hf + 1) * 512], op0=ALU.mult, op1=ALU.add),
                              [rb[ob], r_Wtok[c], r_yacc[c]], [r_yacc[c]])
            xq = [A.alloc([D], F32) for _ in range(2)]; r_xq = [Res("xq0"), Res("xq1")]
            junk3 = A.alloc([D], BF16); r_junk3 = Res("junk3")
            sm4 = A.alloc([4], F32); r_sm4 = Res("sm4")
            for c in range(TPC):
                b = c % 2
                P.dma("sp", lambda e, c=c, b=b: e.dma_start(out=xq[b], in_=XN_d[c * 128:(c + 1) * 128, :]), r_xq[b], [r_XNd])
                V("dve", lambda e, c=c: e.tensor_tensor(out=yacc[:, c, :], in0=yacc[:, c, :], in1=GT2, op=ALU.mult), [r_yacc[c], r_GT2], [r_yacc[c]])
                V("dve", lambda e, c=c, b=b: e.tensor_tensor(out=xq[b], in0=xq[b], in1=yacc[:, c, :], op=ALU.add), [r_xq[b], r_yacc[c]], [r_xq[b]])
                V("act", lambda e, b=b: e.activation(out=junk3, in_=xq[b], func=AF.Square, accum_out=sm4[:, 0:1]), [r_xq[b]], [r_junk3, r_sm4])
                V("act", lambda e: e.activation(out=sm4[:, 1:2], in_=sm4[:, 0:1], func=AF.Ln, scale=1.0 / D, bias=EPS), [r_sm4], [r_sm4])
                V("act", lambda e: e.activation(out=sm4[:, 1:2], in_=sm4[:, 1:2], func=AF.Exp, scale=-0.5), [r_sm4], [r_sm4])
                V("dve", lambda e, b=b: e.scalar_tensor_tensor(out=xq[b], in0=xq[b], scalar=sm4[:, 1:2], in1=FG, op0=ALU.mult, op1=ALU.mult), [r_xq[b], r_sm4, r_FG], [r_xq[b]])
                P.dma("sp", lambda e, c=c, b=b: e.dma_start(out=out[c * 128:(c + 1) * 128, :], in_=xq[b]), r_out, [r_xq[b]], no_waw=True, semres=r_xq[b])
        try:
            body()
        except _Stop:
            P.barrier()
        fin = [r_out] + ([r_dbg] if debug else [])
        P.wait_all("sp", fin)
        P.wait_all("pool", fin)
        P.emit(st)
    return nc


def _rope_tables(L):
    GRID_W = 64
    rows = L // GRID_W
    row = np.repeat(np.arange(rows), GRID_W).astype(np.float32)
    col = np.tile(np.arange(GRID_W), rows).astype(np.float32)
    freqs = (np.float32(10000.0) ** (-np.arange(0, 32, 2, dtype=np.float32) / np.float32(32))).astype(np.float32)
    ang = np.concatenate([row[:, None] * freqs, col[:, None] * freqs], axis=-1).astype(np.float32)
    return np.cos(ang).astype(np.float32), np.sin(ang).astype(np.float32)


def make_in_maps(inputs, NT):
    TPC = NT // NCORES
    NS = NT + 2
    L = NT * 128
    f = lambda a: np.ascontiguousarray(np.asarray(a, dtype=np.float32))
    x = f(inputs["x"]).reshape(L, D)
    ctx = f(inputs["ctx"]).reshape(256, D)
    cos, sin = _rope_tables(L)
    cvecT = np.zeros((128, 16), np.float32)
    cvecT[:, 0:8] = f(inputs["c"]).reshape(8, 128).T
    cvecT[:, 8:16] = f(inputs["c_ctx"]).reshape(8, 128).T
    p = np.arange(128, dtype=np.float32)
    dpos = np.maximum(p[None, :] - p[:, None], 0.0)
    dneg = np.maximum(p[:, None] - p[None, :], 0.0)
    dmat = np.concatenate([dpos, dneg], axis=1).astype(np.float32)
    pcol = np.stack([p + 1.0, 128.0 - p], axis=1).astype(np.float32)
    shared = {
        "cvecT": cvecT,
        "w_ada": f(inputs["w_ada"]).reshape(D, 6 * D), "b_ada": f(inputs["b_ada"]).reshape(1, 6 * D),
        "norm1_g": f(inputs["norm1_g"]).reshape(1, D), "norm2_g": f(inputs["norm2_g"]).reshape(1, D),
        "final_g": f(inputs["final_norm_g"]).reshape(1, D),
        "w_in": f(inputs["w_in"]).reshape(D, 2304),
        "qn": f(inputs["attn_q_norm"]).reshape(1, 64), "kn": f(inputs["attn_k_norm"]).reshape(1, 64),
        "dec": np.concatenate([f(inputs["ret_decay_fwd"]).reshape(1, 4), f(inputs["ret_decay_bwd"]).reshape(1, 4)], axis=1),
        "gn_g": f(inputs["ret_gn_g"]).reshape(1, 512), "gn_b": f(inputs["ret_gn_b"]).reshape(1, 512),
        "w_out": f(inputs["w_out"]).reshape(D, D),
        "w_rt": np.ascontiguousarray(np.concatenate([f(inputs["moe_w_grp"]).reshape(D, 4), f(inputs["moe_w_exp"]).reshape(D, 32)], axis=1)),
        "b_rt": np.concatenate([f(inputs["moe_b_grp"]).reshape(1, 4), f(inputs["moe_b_exp"]).reshape(1, 32)], axis=1),
        "wg": f(inputs["moe_w_gate"]).reshape(32, D, 512), "wu": f(inputs["moe_w_up"]).reshape(32, D, 512),
        "wd": f(inputs["moe_w_down"]).reshape(32, 512, D),
        "dmat_t": dmat, "pcol_t": pcol,
    }
    maps = []
    for r in range(NCORES):
        own = list(range(r * TPC, (r + 1) * TPC))
        others = [t for t in range(NT) if t not in own]
        order = own + others
        rows = np.concatenate([np.arange(t * 128, (t + 1) * 128) for t in order])
        xpm = np.concatenate([x[rows], ctx], axis=0)
        cosp = np.concatenate([cos[rows], np.ones((256, 32), np.float32)], axis=0)
        sinp = np.concatenate([sin[rows], np.zeros((256, 32), np.float32)], axis=0)
        s0 = r * TPC * 128
        e0 = s0 + TPC * 128
        distf = np.zeros((128, NS), np.float32); distb = np.zeros((128, NS), np.float32)
        maskf = np.zeros((128, NS), np.float32); maskb = np.zeros((128, NS), np.float32)
        for s, t in enumerate(order):
            m = t * 128 + p
            if s < TPC:
                distf[:, s] = 127.0 - p; distb[:, s] = p; maskf[:, s] = 1.0; maskb[:, s] = 1.0
            elif t * 128 < s0:
                distf[:, s] = s0 - 1 - m; maskf[:, s] = 1.0
            else:
                distb[:, s] = m - e0; maskb[:, s] = 1.0
        for j in range(2):
            mc = j * 128 + p
            distf[:, NT + j] = s0 + 255 - mc; maskf[:, NT + j] = 1.0
            distb[:, NT + j] = L + mc - e0; maskb[:, NT + j] = 1.0
        d = dict(shared)
        d["xp"] = np.ascontiguousarray(xpm)
        d["cos_t"] = np.ascontiguousarray(cosp)
        d["sin_t"] = np.ascontiguousarray(sinp)
        d["dist_t"] = np.ascontiguousarray(np.concatenate([distf, distb, maskf, maskb], axis=1))
        maps.append(d)
    return maps


_NC_CACHE = {}


def run(inputs, NT, debug=False, stop=99, cores=None):
    key = (NT, debug, stop)
    if key not in _NC_CACHE:
        _NC_CACHE[key] = build_program(NT, debug, stop)
    nc = _NC_CACHE[key]
    maps = make_in_maps(inputs, NT)
    if stop < 6:
        for m in maps:
            for k in ("wg", "wu", "wd"):
                m[k] = m[k][0:1]
    if cores is not None:
        maps = [maps[c] for c in cores]
        return run_bass_kernel_spmd(nc, maps, core_ids=list(range(len(cores))))
    res = run_bass_kernel_spmd(nc, maps, core_ids=list(range(NCORES)))
    return res


def kernel(**inputs):
    NT = 128
    res = run(inputs, NT)
    outp = np.concatenate([np.asarray(res.results[r]["out"]) for r in range(NCORES)], axis=0)
    return outp.reshape(1, NT * 128, D).astype(np.float32)
```

```python
import numpy as np
from contextlib import ExitStack
import concourse.bass as bass
import concourse.mybir as mybir
from concourse.bass_utils import run_bass_kernel_spmd

F32 = mybir.dt.float32
BF16 = mybir.dt.bfloat16
I32 = mybir.dt.int32
U32 = mybir.dt.uint32
AF = mybir.ActivationFunctionType
ALU = mybir.AluOpType
AX = mybir.AxisListType

ENGS = ("pe", "act", "dve", "pool", "sp")
NCORES = 8
D = 1024
EPS = 1e-6


import types


def _freeze(fn):
    if fn is None or fn.__closure__ is None:
        return fn
    cells = []
    for c in fn.__closure__:
        try:
            cells.append(types.CellType(c.cell_contents))
        except ValueError:
            cells.append(c)
    return types.FunctionType(fn.__code__, fn.__globals__, fn.__name__, fn.__defaults__, tuple(cells))


class Res:
    __slots__ = ("name", "w", "rs", "dsem", "dcount", "excl")

    def __init__(self, name, excl=False):
        self.name = name
        self.excl = excl
        self.w = {}
        self.rs = {}
        self.dsem = None
        self.dcount = 0


class Prog:
    def __init__(self, nc):
        self.nc = nc
        self.ops = {e: [] for e in ENGS}
        self.cnt = {e: 0 for e in ENGS}
        self.known = {e: {} for e in ENGS}
        self.semnames = []
        self.dres = []

    def _deps(self, eng, reads, writes):
        best = {}

        def add(d):
            for s, v in d.items():
                if v > best.get(s, 0):
                    best[s] = v
        for r in reads:
            add(r.w)
        for w in writes:
            add(w.w)
            add(w.rs)
        waits = []
        kn = self.known[eng]
        for s, v in best.items():
            if kn.get(s, 0) < v:
                kn[s] = v
                waits.append((s, v))
        return waits

    def op(self, eng, fn, reads=(), writes=(), inc=True, pe_acc=False):
        fn = _freeze(fn)
        xr = [r for r in reads if r.excl]
        if xr:
            writes = list(writes) + xr
            reads = [r for r in reads if not r.excl]
        waits = self._deps(eng, reads, writes)
        if pe_acc:
            waits = [(s, v) for (s, v) in waits if s != "E_pe"]
        sem = "E_" + eng
        val = self.cnt[eng] + 1
        if inc:
            self.cnt[eng] = val
        self.ops[eng].append(("op", fn, waits, sem if inc else None))
        for r in reads:
            if r.rs.get(sem, 0) < val:
                r.rs[sem] = val
        for w in writes:
            w.w = {sem: val}
            w.rs = {}
        return (sem, val)

    def dma(self, eng, fn, dst, reads=(), no_waw=False, semres=None):
        fn = _freeze(fn)
        owner = semres if semres is not None else dst
        wl = [] if no_waw else [dst]
        if semres is not None:
            wl.append(semres)
        rl = list(reads)
        waits = self._deps(eng, rl, wl)
        if owner.dsem is None:
            owner.dsem = "D%d_%s" % (len(self.semnames), owner.name)
            self.semnames.append(owner.dsem)
            self.dres.append(owner)
        owner.dcount += 16
        sem, val = owner.dsem, owner.dcount
        self.ops[eng].append(("dma", fn, waits, sem))
        for r in rl:
            if r.rs.get(sem, 0) < val:
                r.rs[sem] = val
        if semres is not None:
            semres.rs[sem] = val
        if no_waw:
            dst.w[sem] = val
        else:
            dst.w = {sem: val}
            dst.rs = {}
        return (sem, val)

    def wait_all(self, eng, ress):
        waits = self._deps(eng, ress, [])
        self.ops[eng].append(("wait", None, waits, None))

    def barrier(self):
        evs = [("E_" + e, self.cnt[e]) for e in ENGS if self.cnt[e] > 0]
        evs += [(r.dsem, r.dcount) for r in self.dres]
        for e in ENGS:
            kn = self.known[e]
            waits = []
            for (s, v) in evs:
                if kn.get(s, 0) < v:
                    kn[s] = v
                    waits.append((s, v))
            if waits:
                self.ops[e].append(("wait", None, waits, None))

    def emit(self, stack):
        nc = self.nc
        sems = {}
        for e in ENGS:
            sems["E_" + e] = stack.enter_context(nc.semaphore("E_" + e))
        for n in self.semnames:
            sems[n] = stack.enter_context(nc.semaphore(n))
        block = stack.enter_context(nc.Block())
        engmap = {"pe": block.tensor, "act": block.scalar, "dve": block.vector,
                  "pool": block.gpsimd, "sp": block.sync}
        for e in ENGS:
            ops = self.ops[e]
            if not ops:
                continue

            def body(engine, ops=ops):
                for kind, fn, waits, sem in ops:
                    for (s, v) in waits:
                        engine.wait_ge(sems[s], v)
                    if kind == "op":
                        ins = fn(engine)
                        if sem is not None:
                            ins.then_inc(sems[sem], 1)
                    elif kind == "dma":
                        ins = fn(engine)
                        ins.then_inc(sems[sem], 16)
            engmap[e](body)


class Arena:
    def __init__(self, t, nwords, start=0):
        self.t = t
        self.n = nwords
        self.top = start

    def alloc(self, shape, dt):
        esz = 2 if dt == BF16 else 4
        nel = int(np.prod(shape))
        words = (nel * esz + 3) // 4
        words = (words + 7) // 8 * 8
        assert self.top + words <= self.n, ("arena overflow", self.top, words, self.n)
        ap = self.t[:, self.top:self.top + words]
        self.top += words
        if dt != F32:
            ap = ap.bitcast(dt)
        ap = ap[:, 0:nel]
        if len(shape) == 2:
            ap = ap.rearrange("p (a b) -> p a b", a=shape[0])
        elif len(shape) == 3:
            ap = ap.rearrange("p (a b c) -> p a b c", a=shape[0], b=shape[1])
        return ap

    def mark(self):
        return self.top

    def release(self, m):
        self.top = m


class _Stop(Exception):
    pass


def build_program(NT, debug=False, stop=99):
    import os
    SUB = int(os.environ.get('KSUB', '0'))
    TPC = NT // NCORES
    NS = NT + 2
    nc = bass.Bass("TRN2", target_bir_lowering=False)

    def din(name, shape, dt=F32):
        return nc.dram_tensor(name, shape, dt, kind="ExternalInput").ap()

    xp = din("xp", [NS * 128, D])
    cvecT = din("cvecT", [128, 16])
    w_ada = din("w_ada", [D, 6 * D])
    b_ada = din("b_ada", [1, 6 * D])
    norm1_g = din("norm1_g", [1, D])
    norm2_g = din("norm2_g", [1, D])
    final_g = din("final_g", [1, D])
    w_in = din("w_in", [D, 2304])
    qn = din("qn", [1, 64])
    kn = din("kn", [1, 64])
    dec = din("dec", [1, 8])
    gn_g = din("gn_g", [1, 512])
    gn_b = din("gn_b", [1, 512])
    w_out = din("w_out", [D, D])
    w_rt = din("w_rt", [D, 36])
    b_rt = din("b_rt", [1, 36])
    NEXP = 32 if stop >= 6 else 1
    wg = din("wg", [NEXP, D, 512])
    wu = din("wu", [NEXP, D, 512])
    wd = din("wd", [NEXP, 512, D])
    cos_t = din("cos_t", [NS * 128, 32])
    sin_t = din("sin_t", [NS * 128, 32])
    dist_t = din("dist_t", [128, 4 * NS])
    dmat_t = din("dmat_t", [128, 256])
    pcol_t = din("pcol_t", [128, 2])
    out = nc.dram_tensor("out", [TPC * 128, D], F32, kind="ExternalOutput").ap()
    dbg = {}
    if debug:
        for nm, shp in [("d_hx", [128, D]), ("d_attn", [TPC * 128, 512]), ("d_ret", [TPC * 128, 512]),
                        ("d_xnew", [TPC * 128, D]), ("d_wtok", [TPC * 128, 32]), ("d_g1", [128, D]),
                        ("d_kt", [128, NS * 128]), ("d_v1", [128, NS * 130]), ("d_qt", [128, TPC * 512])]:
            dbg[nm] = nc.dram_tensor(nm, shp, F32, kind="ExternalOutput").ap()

    KT_d = nc.dram_tensor("KT_d", [128, NS * 128], BF16).ap()
    V_d = nc.dram_tensor("V_d", [NS, 128, 130], BF16).ap()
    XN_d = nc.dram_tensor("XN_d", [TPC * 128, D], F32).ap()
    r_KTd = Res("KTd"); r_Vd = Res("Vd"); r_XNd = Res("XNd"); r_out = Res("out"); r_dbg = Res("dbg")

    P = Prog(nc)
    st = ExitStack()
    with st:
        ARW = 51 * 1024
        arena_t = st.enter_context(nc.sbuf_tensor("arena", [128, ARW], F32))
        A = Arena(arena_t, ARW)
        pbig = st.enter_context(nc.psum_tensor("pbig", [128, 4096], F32))
        banks = [pbig[:, i * 512:(i + 1) * 512] for i in range(8)]
        rb = [Res("bank%d" % i, excl=True) for i in range(8)]

        def bank_bf(i):
            return banks[i].bitcast(BF16)

        def V(eng, fn, reads, writes, **kw):
            return P.op(eng, fn, reads, writes, **kw)

        def mm_group(ps_ap, ps_res, pairs, reads):
            n = len(pairs)
            for i, (l, r) in enumerate(pairs):
                P.op("pe", lambda e, l=l, r=r, i=i: e.matmul(ps_ap, l, r, start=(i == 0), stop=(i == n - 1)),
                     reads, [ps_res], inc=(i == n - 1), pe_acc=(i > 0))

        def body():
            nonlocal A
            ident_f = A.alloc([128], F32); r_idf = Res("idf")
            ident_b = A.alloc([128], BF16); r_idb = Res("idb")
            V("pool", lambda e: e.memset(ident_f, 0.0), [], [r_idf])
            V("pool", lambda e: e.affine_select(out=ident_f, in_=ident_f, pattern=[[-1, 128]], compare_op=ALU.not_equal,
                                                fill=1.0, base=0, channel_multiplier=1), [r_idf], [r_idf])
            V("dve", lambda e: e.tensor_copy(out=ident_b, in_=ident_f), [r_idf], [r_idb])

            def bload(dram_row, n, name):
                t = A.alloc([n], F32); r = Res(name)
                P.dma("sp", lambda e: e.dma_start(out=t, in_=dram_row.partition_broadcast(128)), r)
                return t, r

            GT1 = A.alloc([D], F32); r_GT1 = Res("GT1")
            G2 = A.alloc([D], F32); r_G2 = Res("G2")
            SH2 = A.alloc([D], F32); r_SH2 = Res("SH2")
            GT2 = A.alloc([D], F32); r_GT2 = Res("GT2")
            dec_bc, r_dec = bload(dec, 8, "dec")
            qn_bc, r_qn = bload(qn, 64, "qn")
            kn_bc, r_kn = bload(kn, 64, "kn")
            lg = A.alloc([8], F32); r_lg = Res("lg")
            V("act", lambda e: e.activation(out=lg, in_=dec_bc, func=AF.Exp, scale=-1.0), [r_dec], [r_lg])
            V("act", lambda e: e.activation(out=lg, in_=lg, func=AF.Ln, bias=1.0, scale=1.0), [r_lg], [r_lg])
            V("dve", lambda e: e.tensor_scalar(out=lg, in0=lg, scalar1=-1.0, scalar2=None, op0=ALU.mult), [r_lg], [r_lg])
            negC = A.alloc([1], F32); r_negC = Res("negC")
            mk_ = A.alloc([1], F32); r_mk = Res("mk")
            V("dve", lambda e: e.tensor_reduce(out=negC, in_=qn_bc, axis=AX.X, op=ALU.max, apply_absolute_value=True), [r_qn], [r_negC])
            V("dve", lambda e: e.tensor_reduce(out=mk_, in_=kn_bc, axis=AX.X, op=ALU.max, apply_absolute_value=True), [r_kn], [r_mk])
            V("dve", lambda e: e.tensor_tensor(out=negC, in0=negC, in1=mk_, op=ALU.mult), [r_negC, r_mk], [r_negC])
            V("dve", lambda e: e.tensor_scalar(out=negC, in0=negC, scalar1=-8.0, scalar2=None, op0=ALU.mult), [r_negC], [r_negC])
            Wtok = A.alloc([TPC, 32], F32); r_Wtok = [Res("Wtok%d" % i) for i in range(TPC)]
            m_scrA0 = A.mark()
            actT = A.alloc([TPC * 1024], BF16)
            hxT_own = actT.rearrange("p (c x) -> p c x", c=TPC); r_hxT_own = [Res("hxTo%d" % i) for i in range(TPC)]
            h2T = actT.rearrange("p (j t) -> p j t", j=8); r_h2T = [Res("h2T%d" % i) for i in range(TPC)]
            m_moe = A.mark()
            retx = A.alloc([TPC, 512], BF16); r_retx = [Res("retx%d" % i) for i in range(TPC)]
            QT = A.alloc([TPC, 512], BF16); r_QT = Res("QT")
            m_persist = A.mark()
            m_scrA1 = A.mark()
            w_in_sb = A.alloc([8, 2304], BF16); r_win = Res("win")
            w_in_v = w_in.rearrange("(j p) n -> p j n", p=128)
            for c0 in range(0, 2304, 256):
                P.dma("pool", lambda e, c0=c0: e.dma_start(out=w_in_sb[:, :, c0:c0 + 256], in_=w_in_v[:, :, c0:c0 + 256]), r_win, no_waw=True)
            Ust = A.alloc([TPC, 512], BF16); r_Ust = [Res("Ust%d" % i) for i in range(TPC)]
            Sacc = A.alloc([512], F32); r_Sacc = Res("Sacc")
            V("pool", lambda e: e.memset(Sacc, 0.0), [], [r_Sacc])
            ri = A.alloc([14, 64], F32); r_ri = Res("ri")
            ro = A.alloc([14, 64], F32); r_ro = Res("ro")
            rt = [A.alloc([14, 32], F32) for _ in range(2)]; r_rt = [Res("rt%d" % i) for i in range(2)]
            m_sw = A.mark()

            G1 = A.alloc([D], F32); r_G1 = Res("G1")
            SH1 = A.alloc([D], F32); r_SH1 = Res("SH1")
            G1c = A.alloc([D], F32); r_G1c = Res("G1c")
            SH1c = A.alloc([D], F32); r_SH1c = Res("SH1c")
            A_main = A
            if m_scrA1 - m_scrA0 >= 15000:
                A = Arena(arena_t, m_scrA1, start=m_scrA0)
            else:
                A = Arena(arena_t, ARW, start=A_main.top)
            cv = A.alloc([16], F32); r_cv = Res("cv")
            P.dma("sp", lambda e: e.dma_start(out=cv, in_=cvecT[:, :]), r_cv)
            V("act", lambda e: e.activation(out=cv, in_=cv, func=AF.Silu), [r_cv], [r_cv])
            rep = A.alloc([16, 128], F32); r_rep = Res("rep")
            V("dve", lambda e: e.tensor_copy(out=rep, in_=cv.unsqueeze(2).to_broadcast([128, 16, 128])), [r_cv], [r_rep])
            bada2 = [A.alloc([512], F32) for _ in range(2)]; r_bada2 = [Res("bada0"), Res("bada1")]
            n1g, r_n1g = bload(norm1_g, D, "n1g")
            n2g, r_n2g = bload(norm2_g, D, "n2g")
            wst = [A.alloc([8, 512], F32) for _ in range(2)]; r_wst = [Res("wst0"), Res("wst1")]
            tmpA = A.alloc([512], F32); r_tmpA = Res("tmpA")
            w_ada_v = w_ada.rearrange("(j p) n -> p j n", p=128)
            plan = {0: ("sh", SH1, r_SH1, SH1c, r_SH1c, None), 1: ("sh", SH1, r_SH1, SH1c, r_SH1c, None),
                    2: ("sc", G1, r_G1, G1c, r_G1c, (n1g, r_n1g)), 3: ("sc", G1, r_G1, G1c, r_G1c, (n1g, r_n1g)),
                    4: ("sh", GT1, r_GT1, None, None, None), 5: ("sh", GT1, r_GT1, None, None, None),
                    6: ("sh", SH2, r_SH2, None, None, None), 7: ("sh", SH2, r_SH2, None, None, None),
                    8: ("sc", G2, r_G2, None, None, (n2g, r_n2g)), 9: ("sc", G2, r_G2, None, None, (n2g, r_n2g)),
                    10: ("sh", GT2, r_GT2, None, None, None), 11: ("sh", GT2, r_GT2, None, None, None)}
            for cb in range(12):
                kind, dst, r_dst, dstc, r_dstc, gg = plan[cb]
                wb = wst[cb % 2]; r_wb = r_wst[cb % 2]
                P.dma("sp" if cb % 2 == 0 else "act", lambda e, wb=wb, cb=cb: e.dma_start(out=wb, in_=w_ada_v[:, :, cb * 512:(cb + 1) * 512]), r_wb)
                bada = bada2[cb % 2]; r_bada = r_bada2[cb % 2]
                P.dma("sp", lambda e, bada=bada, cb=cb: e.dma_start(out=bada, in_=b_ada[:, cb * 512:(cb + 1) * 512].partition_broadcast(128)), r_bada)
                half = (cb % 2) * 512
                for which in range(2):
                    if which == 1 and dstc is None:
                        continue
                    bk = (cb * 2 + which) % 4
                    mm_group(banks[bk], rb[bk], [(rep[:, which * 8 + j, :], wb[:, j, :]) for j in range(8)], [r_rep, r_wb])
                    d_ap = (dst if which == 0 else dstc)[:, half:half + 512]
                    r_d = r_dst if which == 0 else r_dstc
                    bslice = bada
                    if kind == "sh":
                        V("dve", lambda e, bk=bk, d_ap=d_ap, bslice=bslice: e.tensor_tensor(out=d_ap, in0=banks[bk], in1=bslice, op=ALU.add),
                          [rb[bk], r_bada], [r_d])
                    else:
                        g_ap = gg[0][:, half:half + 512]
                        V("dve", lambda e, bk=bk, bslice=bslice: e.tensor_tensor(out=tmpA, in0=banks[bk], in1=bslice, op=ALU.add),
                          [rb[bk], r_bada], [r_tmpA])
                        V("dve", lambda e, d_ap=d_ap, g_ap=g_ap: e.scalar_tensor_tensor(out=d_ap, in0=tmpA, scalar=1.0, in1=g_ap, op0=ALU.add, op1=ALU.mult),
                          [r_tmpA, gg[1]], [r_d])
            if debug:
                P.dma("sp", lambda e: e.dma_start(out=dbg["d_g1"], in_=G1), r_dbg, [r_G1], no_waw=True, semres=r_G1)
            P.barrier()
            if stop == 1: raise _Stop()
            A = A_main

            distt = A.alloc([4 * NS], F32); r_distt = Res("distt")
            P.dma("sp", lambda e: e.dma_start(out=distt, in_=dist_t[:, :]), r_distt)
            Wf = A.alloc([NS, 4], F32); r_Wf = Res("Wf")
            Wb = A.alloc([NS, 4], F32); r_Wb = Res("Wb")
            for h in range(4):
                V("act", lambda e, h=h: e.activation(out=Wf[:, :, h], in_=distt[:, 0:NS], func=AF.Exp, scale=lg[:, h:h + 1]), [r_distt, r_lg], [r_Wf])
                V("act", lambda e, h=h: e.activation(out=Wb[:, :, h], in_=distt[:, NS:2 * NS], func=AF.Exp, scale=lg[:, 4 + h:5 + h]), [r_distt, r_lg], [r_Wb])
            V("dve", lambda e: e.tensor_tensor(out=Wf, in0=Wf, in1=distt[:, 2 * NS:3 * NS].unsqueeze(2).to_broadcast([128, NS, 4]), op=ALU.mult), [r_Wf, r_distt], [r_Wf])
            V("dve", lambda e: e.tensor_tensor(out=Wb, in0=Wb, in1=distt[:, 3 * NS:4 * NS].unsqueeze(2).to_broadcast([128, NS, 4]), op=ALU.mult), [r_Wb, r_distt], [r_Wb])

            NB = 2
            xt = [A.alloc([D], F32) for _ in range(NB)]; r_xt = [Res("xt%d" % i) for i in range(NB)]
            cst = [A.alloc([64], F32) for _ in range(NB)]; r_cst = [Res("cst%d" % i) for i in range(NB)]
            tmpx = A.alloc([D], F32); r_tmpx = Res("tmpx")
            junk = tmpx; r_junk = r_tmpx
            hx1 = A.alloc([D], BF16); hx = [hx1, hx1]; r_hx1 = Res("hx"); r_hx = [r_hx1, r_hx1]
            hxTw1 = A.alloc([8 * 128], BF16); hxTw = [hxTw1, hxTw1]; r_hxTw1 = Res("hxTw"); r_hxTw = [r_hxTw1, r_hxTw1]
            sm = [A.alloc([32], F32) for _ in range(NB)]; r_sm = [Res("sm%d" % i) for i in range(NB)]
            sqh = A.alloc([640], F32); r_sqh = Res("sqh")
            kb = A.alloc([128], BF16); r_kb = Res("kb")
            qb = A.alloc([8, 64], BF16); r_qb = Res("qb")
            ktb = [A.alloc([128], BF16) for _ in range(NB)]; r_ktb = [Res("ktb%d" % i) for i in range(NB)]
            v1 = [A.alloc([2, 65], BF16) for _ in range(NB)]; r_v1 = [Res("v1%d" % i) for i in range(NB)]
            for i in range(NB):
                V("pool", lambda e, i=i: e.memset(v1[i], 1.0), [], [r_v1[i]])
            kaug = A.alloc([4, 128], BF16); r_kaug = Res("kaug")
            vb = A.alloc([512], BF16); r_vb = Res("vb")

            def rope(H0, H1, cs, r_cs):
                H = H1 - H0
                riv = ri[:, H0:H1, :].rearrange("p h (i t) -> p h i t", t=2)
                rov = ro[:, H0:H1, :].rearrange("p h (i t) -> p h i t", t=2)
                cb_ = cs[:, 0:32].unsqueeze(1).to_broadcast([128, H, 32])
                sb_ = cs[:, 32:64].unsqueeze(1).to_broadcast([128, H, 32])
                t = [rt[i][:, 0:H, :] for i in range(2)]
                V("dve", lambda e: e.tensor_tensor(out=t[0], in0=riv[:, :, :, 0], in1=cb_, op=ALU.mult), [r_ri, r_cs], [r_rt[0]])
                V("dve", lambda e: e.tensor_tensor(out=t[1], in0=riv[:, :, :, 1], in1=sb_, op=ALU.mult), [r_ri, r_cs], [r_rt[1]])
                V("dve", lambda e: e.tensor_tensor(out=rov[:, :, :, 0], in0=t[0], in1=t[1], op=ALU.subtract), [r_rt[0], r_rt[1]], [r_ro])
                V("dve", lambda e: e.tensor_tensor(out=t[0], in0=riv[:, :, :, 0], in1=sb_, op=ALU.mult), [r_ri, r_cs], [r_rt[0]])
                V("dve", lambda e: e.tensor_tensor(out=t[1], in0=riv[:, :, :, 1], in1=cb_, op=ALU.mult), [r_ri, r_cs], [r_rt[1]])
                V("dve", lambda e: e.tensor_tensor(out=rov[:, :, :, 1], in0=t[0], in1=t[1], op=ALU.add), [r_rt[0], r_rt[1]], [r_ro])

            def rsqrt_mean(dst, src, r_dst, r_src, n):
                V("act", lambda e: e.activation(out=dst, in_=src, func=AF.Ln, scale=1.0 / n, bias=EPS), [r_src], [r_dst])
                V("act", lambda e: e.activation(out=dst, in_=dst, func=AF.Exp, scale=-0.5), [r_dst], [r_dst])

            def norm_mod_transpose(b, x_ap, r_x, Gm, r_Gm, SHm, r_SHm, dstT, r_dstT, tb):
                s_ = sm[b]; r_s = r_sm[b]
                V("act", lambda e: e.activation(out=junk, in_=x_ap, func=AF.Square, accum_out=s_[:, 0:1]), [r_x], [r_junk, r_s])
                rsqrt_mean(s_[:, 1:2], s_[:, 0:1], r_s, r_s, D)
                V("dve", lambda e: e.scalar_tensor_tensor(out=tmpx, in0=x_ap, scalar=s_[:, 1:2], in1=Gm, op0=ALU.mult, op1=ALU.mult),
                  [r_x, r_s, r_Gm], [r_tmpx])
                V("pool", lambda e: e.tensor_tensor(out=hx[b], in0=tmpx, in1=SHm, op=ALU.add), [r_tmpx, r_SHm], [r_hx[b]])
                pt = bank_bf(tb)
                for j in range(8):
                    V("pe", lambda e, j=j: e.transpose(out=pt[:, j * 128:(j + 1) * 128], in_=hx[b][:, j * 128:(j + 1) * 128], identity=ident_b),
                      [r_hx[b], r_idb], [rb[tb]], inc=(j == 7), pe_acc=(j > 0))
                V("act", lambda e: e.activation(out=dstT, in_=pt, func=AF.Copy), [rb[tb]], [r_dstT])

            xpv = xp.rearrange("(s p) n -> s p n", p=128)
            cosv = cos_t.rearrange("(s p) n -> s p n", p=128)
            sinv = sin_t.rearrange("(s p) n -> s p n", p=128)

            for s in range(NS):
                b = s % NB
                own = s < TPC
                isctx = s >= NT
                P.dma("sp", lambda e, s=s, b=b: e.dma_start(out=xt[b], in_=xpv[s]), r_xt[b])
                P.dma("sp", lambda e, s=s, b=b: e.dma_start(out=cst[b][:, 0:32], in_=cosv[s]), r_cst[b])
                P.dma("sp", lambda e, s=s, b=b: e.dma_start(out=cst[b][:, 32:64], in_=sinv[s]), r_cst[b])
                if own:
                    dstT = hxT_own[:, s, :]; r_dT = r_hxT_own[s]
                else:
                    dstT = hxTw[b]; r_dT = r_hxTw[b]
                norm_mod_transpose(b, xt[b], r_xt[b], G1c if isctx else G1, r_G1c if isctx else r_G1,
                                   SH1c if isctx else SH1, r_SH1c if isctx else r_SH1, dstT, r_dT, tb=b)
                if debug and s == 0:
                    V("dve", lambda e: e.tensor_copy(out=tmpx, in_=hx[0]), [r_hx[0]], [r_tmpx])
                    P.dma("sp", lambda e: e.dma_start(out=dbg["d_hx"], in_=tmpx), r_dbg, [r_tmpx], no_waw=True, semres=r_tmpx)
                hT = dstT.rearrange("p (j t) -> p j t", j=8)
                mm_group(banks[2][:, 0:256], rb[2], [(hT[:, j, :], w_in_sb[:, j, 512:768]) for j in range(8)], [r_dT, r_win])
                mm_group(banks[2][:, 256:512], rb[2], [(hT[:, j, :], w_in_sb[:, j, 1536:1792]) for j in range(8)], [r_dT, r_win])
                mm_group(banks[3], rb[3], [(hT[:, j, :], w_in_sb[:, j, 1024:1536]) for j in range(8)], [r_dT, r_win])
                if own:
                    mm_group(banks[4], rb[4], [(hT[:, j, :], w_in_sb[:, j, 0:512]) for j in range(8)], [r_dT, r_win])
                s_ = sm[b]; r_s = r_sm[b]
                H0 = 0 if own else 8
                V("act", lambda e: e.activation(out=sqh[:, 512:640], in_=banks[2][:, 0:128], func=AF.Square), [rb[2]], [r_sqh])
                if own:
                    V("act", lambda e: e.activation(out=sqh[:, 0:512], in_=banks[4], func=AF.Square), [rb[4]], [r_sqh])
                nh = 10 - H0
                hs = s_[:, 8 + H0:18]
                V("dve", lambda e: e.tensor_reduce(out=hs, in_=sqh[:, H0 * 64:640].rearrange("p (h d) -> p h d", d=64), axis=AX.X, op=ALU.add),
                  [r_sqh], [r_s])
                rsqrt_mean(hs, hs, r_s, r_s, 64)
                V("dve", lambda e: e.tensor_tensor(out=ri[:, 8:10, :], in0=banks[2][:, 0:128].rearrange("p (h d) -> p h d", d=64),
                                                   in1=s_[:, 16:18].unsqueeze(2).to_broadcast([128, 2, 64]), op=ALU.mult), [rb[2], r_s], [r_ri])
                V("dve", lambda e: e.tensor_tensor(out=ri[:, 8:10, :], in0=ri[:, 8:10, :], in1=kn_bc.unsqueeze(1).to_broadcast([128, 2, 64]), op=ALU.mult),
                  [r_ri, r_kn], [r_ri])
                if own:
                    V("dve", lambda e: e.tensor_tensor(out=ri[:, 0:8, :], in0=banks[4].rearrange("p (h d) -> p h d", d=64),
                                                       in1=s_[:, 8:16].unsqueeze(2).to_broadcast([128, 8, 64]), op=ALU.mult), [rb[4], r_s], [r_ri])
                    V("dve", lambda e: e.tensor_tensor(out=ri[:, 0:8, :], in0=ri[:, 0:8, :], in1=qn_bc.unsqueeze(1).to_broadcast([128, 8, 64]), op=ALU.mult),
                      [r_ri, r_qn], [r_ri])
                V("act", lambda e: e.activation(out=ri[:, 10:14, :], in_=banks[3][:, 0:256].rearrange("p (h d) -> p h d", d=64), func=AF.Copy, scale=0.125),
                  [rb[3]], [r_ri])
                rope(H0, 14, cst[b], r_cst[b])
                V("dve", lambda e: e.tensor_copy(out=kb, in_=ro[:, 8:10, :]), [r_ro], [r_kb])
                ptk = bank_bf(5)
                V("pe", lambda e: e.transpose(out=ptk[:, 0:128], in_=kb, identity=ident_b), [r_kb, r_idb], [rb[5]])
                V("act", lambda e, b=b: e.activation(out=ktb[b], in_=ptk[:, 0:128], func=AF.Copy), [rb[5]], [r_ktb[b]])
                P.dma("pool", lambda e, s=s, b=b: e.dma_start(out=KT_d[:, s * 128:(s + 1) * 128], in_=ktb[b]), r_KTd, [r_ktb[b]], no_waw=True, semres=r_ktb[b])
                V("act", lambda e, b=b: e.activation(out=v1[b][:, :, 0:64], in_=banks[2][:, 128:256].rearrange("p (h d) -> p h d", d=64), func=AF.Copy),
                  [rb[2]], [r_v1[b]])
                P.dma("pool", lambda e, s=s, b=b: e.dma_start(out=V_d[s], in_=v1[b]), r_Vd, [r_v1[b]], no_waw=True, semres=r_v1[b])
                if own:
                    ptq = bank_bf(6)
                    V("dve", lambda e: e.tensor_copy(out=qb, in_=ro[:, 0:8, :]), [r_ro], [r_qb])
                    for g in range(4):
                        V("pe", lambda e, g=g: e.transpose(out=ptq[0:64, g * 128:(g + 1) * 128], in_=qb[:, g, :], identity=ident_b),
                          [r_qb, r_idb], [rb[6]], inc=False, pe_acc=(g > 0))
                    for g in range(4):
                        V("pe", lambda e, g=g: e.transpose(out=ptq[64:128, g * 128:(g + 1) * 128], in_=qb[:, 4 + g, :], identity=ident_b),
                          [r_qb, r_idb], [rb[6]], inc=(g == 3), pe_acc=True)
                    V("act", lambda e, s=s: e.activation(out=QT[:, s, :], in_=ptq[:, 0:512], func=AF.Copy), [rb[6]], [r_QT])
                V("dve", lambda e, s=s: e.tensor_tensor(out=kaug[:, :, 0:64], in0=ro[:, 10:14, :], in1=Wf[:, s, :].unsqueeze(2).to_broadcast([128, 4, 64]), op=ALU.mult),
                  [r_ro, r_Wf], [r_kaug])
                V("dve", lambda e, s=s: e.tensor_tensor(out=kaug[:, :, 64:128], in0=ro[:, 10:14, :], in1=Wb[:, s, :].unsqueeze(2).to_broadcast([128, 4, 64]), op=ALU.mult),
                  [r_ro, r_Wb], [r_kaug])
                V("act", lambda e: e.activation(out=vb[:, 0:256], in_=banks[3][:, 256:512], func=AF.Copy), [rb[3]], [r_vb])
                V("act", lambda e: e.activation(out=vb[:, 256:512], in_=banks[2][:, 256:512], func=AF.Copy), [rb[2]], [r_vb])
                for h in range(4):
                    P.op("pe", lambda e, h=h: e.matmul(banks[7][:, h * 128:(h + 1) * 128], kaug[:, h, :], vb[:, h * 128:(h + 1) * 128], start=True, stop=True),
                         [r_kaug, r_vb], [rb[7]], inc=(h == 3), pe_acc=(h > 0))
                if own:
                    V("dve", lambda e, s=s: e.tensor_copy(out=Ust[:, s, :], in_=banks[7]), [rb[7]], [r_Ust[s]])
                else:
                    V("dve", lambda e: e.tensor_tensor(out=Sacc, in0=Sacc, in1=banks[7], op=ALU.add), [r_Sacc, rb[7]], [r_Sacc])

            P.barrier()
            if stop == 2: raise _Stop()
            A.release(m_sw)

            dmt = A.alloc([256], F32); r_dmt = Res("dmt")
            P.dma("sp", lambda e: e.dma_start(out=dmt, in_=dmat_t[:, :]), r_dmt)
            pct = A.alloc([2], F32); r_pct = Res("pct")
            P.dma("sp", lambda e: e.dma_start(out=pct, in_=pcol_t[:, :]), r_pct)
            DT = A.alloc([4, 128], F32); r_DT = Res("DT")
            tmpd = A.alloc([128], F32); r_tmpd = Res("tmpd")
            for h in range(4):
                V("dve", lambda e, h=h: e.tensor_scalar(out=tmpd, in0=dmt[:, 0:128], scalar1=lg[:, h:h + 1], scalar2=None, op0=ALU.mult), [r_dmt, r_lg], [r_tmpd])
                V("dve", lambda e, h=h: e.scalar_tensor_tensor(out=tmpd, in0=dmt[:, 128:256], scalar=lg[:, 4 + h:5 + h], in1=tmpd, op0=ALU.mult, op1=ALU.add),
                  [r_dmt, r_lg, r_tmpd], [r_tmpd])
                V("act", lambda e, h=h: e.activation(out=DT[:, h, :], in_=tmpd, func=AF.Exp), [r_tmpd], [r_DT])
            xif = A.alloc([4], F32); xib = A.alloc([4], F32); r_xi = Res("xi")
            g128 = A.alloc([4], F32); r_g128 = Res("g128")
            for h in range(4):
                V("act", lambda e, h=h: e.activation(out=xif[:, h:h + 1], in_=pct[:, 0:1], func=AF.Exp, scale=lg[:, h:h + 1]), [r_pct, r_lg], [r_xi])
                V("act", lambda e, h=h: e.activation(out=xib[:, h:h + 1], in_=pct[:, 1:2], func=AF.Exp, scale=lg[:, 4 + h:5 + h]), [r_pct, r_lg], [r_xi])
            V("act", lambda e: e.activation(out=g128[0:64, :], in_=lg[0:64, 0:4], func=AF.Exp, scale=128.0), [r_lg], [r_g128])
            V("act", lambda e: e.activation(out=g128[64:128, :], in_=lg[64:128, 4:8], func=AF.Exp, scale=128.0), [r_lg], [r_g128])
            if SUB == 1: raise _Stop()
            Sst = A.alloc([TPC, 512], BF16); r_Sst = [Res("Sst%d" % i) for i in range(TPC)]
            cur = A.alloc([512], F32); r_cur = Res("cur")
            V("dve", lambda e: e.tensor_copy(out=cur, in_=Sacc), [r_Sacc], [r_cur])
            for c in range(TPC):
                V("dve", lambda e, c=c: e.tensor_copy(out=Sst[0:64, c, :], in_=cur[0:64, :]), [r_cur], [r_Sst[c]])
                for h in range(4):
                    V("dve", lambda e, c=c, h=h: e.scalar_tensor_tensor(out=cur[0:64, h * 128:(h + 1) * 128], in0=cur[0:64, h * 128:(h + 1) * 128],
                                                                       scalar=g128[0:64, h:h + 1], in1=Ust[0:64, c, h * 128:(h + 1) * 128],
                                                                       op0=ALU.mult, op1=ALU.add), [r_cur, r_g128, r_Ust[c]], [r_cur])
            for c in range(TPC - 1, -1, -1):
                V("dve", lambda e, c=c: e.tensor_copy(out=Sst[64:128, c, :], in_=cur[64:128, :]), [r_cur], [r_Sst[c]])
                for h in range(4):
                    V("dve", lambda e, c=c, h=h: e.scalar_tensor_tensor(out=cur[64:128, h * 128:(h + 1) * 128], in0=cur[64:128, h * 128:(h + 1) * 128],
                                                                       scalar=g128[64:128, h:h + 1], in1=Ust[64:128, c, h * 128:(h + 1) * 128],
                                                                       op0=ALU.mult, op1=ALU.add), [r_cur, r_g128, r_Ust[c]], [r_cur])
            if SUB == 2: raise _Stop()
            gng, r_gng = bload(gn_g, 512, "gng")
            gnb, r_gnb = bload(gn_b, 512, "gnb")
            cs2 = [A.alloc([64], F32) for _ in range(2)]; r_cs2 = [Res("cs2_0"), Res("cs2_1")]
            qk = A.alloc([8, 64], BF16); r_qk = Res("qk")
            qaug = A.alloc([4, 128], BF16); r_qaug = Res("qaug")
            qkT = A.alloc([4, 128], BF16); r_qkT = Res("qkT")
            qaT = A.alloc([4, 128], BF16); r_qaT = Res("qaT")
            qpad = A.alloc([4, 128], BF16); r_qpad = Res("qpad")
            V("pool", lambda e: e.memset(qpad, 0.0), [], [r_qpad])
            vb2 = A.alloc([512], BF16); r_vb2 = Res("vb2")
            scm = A.alloc([512], BF16); r_scm = Res("scm")
            gate = A.alloc([512], F32); r_gate = Res("gate")
            osb = A.alloc([512], F32); r_osb = Res("osb")
            osq = A.alloc([512], F32); r_osq = Res("osq")
            st2 = A.alloc([16], F32); r_st2 = Res("st2")
            for c in range(TPC):
                b = c % 2
                P.dma("sp", lambda e, c=c, b=b: e.dma_start(out=cs2[b][:, 0:32], in_=cosv[c]), r_cs2[b])
                P.dma("sp", lambda e, c=c, b=b: e.dma_start(out=cs2[b][:, 32:64], in_=sinv[c]), r_cs2[b])
                hT = hxT_own[:, c, :].rearrange("p (j t) -> p j t", j=8)
                r_dT = r_hxT_own[c]
                mm_group(banks[0][:, 0:256], rb[0], [(hT[:, j, :], w_in_sb[:, j, 768:1024]) for j in range(8)], [r_dT, r_win])
                mm_group(banks[0][:, 256:512], rb[0], [(hT[:, j, :], w_in_sb[:, j, 1024:1280]) for j in range(8)], [r_dT, r_win])
                mm_group(banks[1], rb[1], [(hT[:, j, :], w_in_sb[:, j, 1280:1792]) for j in range(8)], [r_dT, r_win])
                mm_group(banks[2], rb[2], [(hT[:, j, :], w_in_sb[:, j, 1792:2304]) for j in range(8)], [r_dT, r_win])
                V("act", lambda e: e.activation(out=ri[:, 0:4, :], in_=banks[0][:, 0:256].rearrange("p (h d) -> p h d", d=64), func=AF.Copy), [rb[0]], [r_ri])
                V("act", lambda e: e.activation(out=ri[:, 4:8, :], in_=banks[0][:, 256:512].rearrange("p (h d) -> p h d", d=64), func=AF.Copy, scale=0.125), [rb[0]], [r_ri])
                if SUB == 3: raise _Stop()
                rope(0, 8, cs2[b], r_cs2[b])
                if SUB == 4: raise _Stop()
                V("dve", lambda e: e.tensor_copy(out=qk, in_=ro[:, 0:8, :]), [r_ro], [r_qk])
                if SUB == 41: raise _Stop()
                V("dve", lambda e: e.tensor_tensor(out=qaug[:, :, 0:64], in0=ro[:, 0:4, :], in1=xif.unsqueeze(2).to_broadcast([128, 4, 64]), op=ALU.mult), [r_ro, r_xi], [r_qaug])
                V("dve", lambda e: e.tensor_tensor(out=qaug[:, :, 64:128], in0=ro[:, 0:4, :], in1=xib.unsqueeze(2).to_broadcast([128, 4, 64]), op=ALU.mult), [r_ro, r_xi], [r_qaug])
                if SUB == 42: raise _Stop()
                V("act", lambda e: e.activation(out=vb2, in_=banks[1], func=AF.Copy), [rb[1]], [r_vb2])
                if SUB == 43: raise _Stop()
                V("act", lambda e: e.activation(out=gate, in_=banks[2], func=AF.Silu), [rb[2]], [r_gate])
                if SUB == 5: raise _Stop()
                pt3 = bank_bf(3)
                qkf = qk.rearrange("p h d -> p (h d)")
                for i in range(4):
                    V("pe", lambda e, i=i: e.transpose(out=pt3[:, i * 128:(i + 1) * 128], in_=qkf[:, i * 128:(i + 1) * 128], identity=ident_b),
                      [r_qk, r_idb], [rb[3]], inc=False, pe_acc=(i > 0))
                for h in range(4):
                    V("pe", lambda e, h=h: e.transpose(out=pt3[:, 512 + h * 128:512 + (h + 1) * 128], in_=qaug[:, h, :], identity=ident_b),
                      [r_qaug, r_idb], [rb[3]], inc=(h == 3), pe_acc=True)
                if SUB == 51: raise _Stop()
                V("dve", lambda e: e.tensor_copy(out=qkT.rearrange("p a b -> p (a b)"), in_=pt3[:, 0:512]), [rb[3]], [r_qkT])
                if SUB == 52: raise _Stop()
                for h in range(4):
                    pr = (h % 2) * 64
                    V("dve", lambda e, h=h, pr=pr: e.tensor_copy(out=qpad[pr:pr + 64, h, :], in_=pt3[pr:pr + 64, (h // 2) * 128:(h // 2 + 1) * 128]), [rb[3]], [r_qpad])
                if SUB == 53: raise _Stop()
                V("act", lambda e: e.activation(out=qaT.rearrange("p a b -> p (a b)"), in_=pt3[:, 512:1024], func=AF.Copy), [rb[3]], [r_qaT])
                if SUB == 6: raise _Stop()
                for h in range(4):
                    P.op("pe", lambda e, h=h: e.matmul(banks[4][:, h * 128:(h + 1) * 128], qkT[:, 2 + h // 2, :], qpad[:, h, :], start=True, stop=True),
                         [r_qkT, r_qpad], [rb[4]], inc=(h == 3), pe_acc=(h > 0))
                V("dve", lambda e: e.tensor_tensor(out=scm, in0=banks[4], in1=DT.rearrange("p a b -> p (a b)"), op=ALU.mult), [rb[4], r_DT], [r_scm])
                for h in range(4):
                    P.op("pe", lambda e, h=h: e.matmul(banks[5][:, h * 128:(h + 1) * 128], scm[:, h * 128:(h + 1) * 128], vb2[:, h * 128:(h + 1) * 128], start=True, stop=False),
                         [r_scm, r_vb2], [rb[5]], inc=False, pe_acc=(h > 0))
                    P.op("pe", lambda e, h=h, c=c: e.matmul(banks[5][:, h * 128:(h + 1) * 128], qaT[:, h, :], Sst[:, c, h * 128:(h + 1) * 128], start=False, stop=True),
                         [r_qaT, r_Sst[c]], [rb[5]], inc=(h == 3), pe_acc=True)
                if SUB == 7: raise _Stop()
                V("act", lambda e: e.activation(out=osb, in_=banks[5], func=AF.Copy), [rb[5]], [r_osb])
                V("dve", lambda e: e.tensor_reduce(out=st2[:, 0:4], in_=osb.rearrange("p (h d) -> p h d", d=128), axis=AX.X, op=ALU.add), [r_osb], [r_st2])
                V("dve", lambda e: e.tensor_tensor(out=osq, in0=osb, in1=osb, op=ALU.mult), [r_osb], [r_osq])
                V("dve", lambda e: e.tensor_reduce(out=st2[:, 4:8], in_=osq.rearrange("p (h d) -> p h d", d=128), axis=AX.X, op=ALU.add), [r_osq], [r_st2])
                V("dve", lambda e: e.tensor_scalar(out=st2[:, 0:4], in0=st2[:, 0:4], scalar1=1.0 / 128, scalar2=None, op0=ALU.mult), [r_st2], [r_st2])
                V("dve", lambda e: e.tensor_tensor(out=st2[:, 8:12], in0=st2[:, 0:4], in1=st2[:, 0:4], op=ALU.mult), [r_st2], [r_st2])
                V("dve", lambda e: e.scalar_tensor_tensor(out=st2[:, 4:8], in0=st2[:, 4:8], scalar=1.0 / 128, in1=st2[:, 8:12], op0=ALU.mult, op1=ALU.subtract), [r_st2], [r_st2])
                V("act", lambda e: e.activation(out=st2[:, 4:8], in_=st2[:, 4:8], func=AF.Ln, bias=EPS, scale=1.0), [r_st2], [r_st2])
                V("act", lambda e: e.activation(out=st2[:, 4:8], in_=st2[:, 4:8], func=AF.Exp, scale=-0.5), [r_st2], [r_st2])
                for h in range(4):
                    V("dve", lambda e, h=h: e.tensor_scalar(out=osb[:, h * 128:(h + 1) * 128], in0=osb[:, h * 128:(h + 1) * 128], scalar1=st2[:, h:h + 1],
                                                           scalar2=st2[:, 4 + h:5 + h], op0=ALU.subtract, op1=ALU.mult), [r_osb, r_st2], [r_osb])
                V("dve", lambda e: e.tensor_tensor(out=osb, in0=osb, in1=gng, op=ALU.mult), [r_osb, r_gng], [r_osb])
                V("dve", lambda e: e.tensor_tensor(out=osb, in0=osb, in1=gnb, op=ALU.add), [r_osb, r_gnb], [r_osb])
                V("dve", lambda e, c=c: e.tensor_tensor(out=retx[:, c, :], in0=osb, in1=gate, op=ALU.mult), [r_osb, r_gate], [r_retx[c]])
                if debug:
                    V("dve", lambda e: e.tensor_tensor(out=osq, in0=osb, in1=gate, op=ALU.mult), [r_osb, r_gate], [r_osq])
                    P.dma("sp", lambda e, c=c: e.dma_start(out=dbg["d_ret"][c * 128:(c + 1) * 128, :], in_=osq), r_dbg, [r_osq], no_waw=True, semres=r_osq)

            P.barrier()
            if stop == 3: raise _Stop()
            A.release(m_persist)

            attnx = A.alloc([TPC, 512], BF16); r_attnx = [Res("attnx%d" % i) for i in range(TPC)]
            m_att = A.mark()
            KT = A.alloc([NS * 128], BF16); r_KT = Res("KT")
            V1 = A.alloc([NS, 130], BF16); r_V1 = Res("V1")
            P.dma("sp", lambda e: e.dma_start(out=KT, in_=KT_d[:, :]), r_KT, [r_KTd])
            SCH = 10
            for s0 in range(0, NS, SCH):
                s1 = min(NS, s0 + SCH)
                P.dma("act", lambda e, s0=s0, s1=s1: e.dma_start(out=V1[:, s0:s1, :], in_=V_d[s0:s1].rearrange("s p c -> p s c")), r_V1, [r_Vd], no_waw=True)
            if debug:
                dtmp = A.alloc([NS * 130], F32); r_dtmp = Res("dtmp")
                V("dve", lambda e: e.tensor_copy(out=dtmp[:, 0:NS * 128], in_=KT), [r_KT], [r_dtmp])
                P.dma("sp", lambda e: e.dma_start(out=dbg["d_kt"], in_=dtmp[:, 0:NS * 128]), r_dbg, [r_dtmp], no_waw=True, semres=r_dtmp)
                V("dve", lambda e: e.tensor_copy(out=dtmp, in_=V1.rearrange("p a b -> p (a b)")), [r_V1], [r_dtmp])
                P.dma("sp", lambda e: e.dma_start(out=dbg["d_v1"], in_=dtmp), r_dbg, [r_dtmp], no_waw=True, semres=r_dtmp)
                V("dve", lambda e: e.tensor_copy(out=dtmp[:, 0:TPC * 512], in_=QT.rearrange("p a b -> p (a b)")), [r_QT], [r_dtmp])
                P.dma("sp", lambda e: e.dma_start(out=dbg["d_qt"], in_=dtmp[:, 0:TPC * 512]), r_dbg, [r_dtmp], no_waw=True, semres=r_dtmp)
            NPT = 3
            PT = [A.alloc([1024], BF16) for _ in range(NPT)]; r_PT = [Res("PT%d" % i) for i in range(NPT)]
            OT = A.alloc([512], F32); r_OT = Res("OT")
            rcp = A.alloc([8], F32); r_rcp = Res("rcp")
            QP = [[A.alloc([512], BF16) for _ in range(2)] for _ in range(2)]
            r_QP = [[Res("QP%d%d" % (i, j)) for j in range(2)] for i in range(2)]
            for i in range(2):
                for j in range(2):
                    V("pool", lambda e, i=i, j=j: e.memset(QP[i][j], 0.0), [], [r_QP[i][j]])
            groups = [(qt, kvh) for qt in range(TPC) for kvh in range(2)]
            NPAIR = NS // 2
            assert NS % 2 == 0
            iters = [(gi, kp) for gi in range(len(groups)) for kp in range(NPAIR)]

            def prep_q(gi):
                qt, kvh = groups[gi]
                pr = kvh * 64
                qp = QP[kvh][qt % 2]; r_qp = r_QP[kvh][qt % 2]
                V("pool", lambda e: e.tensor_copy(out=qp[pr:pr + 64, :], in_=QT[pr:pr + 64, qt, :]), [r_QT], [r_qp])

            def emit_S(i):
                gi, kp = iters[i]
                qt, kvh = groups[gi]
                qp = QP[kvh][qt % 2]; r_qp = r_QP[kvh][qt % 2]
                b0 = 2 * (i % 2)
                pb = i % NPT
                for u in range(2):
                    kt = 2 * kp + u
                    P.op("pe", lambda e, u=u, kt=kt: e.matmul(banks[b0 + u], KT[:, kt * 128:(kt + 1) * 128], qp, start=True, stop=True),
                         [r_KT, r_qp], [rb[b0 + u]], inc=(u == 1), pe_acc=(u == 1))
                V("act", lambda e: e.activation(out=PT[pb], in_=pbig[:, b0 * 512:(b0 + 2) * 512], func=AF.Exp, scale=0.125, bias=negC),
                  [rb[b0], rb[b0 + 1], r_negC], [r_PT[pb]])

            def emit_PV(i):
                gi, kp = iters[i]
                qt, kvh = groups[gi]
                ob = 4 + gi % 2
                pb = i % NPT
                for u in range(2):
                    kt = 2 * kp + u
                    P.op("pe", lambda e, u=u, kt=kt: e.matmul(banks[ob][0:65, :], V1[:, kt, kvh * 65:(kvh + 1) * 65], PT[pb][:, u * 512:(u + 1) * 512],
                                                              start=(kt == 0), stop=(kt == NS - 1)),
                         [r_V1, r_PT[pb]], [rb[ob]], inc=(kt == NS - 1), pe_acc=(kt > 0))

            def post_a(gi):
                ob = 4 + gi % 2
                V("dve", lambda e: e.tensor_copy(out=OT[0:65, :], in_=banks[ob][0:65, :]), [rb[ob]], [r_OT])

            def post_b(gi):
                qt, kvh = groups[gi]
                for g in range(4):
                    V("pe", lambda e, g=g: e.transpose(out=banks[6][:, g * 66:g * 66 + 65], in_=OT[0:65, g * 128:(g + 1) * 128], identity=ident_f[0:65, 0:65]),
                      [r_OT, r_idf], [rb[6]], inc=(g == 3), pe_acc=(g > 0))
                o4 = banks[6][:, 0:264].rearrange("p (g c) -> p g c", c=66)
                V("dve", lambda e: e.reciprocal(out=rcp[:, 0:4], in_=o4[:, :, 64]), [rb[6]], [r_rcp])
                V("dve", lambda e: e.tensor_tensor(out=attnx[:, qt, kvh * 256:(kvh + 1) * 256].rearrange("p (g d) -> p g d", d=64), in0=o4[:, :, 0:64],
                                                   in1=rcp[:, 0:4].unsqueeze(2).to_broadcast([128, 4, 64]), op=ALU.mult), [rb[6], r_rcp], [r_attnx[qt]])

            prep_q(0)
            if len(groups) > 1:
                prep_q(1)
            nI = len(iters)
            pending_b = {}
            for i in range(nI + 1):
                if i < nI:
                    gi, kt = iters[i]
                    if kt == 0 and gi + 2 < len(groups) and gi >= 0:
                        pass
                    emit_S(i)
                if i >= 1:
                    emit_PV(i - 1)
                    gj, ktj = iters[i - 1]
                    if ktj == NPAIR - 1:
                        post_a(gj)
                        pending_b[i + 3] = gj
                        if gj + 2 < len(groups):
                            prep_q(gj + 2)
                if i in pending_b:
                    post_b(pending_b.pop(i))
            for k in sorted(pending_b):
                post_b(pending_b[k])
            if debug:
                for qt in range(TPC):
                    V("dve", lambda e, qt=qt: e.tensor_copy(out=OT, in_=attnx[:, qt, :]), [r_attnx[qt]], [r_OT])
                    P.dma("sp", lambda e, qt=qt: e.dma_start(out=dbg["d_attn"][qt * 128:(qt + 1) * 128, :], in_=OT), r_dbg, [r_OT], no_waw=True, semres=r_OT)
            P.barrier()
            if stop == 4: raise _Stop()
            A.release(m_att)

            w_out_sb = A.alloc([8, D], BF16); r_wout = Res("wout")
            w_out_v = w_out.rearrange("(j p) n -> p j n", p=128)
            for c0 in range(0, D, 256):
                P.dma("pool", lambda e, c0=c0: e.dma_start(out=w_out_sb[:, :, c0:c0 + 256], in_=w_out_v[:, :, c0:c0 + 256]), r_wout, no_waw=True)
            wrt = A.alloc([8, 36], F32); r_wrt = Res("wrt")
            P.dma("sp", lambda e: e.dma_start(out=wrt, in_=w_rt.rearrange("(j p) n -> p j n", p=128)), r_wrt)
            brt, r_brt = bload(b_rt, 36, "brt")
            mixT = A.alloc([8, 128], BF16); r_mixT = Res("mixT")
            xr = [A.alloc([D], F32) for _ in range(2)]; r_xr = [Res("xr0"), Res("xr1")]
            xn = A.alloc([D], F32); r_xn = Res("xn")
            h2 = A.alloc([D], F32); r_h2 = Res("h2")
            h2b = A.alloc([D], BF16); r_h2b = Res("h2b")
            h2Tf = A.alloc([8, 128], F32); r_h2Tf = Res("h2Tf")
            junk2 = A.alloc([D], BF16); r_junk2 = Res("junk2")
            lgt = A.alloc([36], F32); r_lgt = Res("lgt")
            msk = A.alloc([32], F32); r_msk = Res("msk")
            m8 = A.alloc([8], F32); r_m8 = Res("m8")
            sm3 = A.alloc([16], F32); r_sm3 = Res("sm3")
            oh = A.alloc([64], F32); r_oh = Res("oh")
            xown = xp.rearrange("(s p) n -> s p n", p=128)
            for c in range(TPC):
                b = c % 2
                P.dma("sp", lambda e, c=c, b=b: e.dma_start(out=xr[b], in_=xown[c]), r_xr[b])
                pt0 = bank_bf(0)
                for j in range(8):
                    src = attnx[:, c, j * 128:(j + 1) * 128] if j < 4 else retx[:, c, (j - 4) * 128:(j - 3) * 128]
                    rs_ = r_attnx[c] if j < 4 else r_retx[c]
                    V("pe", lambda e, j=j, src=src: e.transpose(out=pt0[:, j * 128:(j + 1) * 128], in_=src, identity=ident_b), [rs_, r_idb], [rb[0]], inc=(j == 7), pe_acc=(j > 0))
                V("act", lambda e: e.activation(out=mixT.rearrange("p a b -> p (a b)"), in_=pt0, func=AF.Copy), [rb[0]], [r_mixT])
                for hf in range(2):
                    mm_group(banks[1 + hf], rb[1 + hf], [(mixT[:, j, :], w_out_sb[:, j, hf * 512:(hf + 1) * 512]) for j in range(8)], [r_mixT, r_wout])
                for hf in range(2):
                    sl = slice(hf * 512, (hf + 1) * 512)
                    V("dve", lambda e, hf=hf, sl=sl: e.tensor_tensor(out=xn[:, sl], in0=banks[1 + hf], in1=GT1[:, sl], op=ALU.mult), [rb[1 + hf], r_GT1], [r_xn])
                V("dve", lambda e, b=b: e.tensor_tensor(out=xn, in0=xn, in1=xr[b], op=ALU.add), [r_xn, r_xr[b]], [r_xn])
                P.dma("sp", lambda e, c=c: e.dma_start(out=XN_d[c * 128:(c + 1) * 128, :], in_=xn), r_XNd, [r_xn], no_waw=True, semres=r_xn)
                if debug:
                    P.dma("sp", lambda e, c=c: e.dma_start(out=dbg["d_xnew"][c * 128:(c + 1) * 128, :], in_=xn), r_dbg, [r_xn], no_waw=True, semres=r_xn)
                V("act", lambda e: e.activation(out=junk2, in_=xn, func=AF.Square, accum_out=sm3[:, 0:1]), [r_xn], [r_junk2, r_sm3])
                V("act", lambda e: e.activation(out=sm3[:, 1:2], in_=sm3[:, 0:1], func=AF.Ln, scale=1.0 / D, bias=EPS), [r_sm3], [r_sm3])
                V("act", lambda e: e.activation(out=sm3[:, 1:2], in_=sm3[:, 1:2], func=AF.Exp, scale=-0.5), [r_sm3], [r_sm3])
                V("dve", lambda e: e.scalar_tensor_tensor(out=h2, in0=xn, scalar=sm3[:, 1:2], in1=G2, op0=ALU.mult, op1=ALU.mult), [r_xn, r_sm3, r_G2], [r_h2])
                V("dve", lambda e: e.tensor_tensor(out=h2, in0=h2, in1=SH2, op=ALU.add), [r_h2, r_SH2], [r_h2])
                V("pool", lambda e: e.tensor_copy(out=h2b, in_=h2), [r_h2], [r_h2b])
                pt3 = bank_bf(3)
                for j in range(8):
                    V("pe", lambda e, j=j: e.transpose(out=pt3[:, j * 128:(j + 1) * 128], in_=h2b[:, j * 128:(j + 1) * 128], identity=ident_b), [r_h2b, r_idb], [rb[3]], inc=(j == 7), pe_acc=(j > 0))
                V("act", lambda e, c=c: e.activation(out=h2T[:, :, c * 128:(c + 1) * 128], in_=pt3.rearrange("p (j t) -> p j t", j=8), func=AF.Copy), [rb[3]], [r_h2T[c]])
                for j in range(8):
                    bk = 4 + j // 4
                    V("pe", lambda e, j=j, bk=bk: e.transpose(out=banks[bk][:, (j % 4) * 128:(j % 4 + 1) * 128], in_=h2[:, j * 128:(j + 1) * 128], identity=ident_f), [r_h2, r_idf], [rb[bk]],
                      inc=(j % 4 == 3), pe_acc=(j % 4 > 0))
                V("dve", lambda e: e.tensor_copy(out=h2Tf[:, 0:4, :].rearrange("p a b -> p (a b)"), in_=banks[4]), [rb[4]], [r_h2Tf])
                V("dve", lambda e: e.tensor_copy(out=h2Tf[:, 4:8, :].rearrange("p a b -> p (a b)"), in_=banks[5]), [rb[5]], [r_h2Tf])
                mm_group(banks[6][:, 0:36], rb[6], [(h2Tf[:, j, :], wrt[:, j, :]) for j in range(8)], [r_h2Tf, r_wrt])
                V("dve", lambda e: e.tensor_tensor(out=lgt, in0=banks[6][:, 0:36], in1=brt, op=ALU.add), [rb[6], r_brt], [r_lgt])
                V("dve", lambda e: e.tensor_reduce(out=sm3[:, 2:3], in_=lgt[:, 0:4], axis=AX.X, op=ALU.max), [r_lgt], [r_sm3])
                V("dve", lambda e: e.tensor_scalar(out=oh[:, 0:4], in0=lgt[:, 0:4], scalar1=sm3[:, 2:3], scalar2=None, op0=ALU.is_equal), [r_lgt, r_sm3], [r_oh])
                V("dve", lambda e: e.tensor_scalar(out=sm3[:, 3:4], in0=sm3[:, 2:3], scalar1=-1.0, scalar2=None, op0=ALU.mult), [r_sm3], [r_sm3])
                V("act", lambda e: e.activation(out=oh[:, 8:12], in_=lgt[:, 0:4], func=AF.Exp, bias=sm3[:, 3:4], scale=1.0, accum_out=sm3[:, 4:5]), [r_lgt, r_sm3], [r_oh, r_sm3])
                V("dve", lambda e: e.reciprocal(out=sm3[:, 5:6], in_=sm3[:, 4:5]), [r_sm3], [r_sm3])
                V("dve", lambda e: e.tensor_scalar(out=oh[:, 4:8], in0=oh[:, 0:4], scalar1=-1.0, scalar2=1e30, op0=ALU.add, op1=ALU.mult), [r_oh], [r_oh])
                V("dve", lambda e: e.tensor_tensor(out=msk.rearrange("p (g j) -> p g j", j=8), in0=lgt[:, 4:36].rearrange("p (g j) -> p g j", j=8),
                                                   in1=oh[:, 4:8].unsqueeze(2).to_broadcast([128, 4, 8]), op=ALU.add), [r_lgt, r_oh], [r_msk])
                V("dve", lambda e: e.max(out=m8, in_=msk), [r_msk], [r_m8])
                V("dve", lambda e: e.tensor_tensor(out=sm3[:, 6:7], in0=m8[:, 1:2], in1=m8[:, 0:1], op=ALU.subtract), [r_m8], [r_sm3])
                V("act", lambda e: e.activation(out=sm3[:, 6:7], in_=sm3[:, 6:7], func=AF.Exp), [r_sm3], [r_sm3])
                V("dve", lambda e: e.tensor_scalar(out=sm3[:, 6:7], in0=sm3[:, 6:7], scalar1=1.0, scalar2=None, op0=ALU.add), [r_sm3], [r_sm3])
                V("dve", lambda e: e.reciprocal(out=sm3[:, 7:8], in_=sm3[:, 6:7]), [r_sm3], [r_sm3])
                V("dve", lambda e: e.tensor_tensor(out=sm3[:, 8:9], in0=sm3[:, 7:8], in1=sm3[:, 5:6], op=ALU.mult), [r_sm3], [r_sm3])
                V("dve", lambda e: e.tensor_tensor(out=sm3[:, 9:10], in0=sm3[:, 5:6], in1=sm3[:, 8:9], op=ALU.subtract), [r_sm3], [r_sm3])
                V("dve", lambda e: e.tensor_scalar(out=oh[:, 0:32], in0=msk, scalar1=m8[:, 0:1], scalar2=sm3[:, 8:9], op0=ALU.is_equal, op1=ALU.mult), [r_msk, r_m8, r_sm3], [r_oh])
                V("dve", lambda e: e.tensor_scalar(out=oh[:, 32:64], in0=msk, scalar1=m8[:, 1:2], scalar2=sm3[:, 9:10], op0=ALU.is_equal, op1=ALU.mult), [r_msk, r_m8, r_sm3], [r_oh])
                V("dve", lambda e, c=c: e.tensor_tensor(out=Wtok[:, c, :], in0=oh[:, 0:32], in1=oh[:, 32:64], op=ALU.add), [r_oh], [r_Wtok[c]])
                if debug:
                    P.dma("sp", lambda e, c=c: e.dma_start(out=dbg["d_wtok"][c * 128:(c + 1) * 128, :], in_=Wtok[:, c, :]), r_dbg, [r_Wtok[c]], no_waw=True, semres=r_Wtok[c])
            P.barrier()
            if stop == 5: raise _Stop()
            A.release(m_moe)
            FG, r_FG = bload(final_g, D, "FG")

            yacc = A.alloc([TPC, D], F32); r_yacc = [Res("yacc%d" % i) for i in range(TPC)]
            for c in range(TPC):
                V("pool", lambda e, c=c: e.memset(yacc[:, c, :], 0.0), [], [r_yacc[c]])
            NWB = 2
            wgs = [A.alloc([8, 512], BF16) for _ in range(NWB)]
            wus = [A.alloc([8, 512], BF16) for _ in range(NWB)]
            wds = [A.alloc([4, D], BF16) for _ in range(NWB)]
            r_wgs = [Res("wgs%d" % i) for i in range(NWB)]; r_wus = [Res("wus%d" % i) for i in range(NWB)]; r_wds = [Res("wds%d" % i) for i in range(NWB)]
            sg = [A.alloc([512], F32) for _ in range(2)]; r_sg = [Res("sg0"), Res("sg1")]
            TG = min(4, TPC)
            NG = TPC // TG
            h1T = [A.alloc([4, TG * 128], BF16) for _ in range(2)]; r_h1T = [Res("h1T0"), Res("h1T1")]
            gi = 0
            for ex in range(32):
                wbf = ex % NWB
                for c0 in range(0, 512, 256):
                    P.dma("pool", lambda e, ex=ex, wbf=wbf, c0=c0: e.dma_start(out=wgs[wbf][:, :, c0:c0 + 256], in_=wg[ex].rearrange("(j p) n -> p j n", p=128)[:, :, c0:c0 + 256]), r_wgs[wbf], no_waw=(c0 > 0))
                for c0 in range(0, 512, 256):
                    P.dma("pool", lambda e, ex=ex, wbf=wbf, c0=c0: e.dma_start(out=wus[wbf][:, :, c0:c0 + 256], in_=wu[ex].rearrange("(j p) n -> p j n", p=128)[:, :, c0:c0 + 256]), r_wus[wbf], no_waw=(c0 > 0))
                for c0 in range(0, D, 256):
                    P.dma("pool", lambda e, ex=ex, wbf=wbf, c0=c0: e.dma_start(out=wds[wbf][:, :, c0:c0 + 256], in_=wd[ex].rearrange("(j p) n -> p j n", p=128)[:, :, c0:c0 + 256]), r_wds[wbf], no_waw=(c0 > 0))
                for tg in range(NG):
                    hb = gi % 2
                    gi += 1
                    N = TG * 128
                    tsl = slice(tg * N, (tg + 1) * N)
                    r_h2g = [r_h2T[tg * TG + i] for i in range(TG)]
                    for ec in range(4):
                        gb_ = (ec % 2) * 2
                        mm_group(banks[gb_][:, 0:N], rb[gb_], [(wgs[wbf][:, j, ec * 128:(ec + 1) * 128], h2T[:, j, tsl]) for j in range(8)], r_h2g + [r_wgs[wbf]])
                        mm_group(banks[gb_ + 1][:, 0:N], rb[gb_ + 1], [(wus[wbf][:, j, ec * 128:(ec + 1) * 128], h2T[:, j, tsl]) for j in range(8)], r_h2g + [r_wus[wbf]])
                        sb2 = ec % 2
                        V("act", lambda e, gb_=gb_, sb2=sb2, N=N: e.activation(out=sg[sb2][:, 0:N], in_=banks[gb_][:, 0:N], func=AF.Silu), [rb[gb_]], [r_sg[sb2]])
                        V("dve", lambda e, gb_=gb_, sb2=sb2, hb=hb, ec=ec, N=N: e.tensor_tensor(out=h1T[hb][:, ec, :], in0=banks[gb_ + 1][:, 0:N], in1=sg[sb2][:, 0:N], op=ALU.mult),
                          [rb[gb_ + 1], r_sg[sb2]], [r_h1T[hb]])
                    for i in range(TG):
                        c = tg * TG + i
                        for hf in range(2):
                            ob = 4 + (i * 2 + hf) % 4
                            mm_group(banks[ob], rb[ob], [(h1T[hb][:, ec, i * 128:(i + 1) * 128], wds[wbf][:, ec, hf * 512:(hf + 1) * 512]) for ec in range(4)], [r_h1T[hb], r_wds[wbf]])
                            V("dve", lambda e, ob=ob, c=c, hf=hf, ex=ex: e.scalar_tensor_tensor(out=yacc[:, c, hf * 512:(hf + 1) * 512], in0=banks[ob], scalar=Wtok[:, c, ex:ex + 1],
                                                                                       in1=yacc[:, c, hf * 512:(hf + 1) * 512], op0=ALU.mult, op1=ALU.add),
                              [rb[ob], r_Wtok[c], r_yacc[c]], [r_yacc[c]])
            xq = [A.alloc([D], F32) for _ in range(2)]; r_xq = [Res("xq0"), Res("xq1")]
            junk3 = A.alloc([D], BF16); r_junk3 = Res("junk3")
            sm4 = A.alloc([4], F32); r_sm4 = Res("sm4")
            for c in range(TPC):
                b = c % 2
                P.dma("sp", lambda e, c=c, b=b: e.dma_start(out=xq[b], in_=XN_d[c * 128:(c + 1) * 128, :]), r_xq[b], [r_XNd])
                V("dve", lambda e, c=c: e.tensor_tensor(out=yacc[:, c, :], in0=yacc[:, c, :], in1=GT2, op=ALU.mult), [r_yacc[c], r_GT2], [r_yacc[c]])
                V("dve", lambda e, c=c, b=b: e.tensor_tensor(out=xq[b], in0=xq[b], in1=yacc[:, c, :], op=ALU.add), [r_xq[b], r_yacc[c]], [r_xq[b]])
                V("act", lambda e, b=b: e.activation(out=junk3, in_=xq[b], func=AF.Square, accum_out=sm4[:, 0:1]), [r_xq[b]], [r_junk3, r_sm4])
                V("act", lambda e: e.activation(out=sm4[:, 1:2], in_=sm4[:, 0:1], func=AF.Ln, scale=1.0 / D, bias=EPS), [r_sm4], [r_sm4])
                V("act", lambda e: e.activation(out=sm4[:, 1:2], in_=sm4[:, 1:2], func=AF.Exp, scale=-0.5), [r_sm4], [r_sm4])
                V("dve", lambda e, b=b: e.scalar_tensor_tensor(out=xq[b], in0=xq[b], scalar=sm4[:, 1:2], in1=FG, op0=ALU.mult, op1=ALU.mult), [r_xq[b], r_sm4, r_FG], [r_xq[b]])
                P.dma("sp", lambda e, c=c, b=b: e.dma_start(out=out[c * 128:(c + 1) * 128, :], in_=xq[b]), r_out, [r_xq[b]], no_waw=True, semres=r_xq[b])
        try:
            body()
        except _Stop:
            P.barrier()
        fin = [r_out] + ([r_dbg] if debug else [])
        P.wait_all("sp", fin)
        P.wait_all("pool", fin)
        P.emit(st)
    return nc


def _rope_tables(L):
    GRID_W = 64
    rows = L // GRID_W
    row = np.repeat(np.arange(rows), GRID_W).astype(np.float32)
    col = np.tile(np.arange(GRID_W), rows).astype(np.float32)
    freqs = (np.float32(10000.0) ** (-np.arange(0, 32, 2, dtype=np.float32) / np.float32(32))).astype(np.float32)
    ang = np.concatenate([row[:, None] * freqs, col[:, None] * freqs], axis=-1).astype(np.float32)
    return np.cos(ang).astype(np.float32), np.sin(ang).astype(np.float32)


def make_in_maps(inputs, NT):
    TPC = NT // NCORES
    NS = NT + 2
    L = NT * 128
    f = lambda a: np.ascontiguousarray(np.asarray(a, dtype=np.float32))
    x = f(inputs["x"]).reshape(L, D)
    ctx = f(inputs["ctx"]).reshape(256, D)
    cos, sin = _rope_tables(L)
    cvecT = np.zeros((128, 16), np.float32)
    cvecT[:, 0:8] = f(inputs["c"]).reshape(8, 128).T
    cvecT[:, 8:16] = f(inputs["c_ctx"]).reshape(8, 128).T
    p = np.arange(128, dtype=np.float32)
    dpos = np.maximum(p[None, :] - p[:, None], 0.0)
    dneg = np.maximum(p[:, None] - p[None, :], 0.0)
    dmat = np.concatenate([dpos, dneg], axis=1).astype(np.float32)
    pcol = np.stack([p + 1.0, 128.0 - p], axis=1).astype(np.float32)
    shared = {
        "cvecT": cvecT,
        "w_ada": f(inputs["w_ada"]).reshape(D, 6 * D), "b_ada": f(inputs["b_ada"]).reshape(1, 6 * D),
        "norm1_g": f(inputs["norm1_g"]).reshape(1, D), "norm2_g": f(inputs["norm2_g"]).reshape(1, D),
        "final_g": f(inputs["final_norm_g"]).reshape(1, D),
        "w_in": f(inputs["w_in"]).reshape(D, 2304),
        "qn": f(inputs["attn_q_norm"]).reshape(1, 64), "kn": f(inputs["attn_k_norm"]).reshape(1, 64),
        "dec": np.concatenate([f(inputs["ret_decay_fwd"]).reshape(1, 4), f(inputs["ret_decay_bwd"]).reshape(1, 4)], axis=1),
        "gn_g": f(inputs["ret_gn_g"]).reshape(1, 512), "gn_b": f(inputs["ret_gn_b"]).reshape(1, 512),
        "w_out": f(inputs["w_out"]).reshape(D, D),
        "w_rt": np.ascontiguousarray(np.concatenate([f(inputs["moe_w_grp"]).reshape(D, 4), f(inputs["moe_w_exp"]).reshape(D, 32)], axis=1)),
        "b_rt": np.concatenate([f(inputs["moe_b_grp"]).reshape(1, 4), f(inputs["moe_b_exp"]).reshape(1, 32)], axis=1),
        "wg": f(inputs["moe_w_gate"]).reshape(32, D, 512), "wu": f(inputs["moe_w_up"]).reshape(32, D, 512),
        "wd": f(inputs["moe_w_down"]).reshape(32, 512, D),
        "dmat_t": dmat, "pcol_t": pcol,
    }
    maps = []
    for r in range(NCORES):
        own = list(range(r * TPC, (r + 1) * TPC))
        others = [t for t in range(NT) if t not in own]
        order = own + others
        rows = np.concatenate([np.arange(t * 128, (t + 1) * 128) for t in order])
        xpm = np.concatenate([x[rows], ctx], axis=0)
        cosp = np.concatenate([cos[rows], np.ones((256, 32), np.float32)], axis=0)
        sinp = np.concatenate([sin[rows], np.zeros((256, 32), np.float32)], axis=0)
        s0 = r * TPC * 128
        e0 = s0 + TPC * 128
        distf = np.zeros((128, NS), np.float32); distb = np.zeros((128, NS), np.float32)
        maskf = np.zeros((128, NS), np.float32); maskb = np.zeros((128, NS), np.float32)
        for s, t in enumerate(order):
            m = t * 128 + p
            if s < TPC:
                distf[:, s] = 127.0 - p; distb[:, s] = p; maskf[:, s] = 1.0; maskb[:, s] = 1.0
            elif t * 128 < s0:
                distf[:, s] = s0 - 1 - m; maskf[:, s] = 1.0
            else:
                distb[:, s] = m - e0; maskb[:, s] = 1.0
        for j in range(2):
            mc = j * 128 + p
            distf[:, NT + j] = s0 + 255 - mc; maskf[:, NT + j] = 1.0
            distb[:, NT + j] = L + mc - e0; maskb[:, NT + j] = 1.0
        d = dict(shared)
        d["xp"] = np.ascontiguousarray(xpm)
        d["cos_t"] = np.ascontiguousarray(cosp)
        d["sin_t"] = np.ascontiguousarray(sinp)
        d["dist_t"] = np.ascontiguousarray(np.concatenate([distf, distb, maskf, maskb], axis=1))
        maps.append(d)
    return maps


_NC_CACHE = {}


def run(inputs, NT, debug=False, stop=99, cores=None):
    key = (NT, debug, stop)
    if key not in _NC_CACHE:
        _NC_CACHE[key] = build_program(NT, debug, stop)
    nc = _NC_CACHE[key]
    maps = make_in_maps(inputs, NT)
    if stop < 6:
        for m in maps:
            for k in ("wg", "wu", "wd"):
                m[k] = m[k][0:1]
    if cores is not None:
        maps = [maps[c] for c in cores]
        return run_bass_kernel_spmd(nc, maps, core_ids=list(range(len(cores))))
    res = run_bass_kernel_spmd(nc, maps, core_ids=list(range(NCORES)))
    return res


def kernel(**inputs):
    NT = 128
    res = run(inputs, NT)
    outp = np.concatenate([np.asarray(res.results[r]["out"]) for r in range(NCORES)], axis=0)
    return outp.reshape(1, NT * 128, D).astype(np.float32)
```

```python
import numpy as np
from contextlib import ExitStack
import concourse.bass as bass
import concourse.mybir as mybir
from concourse.bass_utils import run_bass_kernel_spmd

F32 = mybir.dt.float32
BF16 = mybir.dt.bfloat16
I32 = mybir.dt.int32
U32 = mybir.dt.uint32
AF = mybir.ActivationFunctionType
ALU = mybir.AluOpType
AX = mybir.AxisListType

ENGS = ("pe", "act", "dve", "pool", "sp")
NCORES = 8
D = 1024
EPS = 1e-6


import types


def _freeze(fn):
    if fn is None or fn.__closure__ is None:
        return fn
    cells = []
    for c in fn.__closure__:
        try:
            cells.append(types.CellType(c.cell_contents))
        except ValueError:
            cells.append(c)
    return types.FunctionType(fn.__code__, fn.__globals__, fn.__name__, fn.__defaults__, tuple(cells))


class Res:
    __slots__ = ("name", "w", "rs", "dsem", "dcount", "excl")

    def __init__(self, name, excl=False):
        self.name = name
        self.excl = excl
        self.w = {}
        self.rs = {}
        self.dsem = None
        self.dcount = 0


class Prog:
    def __init__(self, nc):
        self.nc = nc
        self.ops = {e: [] for e in ENGS}
        self.cnt = {e: 0 for e in ENGS}
        self.known = {e: {} for e in ENGS}
        self.semnames = []
        self.dres = []

    def _deps(self, eng, reads, writes):
        best = {}

        def add(d):
            for s, v in d.items():
                if v > best.get(s, 0):
                    best[s] = v
        for r in reads:
            add(r.w)
        for w in writes:
            add(w.w)
            add(w.rs)
        waits = []
        kn = self.known[eng]
        for s, v in best.items():
            if kn.get(s, 0) < v:
                kn[s] = v
                waits.append((s, v))
        return waits

    def op(self, eng, fn, reads=(), writes=(), inc=True, pe_acc=False):
        fn = _freeze(fn)
        xr = [r for r in reads if r.excl]
        if xr:
            writes = list(writes) + xr
            reads = [r for r in reads if not r.excl]
        waits = self._deps(eng, reads, writes)
        if pe_acc:
            waits = [(s, v) for (s, v) in waits if s != "E_pe"]
        sem = "E_" + eng
        val = self.cnt[eng] + 1
        if inc:
            self.cnt[eng] = val
        self.ops[eng].append(("op", fn, waits, sem if inc else None))
        for r in reads:
            if r.rs.get(sem, 0) < val:
                r.rs[sem] = val
        for w in writes:
            w.w = {sem: val}
            w.rs = {}
        return (sem, val)

    def dma(self, eng, fn, dst, reads=(), no_waw=False, semres=None):
        fn = _freeze(fn)
        owner = semres if semres is not None else dst
        wl = [] if no_waw else [dst]
        if semres is not None:
            wl.append(semres)
        rl = list(reads)
        waits = self._deps(eng, rl, wl)
        if owner.dsem is None:
            owner.dsem = "D%d_%s" % (len(self.semnames), owner.name)
            self.semnames.append(owner.dsem)
            self.dres.append(owner)
        owner.dcount += 16
        sem, val = owner.dsem, owner.dcount
        self.ops[eng].append(("dma", fn, waits, sem))
        for r in rl:
            if r.rs.get(sem, 0) < val:
                r.rs[sem] = val
        if semres is not None:
            semres.rs[sem] = val
        if no_waw:
            dst.w[sem] = val
        else:
            dst.w = {sem: val}
            dst.rs = {}
        return (sem, val)

    def wait_all(self, eng, ress):
        waits = self._deps(eng, ress, [])
        self.ops[eng].append(("wait", None, waits, None))

    def barrier(self):
        evs = [("E_" + e, self.cnt[e]) for e in ENGS if self.cnt[e] > 0]
        evs += [(r.dsem, r.dcount) for r in self.dres]
        for e in ENGS:
            kn = self.known[e]
            waits = []
            for (s, v) in evs:
                if kn.get(s, 0) < v:
                    kn[s] = v
                    waits.append((s, v))
            if waits:
                self.ops[e].append(("wait", None, waits, None))

    def emit(self, stack):
        nc = self.nc
        sems = {}
        for e in ENGS:
            sems["E_" + e] = stack.enter_context(nc.semaphore("E_" + e))
        for n in self.semnames:
            sems[n] = stack.enter_context(nc.semaphore(n))
        block = stack.enter_context(nc.Block())
        engmap = {"pe": block.tensor, "act": block.scalar, "dve": block.vector,
                  "pool": block.gpsimd, "sp": block.sync}
        for e in ENGS:
            ops = self.ops[e]
            if not ops:
                continue

            def body(engine, ops=ops):
                for kind, fn, waits, sem in ops:
                    for (s, v) in waits:
                        engine.wait_ge(sems[s], v)
                    if kind == "op":
                        ins = fn(engine)
                        if sem is not None:
                            ins.then_inc(sems[sem], 1)
                    elif kind == "dma":
                        ins = fn(engine)
                        ins.then_inc(sems[sem], 16)
            engmap[e](body)


class Arena:
    def __init__(self, t, nwords, start=0):
        self.t = t
        self.n = nwords
        self.top = start

    def alloc(self, shape, dt):
        esz = 2 if dt == BF16 else 4
        nel = int(np.prod(shape))
        words = (nel * esz + 3) // 4
        words = (words + 7) // 8 * 8
        assert self.top + words <= self.n, ("arena overflow", self.top, words, self.n)
        ap = self.t[:, self.top:self.top + words]
        self.top += words
        if dt != F32:
            ap = ap.bitcast(dt)
        ap = ap[:, 0:nel]
        if len(shape) == 2:
            ap = ap.rearrange("p (a b) -> p a b", a=shape[0])
        elif len(shape) == 3:
            ap = ap.rearrange("p (a b c) -> p a b c", a=shape[0], b=shape[1])
        return ap

    def mark(self):
        return self.top

    def release(self, m):
        self.top = m


class _Stop(Exception):
    pass


def build_program(NT, debug=False, stop=99):
    import os
    SUB = int(os.environ.get('KSUB', '0'))
    TPC = NT // NCORES
    NS = NT + 2
    nc = bass.Bass("TRN2", target_bir_lowering=False)

    def din(name, shape, dt=F32):
        return nc.dram_tensor(name, shape, dt, kind="ExternalInput").ap()

    xp = din("xp", [NS * 128, D])
    cvecT = din("cvecT", [128, 16])
    w_ada = din("w_ada", [D, 6 * D])
    b_ada = din("b_ada", [1, 6 * D])
    norm1_g = din("norm1_g", [1, D])
    norm2_g = din("norm2_g", [1, D])
    final_g = din("final_g", [1, D])
    w_in = din("w_in", [D, 2304])
    qn = din("qn", [1, 64])
    kn = din("kn", [1, 64])
    dec = din("dec", [1, 8])
    gn_g = din("gn_g", [1, 512])
    gn_b = din("gn_b", [1, 512])
    w_out = din("w_out", [D, D])
    w_rt = din("w_rt", [D, 36])
    b_rt = din("b_rt", [1, 36])
    NEXP = 32 if stop >= 6 else 1
    wg = din("wg", [NEXP, D, 512])
    wu = din("wu", [NEXP, D, 512])
    wd = din("wd", [NEXP, 512, D])
    cos_t = din("cos_t", [NS * 128, 32])
    sin_t = din("sin_t", [NS * 128, 32])
    dist_t = din("dist_t", [128, 4 * NS])
    dmat_t = din("dmat_t", [128, 256])
    pcol_t = din("pcol_t", [128, 2])
    out = nc.dram_tensor("out", [TPC * 128, D], F32, kind="ExternalOutput").ap()
    dbg = {}
    if debug:
        for nm, shp in [("d_hx", [128, D]), ("d_attn", [TPC * 128, 512]), ("d_ret", [TPC * 128, 512]),
                        ("d_xnew", [TPC * 128, D]), ("d_wtok", [TPC * 128, 32]), ("d_g1", [128, D]),
                        ("d_kt", [128, NS * 128]), ("d_v1", [128, NS * 130]), ("d_qt", [128, TPC * 512])]:
            dbg[nm] = nc.dram_tensor(nm, shp, F32, kind="ExternalOutput").ap()

    KT_d = nc.dram_tensor("KT_d", [128, NS * 128], BF16).ap()
    V_d = nc.dram_tensor("V_d", [NS, 128, 130], BF16).ap()
    XN_d = nc.dram_tensor("XN_d", [TPC * 128, D], F32).ap()
    r_KTd = Res("KTd"); r_Vd = Res("Vd"); r_XNd = Res("XNd"); r_out = Res("out"); r_dbg = Res("dbg")

    P = Prog(nc)
    st = ExitStack()
    with st:
        ARW = 51 * 1024
        arena_t = st.enter_context(nc.sbuf_tensor("arena", [128, ARW], F32))
        A = Arena(arena_t, ARW)
        pbig = st.enter_context(nc.psum_tensor("pbig", [128, 4096], F32))
        banks = [pbig[:, i * 512:(i + 1) * 512] for i in range(8)]
        rb = [Res("bank%d" % i, excl=True) for i in range(8)]

        def bank_bf(i):
            return banks[i].bitcast(BF16)

        def V(eng, fn, reads, writes, **kw):
            return P.op(eng, fn, reads, writes, **kw)

        def mm_group(ps_ap, ps_res, pairs, reads):
            n = len(pairs)
            for i, (l, r) in enumerate(pairs):
                P.op("pe", lambda e, l=l, r=r, i=i: e.matmul(ps_ap, l, r, start=(i == 0), stop=(i == n - 1)),
                     reads, [ps_res], inc=(i == n - 1), pe_acc=(i > 0))

        def body():
            nonlocal A
            ident_f = A.alloc([128], F32); r_idf = Res("idf")
            ident_b = A.alloc([128], BF16); r_idb = Res("idb")
            V("pool", lambda e: e.memset(ident_f, 0.0), [], [r_idf])
            V("pool", lambda e: e.affine_select(out=ident_f, in_=ident_f, pattern=[[-1, 128]], compare_op=ALU.not_equal,
                                                fill=1.0, base=0, channel_multiplier=1), [r_idf], [r_idf])
            V("dve", lambda e: e.tensor_copy(out=ident_b, in_=ident_f), [r_idf], [r_idb])

            def bload(dram_row, n, name):
                t = A.alloc([n], F32); r = Res(name)
                P.dma("sp", lambda e: e.dma_start(out=t, in_=dram_row.partition_broadcast(128)), r)
                return t, r

            GT1 = A.alloc([D], F32); r_GT1 = Res("GT1")
            G2 = A.alloc([D], F32); r_G2 = Res("G2")
            SH2 = A.alloc([D], F32); r_SH2 = Res("SH2")
            GT2 = A.alloc([D], F32); r_GT2 = Res("GT2")
            dec_bc, r_dec = bload(dec, 8, "dec")
            qn_bc, r_qn = bload(qn, 64, "qn")
            kn_bc, r_kn = bload(kn, 64, "kn")
            lg = A.alloc([8], F32); r_lg = Res("lg")
            V("act", lambda e: e.activation(out=lg, in_=dec_bc, func=AF.Exp, scale=-1.0), [r_dec], [r_lg])
            V("act", lambda e: e.activation(out=lg, in_=lg, func=AF.Ln, bias=1.0, scale=1.0), [r_lg], [r_lg])
            V("dve", lambda e: e.tensor_scalar(out=lg, in0=lg, scalar1=-1.0, scalar2=None, op0=ALU.mult), [r_lg], [r_lg])
            negC = A.alloc([1], F32); r_negC = Res("negC")
            mk_ = A.alloc([1], F32); r_mk = Res("mk")
            V("dve", lambda e: e.tensor_reduce(out=negC, in_=qn_bc, axis=AX.X, op=ALU.max, apply_absolute_value=True), [r_qn], [r_negC])
            V("dve", lambda e: e.tensor_reduce(out=mk_, in_=kn_bc, axis=AX.X, op=ALU.max, apply_absolute_value=True), [r_kn], [r_mk])
            V("dve", lambda e: e.tensor_tensor(out=negC, in0=negC, in1=mk_, op=ALU.mult), [r_negC, r_mk], [r_negC])
            V("dve", lambda e: e.tensor_scalar(out=negC, in0=negC, scalar1=-8.0, scalar2=None, op0=ALU.mult), [r_negC], [r_negC])
            Wtok = A.alloc([TPC, 32], F32); r_Wtok = [Res("Wtok%d" % i) for i in range(TPC)]
            m_scrA0 = A.mark()
            actT = A.alloc([TPC * 1024], BF16)
            hxT_own = actT.rearrange("p (c x) -> p c x", c=TPC); r_hxT_own = [Res("hxTo%d" % i) for i in range(TPC)]
            h2T = actT.rearrange("p (j t) -> p j t", j=8); r_h2T = [Res("h2T%d" % i) for i in range(TPC)]
            m_moe = A.mark()
            retx = A.alloc([TPC, 512], BF16); r_retx = [Res("retx%d" % i) for i in range(TPC)]
            QT = A.alloc([TPC, 512], BF16); r_QT = Res("QT")
            m_persist = A.mark()
            m_scrA1 = A.mark()
            w_in_sb = A.alloc([8, 2304], BF16); r_win = Res("win")
            w_in_v = w_in.rearrange("(j p) n -> p j n", p=128)
            for c0 in range(0, 2304, 256):
                P.dma("pool", lambda e, c0=c0: e.dma_start(out=w_in_sb[:, :, c0:c0 + 256], in_=w_in_v[:, :, c0:c0 + 256]), r_win, no_waw=True)
            Ust = A.alloc([TPC, 512], BF16); r_Ust = [Res("Ust%d" % i) for i in range(TPC)]
            Sacc = A.alloc([512], F32); r_Sacc = Res("Sacc")
            V("pool", lambda e: e.memset(Sacc, 0.0), [], [r_Sacc])
            ri = A.alloc([14, 64], F32); r_ri = Res("ri")
            ro = A.alloc([14, 64], F32); r_ro = Res("ro")
            rt = [A.alloc([14, 32], F32) for _ in range(2)]; r_rt = [Res("rt%d" % i) for i in range(2)]
            m_sw = A.mark()

            G1 = A.alloc([D], F32); r_G1 = Res("G1")
            SH1 = A.alloc([D], F32); r_SH1 = Res("SH1")
            G1c = A.alloc([D], F32); r_G1c = Res("G1c")
            SH1c = A.alloc([D], F32); r_SH1c = Res("SH1c")
            A_main = A
            if m_scrA1 - m_scrA0 >= 15000:
                A = Arena(arena_t, m_scrA1, start=m_scrA0)
            else:
                A = Arena(arena_t, ARW, start=A_main.top)
            cv = A.alloc([16], F32); r_cv = Res("cv")
            P.dma("sp", lambda e: e.dma_start(out=cv, in_=cvecT[:, :]), r_cv)
            V("act", lambda e: e.activation(out=cv, in_=cv, func=AF.Silu), [r_cv], [r_cv])
            rep = A.alloc([16, 128], F32); r_rep = Res("rep")
            V("dve", lambda e: e.tensor_copy(out=rep, in_=cv.unsqueeze(2).to_broadcast([128, 16, 128])), [r_cv], [r_rep])
            bada2 = [A.alloc([512], F32) for _ in range(2)]; r_bada2 = [Res("bada0"), Res("bada1")]
            n1g, r_n1g = bload(norm1_g, D, "n1g")
            n2g, r_n2g = bload(norm2_g, D, "n2g")
            wst = [A.alloc([8, 512], F32) for _ in range(2)]; r_wst = [Res("wst0"), Res("wst1")]
            tmpA = A.alloc([512], F32); r_tmpA = Res("tmpA")
            w_ada_v = w_ada.rearrange("(j p) n -> p j n", p=128)
            plan = {0: ("sh", SH1, r_SH1, SH1c, r_SH1c, None), 1: ("sh", SH1, r_SH1, SH1c, r_SH1c, None),
                    2: ("sc", G1, r_G1, G1c, r_G1c, (n1g, r_n1g)), 3: ("sc", G1, r_G1, G1c, r_G1c, (n1g, r_n1g)),
                    4: ("sh", GT1, r_GT1, None, None, None), 5: ("sh", GT1, r_GT1, None, None, None),
                    6: ("sh", SH2, r_SH2, None, None, None), 7: ("sh", SH2, r_SH2, None, None, None),
                    8: ("sc", G2, r_G2, None, None, (n2g, r_n2g)), 9: ("sc", G2, r_G2, None, None, (n2g, r_n2g)),
                    10: ("sh", GT2, r_GT2, None, None, None), 11: ("sh", GT2, r_GT2, None, None, None)}
            for cb in range(12):
                kind, dst, r_dst, dstc, r_dstc, gg = plan[cb]
                wb = wst[cb % 2]; r_wb = r_wst[cb % 2]
                P.dma("sp" if cb % 2 == 0 else "act", lambda e, wb=wb, cb=cb: e.dma_start(out=wb, in_=w_ada_v[:, :, cb * 512:(cb + 1) * 512]), r_wb)
                bada = bada2[cb % 2]; r_bada = r_bada2[cb % 2]
                P.dma("sp", lambda e, bada=bada, cb=cb: e.dma_start(out=bada, in_=b_ada[:, cb * 512:(cb + 1) * 512].partition_broadcast(128)), r_bada)
                half = (cb % 2) * 512
                for which in range(2):
                    if which == 1 and dstc is None:
                        continue
                    bk = (cb * 2 + which) % 4
                    mm_group(banks[bk], rb[bk], [(rep[:, which * 8 + j, :], wb[:, j, :]) for j in range(8)], [r_rep, r_wb])
                    d_ap = (dst if which == 0 else dstc)[:, half:half + 512]
                    r_d = r_dst if which == 0 else r_dstc
                    bslice = bada
                    if kind == "sh":
                        V("dve", lambda e, bk=bk, d_ap=d_ap, bslice=bslice: e.tensor_tensor(out=d_ap, in0=banks[bk], in1=bslice, op=ALU.add),
                          [rb[bk], r_bada], [r_d])
                    else:
                        g_ap = gg[0][:, half:half + 512]
                        V("dve", lambda e, bk=bk, bslice=bslice: e.tensor_tensor(out=tmpA, in0=banks[bk], in1=bslice, op=ALU.add),
                          [rb[bk], r_bada], [r_tmpA])
                        V("dve", lambda e, d_ap=d_ap, g_ap=g_ap: e.scalar_tensor_tensor(out=d_ap, in0=tmpA, scalar=1.0, in1=g_ap, op0=ALU.add, op1=ALU.mult),
                          [r_tmpA, gg[1]], [r_d])
            if debug:
                P.dma("sp", lambda e: e.dma_start(out=dbg["d_g1"], in_=G1), r_dbg, [r_G1], no_waw=True, semres=r_G1)
            P.barrier()
            if stop == 1: raise _Stop()
            A = A_main

            distt = A.alloc([4 * NS], F32); r_distt = Res("distt")
            P.dma("sp", lambda e: e.dma_start(out=distt, in_=dist_t[:, :]), r_distt)
            Wf = A.alloc([NS, 4], F32); r_Wf = Res("Wf")
            Wb = A.alloc([NS, 4], F32); r_Wb = Res("Wb")
            for h in range(4):
                V("act", lambda e, h=h: e.activation(out=Wf[:, :, h], in_=distt[:, 0:NS], func=AF.Exp, scale=lg[:, h:h + 1]), [r_distt, r_lg], [r_Wf])
                V("act", lambda e, h=h: e.activation(out=Wb[:, :, h], in_=distt[:, NS:2 * NS], func=AF.Exp, scale=lg[:, 4 + h:5 + h]), [r_distt, r_lg], [r_Wb])
            V("dve", lambda e: e.tensor_tensor(out=Wf, in0=Wf, in1=distt[:, 2 * NS:3 * NS].unsqueeze(2).to_broadcast([128, NS, 4]), op=ALU.mult), [r_Wf, r_distt], [r_Wf])
            V("dve", lambda e: e.tensor_tensor(out=Wb, in0=Wb, in1=distt[:, 3 * NS:4 * NS].unsqueeze(2).to_broadcast([128, NS, 4]), op=ALU.mult), [r_Wb, r_distt], [r_Wb])

            NB = 2
            xt = [A.alloc([D], F32) for _ in range(NB)]; r_xt = [Res("xt%d" % i) for i in range(NB)]
            cst = [A.alloc([64], F32) for _ in range(3)]; r_cst = [Res("cst%d" % i) for i in range(3)]
            tmpx = A.alloc([D], F32); r_tmpx = Res("tmpx")
            junk = tmpx; r_junk = r_tmpx
            hx1 = A.alloc([D], BF16); hx = [hx1, hx1]; r_hx1 = Res("hx"); r_hx = [r_hx1, r_hx1]
            hxTw1 = A.alloc([8 * 128], BF16); hxTw = [hxTw1, hxTw1]; r_hxTw1 = Res("hxTw"); r_hxTw = [r_hxTw1, r_hxTw1]
            sm = [A.alloc([32], F32) for _ in range(3)]; r_sm = [Res("sm%d" % i) for i in range(3)]
            sqh = A.alloc([640], F32); r_sqh = Res("sqh")
            kb = A.alloc([128], BF16); r_kb = Res("kb")
            qb = A.alloc([8, 64], BF16); r_qb = Res("qb")
            ktb = [A.alloc([128], BF16) for _ in range(NB)]; r_ktb = [Res("ktb%d" % i) for i in range(NB)]
            v1 = [A.alloc([2, 65], BF16) for _ in range(NB)]; r_v1 = [Res("v1%d" % i) for i in range(NB)]
            for i in range(NB):
                V("pool", lambda e, i=i: e.memset(v1[i], 1.0), [], [r_v1[i]])
            kaug = A.alloc([4, 128], BF16); r_kaug = Res("kaug")
            vb = A.alloc([512], BF16); r_vb = Res("vb")

            def rope(H0, H1, cs, r_cs):
                H = H1 - H0
                riv = ri[:, H0:H1, :].rearrange("p h (i t) -> p h i t", t=2)
                rov = ro[:, H0:H1, :].rearrange("p h (i t) -> p h i t", t=2)
                cb_ = cs[:, 0:32].unsqueeze(1).to_broadcast([128, H, 32])
                sb_ = cs[:, 32:64].unsqueeze(1).to_broadcast([128, H, 32])
                t = [rt[i][:, 0:H, :] for i in range(2)]
                V("dve", lambda e: e.tensor_tensor(out=t[0], in0=riv[:, :, :, 0], in1=cb_, op=ALU.mult), [r_ri, r_cs], [r_rt[0]])
                V("dve", lambda e: e.tensor_tensor(out=t[1], in0=riv[:, :, :, 1], in1=sb_, op=ALU.mult), [r_ri, r_cs], [r_rt[1]])
                V("dve", lambda e: e.tensor_tensor(out=rov[:, :, :, 0], in0=t[0], in1=t[1], op=ALU.subtract), [r_rt[0], r_rt[1]], [r_ro])
                V("dve", lambda e: e.tensor_tensor(out=t[0], in0=riv[:, :, :, 0], in1=sb_, op=ALU.mult), [r_ri, r_cs], [r_rt[0]])
                V("dve", lambda e: e.tensor_tensor(out=t[1], in0=riv[:, :, :, 1], in1=cb_, op=ALU.mult), [r_ri, r_cs], [r_rt[1]])
                V("dve", lambda e: e.tensor_tensor(out=rov[:, :, :, 1], in0=t[0], in1=t[1], op=ALU.add), [r_rt[0], r_rt[1]], [r_ro])

            def rsqrt_mean(dst, src, r_dst, r_src, n):
                V("act", lambda e: e.activation(out=dst, in_=src, func=AF.Ln, scale=1.0 / n, bias=EPS), [r_src], [r_dst])
                V("act", lambda e: e.activation(out=dst, in_=dst, func=AF.Exp, scale=-0.5), [r_dst], [r_dst])

            def norm_mod_transpose(b, x_ap, r_x, Gm, r_Gm, SHm, r_SHm, dstT, r_dstT, tb):
                s_ = sm[b]; r_s = r_sm[b]
                V("act", lambda e: e.activation(out=junk, in_=x_ap, func=AF.Square, accum_out=s_[:, 0:1]), [r_x], [r_junk, r_s])
                rsqrt_mean(s_[:, 1:2], s_[:, 0:1], r_s, r_s, D)
                V("dve", lambda e: e.scalar_tensor_tensor(out=tmpx, in0=x_ap, scalar=s_[:, 1:2], in1=Gm, op0=ALU.mult, op1=ALU.mult),
                  [r_x, r_s, r_Gm], [r_tmpx])
                V("pool", lambda e: e.tensor_tensor(out=hx[b], in0=tmpx, in1=SHm, op=ALU.add), [r_tmpx, r_SHm], [r_hx[b]])
                pt = bank_bf(tb)
                for j in range(8):
                    V("pe", lambda e, j=j: e.transpose(out=pt[:, j * 128:(j + 1) * 128], in_=hx[b][:, j * 128:(j + 1) * 128], identity=ident_b),
                      [r_hx[b], r_idb], [rb[tb]], inc=(j == 7), pe_acc=(j > 0))
                V("act", lambda e: e.activation(out=dstT, in_=pt, func=AF.Copy), [rb[tb]], [r_dstT])

            xpv = xp.rearrange("(s p) n -> s p n", p=128)
            cosv = cos_t.rearrange("(s p) n -> s p n", p=128)
            sinv = sin_t.rearrange("(s p) n -> s p n", p=128)

            def slot_info(s):
                own = s < TPC
                isctx = s >= NT
                if own:
                    dstT = hxT_own[:, s, :]; r_dT = r_hxT_own[s]
                else:
                    dstT = hxTw[0]; r_dT = r_hxTw[0]
                return own, isctx, dstT, r_dT

            def S1a(s):
                b = s % NB
                c3 = s % 3
                own, isctx, dstT, r_dT = slot_info(s)
                P.dma("sp", lambda e: e.dma_start(out=xt[b], in_=xpv[s]), r_xt[b])
                P.dma("sp", lambda e: e.dma_start(out=cst[c3][:, 0:32], in_=cosv[s]), r_cst[c3])
                P.dma("sp", lambda e: e.dma_start(out=cst[c3][:, 32:64], in_=sinv[s]), r_cst[c3])
                Gm, r_Gm = (G1c, r_G1c) if isctx else (G1, r_G1)
                SHm, r_SHm = (SH1c, r_SH1c) if isctx else (SH1, r_SH1)
                s_ = sm[c3]; r_s = r_sm[c3]
                x_ap = xt[b]; r_x = r_xt[b]
                V("act", lambda e: e.activation(out=junk, in_=x_ap, func=AF.Square, accum_out=s_[:, 0:1]), [r_x], [r_junk, r_s])
                rsqrt_mean(s_[:, 1:2], s_[:, 0:1], r_s, r_s, D)
                V("dve", lambda e: e.scalar_tensor_tensor(out=tmpx, in0=x_ap, scalar=s_[:, 1:2], in1=Gm, op0=ALU.mult, op1=ALU.mult),
                  [r_x, r_s, r_Gm], [r_tmpx])
                V("pool", lambda e: e.tensor_tensor(out=hx[0], in0=tmpx, in1=SHm, op=ALU.add), [r_tmpx, r_SHm], [r_hx[0]])
                tb = s % 2
                pt = bank_bf(tb)
                for j in range(8):
                    V("pe", lambda e, j=j: e.transpose(out=pt[:, j * 128:(j + 1) * 128], in_=hx[0][:, j * 128:(j + 1) * 128], identity=ident_b),
                      [r_hx[0], r_idb], [rb[tb]], inc=(j == 7), pe_acc=(j > 0))
                if debug and s == 0:
                    V("dve", lambda e: e.tensor_copy(out=tmpx, in_=hx[0]), [r_hx[0]], [r_tmpx])
                    P.dma("sp", lambda e: e.dma_start(out=dbg["d_hx"], in_=tmpx), r_dbg, [r_tmpx], no_waw=True, semres=r_tmpx)

            def pbanks(s):
                return (2, 3) if s % 2 == 0 else (5, 6)

            def S1b(s):
                own, isctx, dstT, r_dT = slot_info(s)
                tb = s % 2
                V("act", lambda e: e.activation(out=dstT, in_=bank_bf(tb), func=AF.Copy), [rb[tb]], [r_dT])
                hT = dstT.rearrange("p (j t) -> p j t", j=8)
                bA, bB = pbanks(s)
                mm_group(banks[bA][:, 0:256], rb[bA], [(hT[:, j, :], w_in_sb[:, j, 512:768]) for j in range(8)], [r_dT, r_win])
                mm_group(banks[bA][:, 256:512], rb[bA], [(hT[:, j, :], w_in_sb[:, j, 1536:1792]) for j in range(8)], [r_dT, r_win])
                mm_group(banks[bB], rb[bB], [(hT[:, j, :], w_in_sb[:, j, 1024:1536]) for j in range(8)], [r_dT, r_win])

            def S1c(s):
                own, isctx, dstT, r_dT = slot_info(s)
                if own:
                    hT = dstT.rearrange("p (j t) -> p j t", j=8)
                    mm_group(banks[4], rb[4], [(hT[:, j, :], w_in_sb[:, j, 0:512]) for j in range(8)], [r_dT, r_win])

            def S2(s):
                b = s % NB
                c3 = s % 3
                own, isctx, dstT, r_dT = slot_info(s)
                bA, bB = pbanks(s)
                s_ = sm[c3]; r_s = r_sm[c3]
                H0 = 0 if own else 8
                V("act", lambda e: e.activation(out=sqh[:, 512:640], in_=banks[bA][:, 0:128], func=AF.Square), [rb[bA]], [r_sqh])
                if own:
                    V("act", lambda e: e.activation(out=sqh[:, 0:512], in_=banks[4], func=AF.Square), [rb[4]], [r_sqh])
                hs = s_[:, 8 + H0:18]
                V("dve", lambda e: e.tensor_reduce(out=hs, in_=sqh[:, H0 * 64:640].rearrange("p (h d) -> p h d", d=64), axis=AX.X, op=ALU.add),
                  [r_sqh], [r_s])
                rsqrt_mean(hs, hs, r_s, r_s, 64)
                V("dve", lambda e: e.tensor_tensor(out=ri[:, 8:10, :], in0=banks[bA][:, 0:128].rearrange("p (h d) -> p h d", d=64),
                                                   in1=s_[:, 16:18].unsqueeze(2).to_broadcast([128, 2, 64]), op=ALU.mult), [rb[bA], r_s], [r_ri])
                V("dve", lambda e: e.tensor_tensor(out=ri[:, 8:10, :], in0=ri[:, 8:10, :], in1=kn_bc.unsqueeze(1).to_broadcast([128, 2, 64]), op=ALU.mult),
                  [r_ri, r_kn], [r_ri])
                if own:
                    V("dve", lambda e: e.tensor_tensor(out=ri[:, 0:8, :], in0=banks[4].rearrange("p (h d) -> p h d", d=64),
                                                       in1=s_[:, 8:16].unsqueeze(2).to_broadcast([128, 8, 64]), op=ALU.mult), [rb[4], r_s], [r_ri])
                    V("dve", lambda e: e.tensor_tensor(out=ri[:, 0:8, :], in0=ri[:, 0:8, :], in1=qn_bc.unsqueeze(1).to_broadcast([128, 8, 64]), op=ALU.mult),
                      [r_ri, r_qn], [r_ri])
                V("act", lambda e: e.activation(out=ri[:, 10:14, :], in_=banks[bB][:, 0:256].rearrange("p (h d) -> p h d", d=64), func=AF.Copy, scale=0.125),
                  [rb[bB]], [r_ri])
                V("act", lambda e: e.activation(out=v1[b][:, :, 0:64], in_=banks[bA][:, 128:256].rearrange("p (h d) -> p h d", d=64), func=AF.Copy),
                  [rb[bA]], [r_v1[b]])
                P.dma("pool", lambda e: e.dma_start(out=V_d[s], in_=v1[b]), r_Vd, [r_v1[b]], no_waw=True, semres=r_v1[b])
                V("act", lambda e: e.activation(out=vb[:, 0:256], in_=banks[bB][:, 256:512], func=AF.Copy), [rb[bB]], [r_vb])
                V("act", lambda e: e.activation(out=vb[:, 256:512], in_=banks[bA][:, 256:512], func=AF.Copy), [rb[bA]], [r_vb])
                rope(H0, 14, cst[c3], r_cst[c3])
                V("dve", lambda e: e.tensor_copy(out=kb, in_=ro[:, 8:10, :]), [r_ro], [r_kb])
                pt7 = bank_bf(7)
                V("pe", lambda e: e.transpose(out=pt7[:, 0:128], in_=kb, identity=ident_b), [r_kb, r_idb], [rb[7]])
                V("act", lambda e: e.activation(out=ktb[b], in_=pt7[:, 0:128], func=AF.Copy), [rb[7]], [r_ktb[b]])
                P.dma("pool", lambda e: e.dma_start(out=KT_d[:, s * 128:(s + 1) * 128], in_=ktb[b]), r_KTd, [r_ktb[b]], no_waw=True, semres=r_ktb[b])
                if own:
                    V("dve", lambda e: e.tensor_copy(out=qb, in_=ro[:, 0:8, :]), [r_ro], [r_qb])
                    for g in range(4):
                        V("pe", lambda e, g=g: e.transpose(out=pt7[0:64, g * 128:(g + 1) * 128], in_=qb[:, g, :], identity=ident_b),
                          [r_qb, r_idb], [rb[7]], inc=False, pe_acc=(g > 0))
                    for g in range(4):
                        V("pe", lambda e, g=g: e.transpose(out=pt7[64:128, g * 128:(g + 1) * 128], in_=qb[:, 4 + g, :], identity=ident_b),
                          [r_qb, r_idb], [rb[7]], inc=(g == 3), pe_acc=True)
                    V("act", lambda e: e.activation(out=QT[:, s, :], in_=pt7[:, 0:512], func=AF.Copy), [rb[7]], [r_QT])
                V("dve", lambda e: e.tensor_tensor(out=kaug[:, :, 0:64], in0=ro[:, 10:14, :], in1=Wf[:, s, :].unsqueeze(2).to_broadcast([128, 4, 64]), op=ALU.mult),
                  [r_ro, r_Wf], [r_kaug])
                V("dve", lambda e: e.tensor_tensor(out=kaug[:, :, 64:128], in0=ro[:, 10:14, :], in1=Wb[:, s, :].unsqueeze(2).to_broadcast([128, 4, 64]), op=ALU.mult),
                  [r_ro, r_Wb], [r_kaug])
                for h in range(4):
                    P.op("pe", lambda e, h=h: e.matmul(banks[7][:, h * 128:(h + 1) * 128], kaug[:, h, :], vb[:, h * 128:(h + 1) * 128], start=True, stop=True),
                         [r_kaug, r_vb], [rb[7]], inc=(h == 3), pe_acc=(h > 0))
                if own:
                    V("dve", lambda e: e.tensor_copy(out=Ust[:, s, :], in_=banks[7]), [rb[7]], [r_Ust[s]])
                else:
                    V("dve", lambda e: e.tensor_tensor(out=Sacc, in0=Sacc, in1=banks[7], op=ALU.add), [r_Sacc, rb[7]], [r_Sacc])

            S1a(0)
            if NS > 1:
                S1a(1)
            S1b(0)
            S1c(0)
            for s in range(NS):
                if s + 2 < NS:
                    S1a(s + 2)
                if s + 1 < NS:
                    S1b(s + 1)
                S2(s)
                if s + 1 < NS:
                    S1c(s + 1)

            P.barrier()
            if stop == 2: raise _Stop()
            A.release(m_sw)

            dmt = A.alloc([256], F32); r_dmt = Res("dmt")
            P.dma("sp", lambda e: e.dma_start(out=dmt, in_=dmat_t[:, :]), r_dmt)
            pct = A.alloc([2], F32); r_pct = Res("pct")
            P.dma("sp", lambda e: e.dma_start(out=pct, in_=pcol_t[:, :]), r_pct)
            DT = A.alloc([4, 128], F32); r_DT = Res("DT")
            tmpd = A.alloc([128], F32); r_tmpd = Res("tmpd")
            for h in range(4):
                V("dve", lambda e, h=h: e.tensor_scalar(out=tmpd, in0=dmt[:, 0:128], scalar1=lg[:, h:h + 1], scalar2=None, op0=ALU.mult), [r_dmt, r_lg], [r_tmpd])
                V("dve", lambda e, h=h: e.scalar_tensor_tensor(out=tmpd, in0=dmt[:, 128:256], scalar=lg[:, 4 + h:5 + h], in1=tmpd, op0=ALU.mult, op1=ALU.add),
                  [r_dmt, r_lg, r_tmpd], [r_tmpd])
                V("act", lambda e, h=h: e.activation(out=DT[:, h, :], in_=tmpd, func=AF.Exp), [r_tmpd], [r_DT])
            xif = A.alloc([4], F32); xib = A.alloc([4], F32); r_xi = Res("xi")
            g128 = A.alloc([4], F32); r_g128 = Res("g128")
            for h in range(4):
                V("act", lambda e, h=h: e.activation(out=xif[:, h:h + 1], in_=pct[:, 0:1], func=AF.Exp, scale=lg[:, h:h + 1]), [r_pct, r_lg], [r_xi])
                V("act", lambda e, h=h: e.activation(out=xib[:, h:h + 1], in_=pct[:, 1:2], func=AF.Exp, scale=lg[:, 4 + h:5 + h]), [r_pct, r_lg], [r_xi])
            V("act", lambda e: e.activation(out=g128[0:64, :], in_=lg[0:64, 0:4], func=AF.Exp, scale=128.0), [r_lg], [r_g128])
            V("act", lambda e: e.activation(out=g128[64:128, :], in_=lg[64:128, 4:8], func=AF.Exp, scale=128.0), [r_lg], [r_g128])
            if SUB == 1: raise _Stop()
            Sst = A.alloc([TPC, 512], BF16); r_Sst = [Res("Sst%d" % i) for i in range(TPC)]
            cur = A.alloc([512], F32); r_cur = Res("cur")
            V("dve", lambda e: e.tensor_copy(out=cur, in_=Sacc), [r_Sacc], [r_cur])
            for c in range(TPC):
                V("dve", lambda e, c=c: e.tensor_copy(out=Sst[0:64, c, :], in_=cur[0:64, :]), [r_cur], [r_Sst[c]])
                for h in range(4):
                    V("dve", lambda e, c=c, h=h: e.scalar_tensor_tensor(out=cur[0:64, h * 128:(h + 1) * 128], in0=cur[0:64, h * 128:(h + 1) * 128],
                                                                       scalar=g128[0:64, h:h + 1], in1=Ust[0:64, c, h * 128:(h + 1) * 128],
                                                                       op0=ALU.mult, op1=ALU.add), [r_cur, r_g128, r_Ust[c]], [r_cur])
            for c in range(TPC - 1, -1, -1):
                V("dve", lambda e, c=c: e.tensor_copy(out=Sst[64:128, c, :], in_=cur[64:128, :]), [r_cur], [r_Sst[c]])
                for h in range(4):
                    V("dve", lambda e, c=c, h=h: e.scalar_tensor_tensor(out=cur[64:128, h * 128:(h + 1) * 128], in0=cur[64:128, h * 128:(h + 1) * 128],
                                                                       scalar=g128[64:128, h:h + 1], in1=Ust[64:128, c, h * 128:(h + 1) * 128],
                                                                       op0=ALU.mult, op1=ALU.add), [r_cur, r_g128, r_Ust[c]], [r_cur])
            if SUB == 2: raise _Stop()
            gng, r_gng = bload(gn_g, 512, "gng")
            gnb, r_gnb = bload(gn_b, 512, "gnb")
            cs2 = [A.alloc([64], F32) for _ in range(2)]; r_cs2 = [Res("cs2_0"), Res("cs2_1")]
            qk = A.alloc([8, 64], BF16); r_qk = Res("qk")
            qaug = A.alloc([4, 128], BF16); r_qaug = Res("qaug")
            qkT = A.alloc([4, 128], BF16); r_qkT = Res("qkT")
            qaT = A.alloc([4, 128], BF16); r_qaT = Res("qaT")
            qpad = A.alloc([4, 128], BF16); r_qpad = Res("qpad")
            V("pool", lambda e: e.memset(qpad, 0.0), [], [r_qpad])
            vb2 = A.alloc([512], BF16); r_vb2 = Res("vb2")
            scm = A.alloc([512], BF16); r_scm = Res("scm")
            gate = A.alloc([512], F32); r_gate = Res("gate")
            osb = A.alloc([512], F32); r_osb = Res("osb")
            osq = A.alloc([512], F32); r_osq = Res("osq")
            st2 = A.alloc([16], F32); r_st2 = Res("st2")
            for c in range(TPC):
                b = c % 2
                P.dma("sp", lambda e, c=c, b=b: e.dma_start(out=cs2[b][:, 0:32], in_=cosv[c]), r_cs2[b])
                P.dma("sp", lambda e, c=c, b=b: e.dma_start(out=cs2[b][:, 32:64], in_=sinv[c]), r_cs2[b])
                hT = hxT_own[:, c, :].rearrange("p (j t) -> p j t", j=8)
                r_dT = r_hxT_own[c]
                mm_group(banks[0][:, 0:256], rb[0], [(hT[:, j, :], w_in_sb[:, j, 768:1024]) for j in range(8)], [r_dT, r_win])
                mm_group(banks[0][:, 256:512], rb[0], [(hT[:, j, :], w_in_sb[:, j, 1024:1280]) for j in range(8)], [r_dT, r_win])
                mm_group(banks[1], rb[1], [(hT[:, j, :], w_in_sb[:, j, 1280:1792]) for j in range(8)], [r_dT, r_win])
                mm_group(banks[2], rb[2], [(hT[:, j, :], w_in_sb[:, j, 1792:2304]) for j in range(8)], [r_dT, r_win])
                V("act", lambda e: e.activation(out=ri[:, 0:4, :], in_=banks[0][:, 0:256].rearrange("p (h d) -> p h d", d=64), func=AF.Copy), [rb[0]], [r_ri])
                V("act", lambda e: e.activation(out=ri[:, 4:8, :], in_=banks[0][:, 256:512].rearrange("p (h d) -> p h d", d=64), func=AF.Copy, scale=0.125), [rb[0]], [r_ri])
                if SUB == 3: raise _Stop()
                rope(0, 8, cs2[b], r_cs2[b])
                if SUB == 4: raise _Stop()
                V("dve", lambda e: e.tensor_copy(out=qk, in_=ro[:, 0:8, :]), [r_ro], [r_qk])
                if SUB == 41: raise _Stop()
                V("dve", lambda e: e.tensor_tensor(out=qaug[:, :, 0:64], in0=ro[:, 0:4, :], in1=xif.unsqueeze(2).to_broadcast([128, 4, 64]), op=ALU.mult), [r_ro, r_xi], [r_qaug])
                V("dve", lambda e: e.tensor_tensor(out=qaug[:, :, 64:128], in0=ro[:, 0:4, :], in1=xib.unsqueeze(2).to_broadcast([128, 4, 64]), op=ALU.mult), [r_ro, r_xi], [r_qaug])
                if SUB == 42: raise _Stop()
                V("act", lambda e: e.activation(out=vb2, in_=banks[1], func=AF.Copy), [rb[1]], [r_vb2])
                if SUB == 43: raise _Stop()
                V("act", lambda e: e.activation(out=gate, in_=banks[2], func=AF.Silu), [rb[2]], [r_gate])
                if SUB == 5: raise _Stop()
                pt3 = bank_bf(3)
                qkf = qk.rearrange("p h d -> p (h d)")
                for i in range(4):
                    V("pe", lambda e, i=i: e.transpose(out=pt3[:, i * 128:(i + 1) * 128], in_=qkf[:, i * 128:(i + 1) * 128], identity=ident_b),
                      [r_qk, r_idb], [rb[3]], inc=False, pe_acc=(i > 0))
                for h in range(4):
                    V("pe", lambda e, h=h: e.transpose(out=pt3[:, 512 + h * 128:512 + (h + 1) * 128], in_=qaug[:, h, :], identity=ident_b),
                      [r_qaug, r_idb], [rb[3]], inc=(h == 3), pe_acc=True)
                if SUB == 51: raise _Stop()
                V("dve", lambda e: e.tensor_copy(out=qkT.rearrange("p a b -> p (a b)"), in_=pt3[:, 0:512]), [rb[3]], [r_qkT])
                if SUB == 52: raise _Stop()
                for h in range(4):
                    pr = (h % 2) * 64
                    V("dve", lambda e, h=h, pr=pr: e.tensor_copy(out=qpad[pr:pr + 64, h, :], in_=pt3[pr:pr + 64, (h // 2) * 128:(h // 2 + 1) * 128]), [rb[3]], [r_qpad])
                if SUB == 53: raise _Stop()
                V("act", lambda e: e.activation(out=qaT.rearrange("p a b -> p (a b)"), in_=pt3[:, 512:1024], func=AF.Copy), [rb[3]], [r_qaT])
                if SUB == 6: raise _Stop()
                for h in range(4):
                    P.op("pe", lambda e, h=h: e.matmul(banks[4][:, h * 128:(h + 1) * 128], qkT[:, 2 + h // 2, :], qpad[:, h, :], start=True, stop=True),
                         [r_qkT, r_qpad], [rb[4]], inc=(h == 3), pe_acc=(h > 0))
                V("dve", lambda e: e.tensor_tensor(out=scm, in0=banks[4], in1=DT.rearrange("p a b -> p (a b)"), op=ALU.mult), [rb[4], r_DT], [r_scm])
                for h in range(4):
                    P.op("pe", lambda e, h=h: e.matmul(banks[5][:, h * 128:(h + 1) * 128], scm[:, h * 128:(h + 1) * 128], vb2[:, h * 128:(h + 1) * 128], start=True, stop=False),
                         [r_scm, r_vb2], [rb[5]], inc=False, pe_acc=(h > 0))
                    P.op("pe", lambda e, h=h, c=c: e.matmul(banks[5][:, h * 128:(h + 1) * 128], qaT[:, h, :], Sst[:, c, h * 128:(h + 1) * 128], start=False, stop=True),
                         [r_qaT, r_Sst[c]], [rb[5]], inc=(h == 3), pe_acc=True)
                if SUB == 7: raise _Stop()
                V("act", lambda e: e.activation(out=osb, in_=banks[5], func=AF.Copy), [rb[5]], [r_osb])
                V("dve", lambda e: e.tensor_reduce(out=st2[:, 0:4], in_=osb.rearrange("p (h d) -> p h d", d=128), axis=AX.X, op=ALU.add), [r_osb], [r_st2])
                V("dve", lambda e: e.tensor_tensor(out=osq, in0=osb, in1=osb, op=ALU.mult), [r_osb], [r_osq])
                V("dve", lambda e: e.tensor_reduce(out=st2[:, 4:8], in_=osq.rearrange("p (h d) -> p h d", d=128), axis=AX.X, op=ALU.add), [r_osq], [r_st2])
                V("dve", lambda e: e.tensor_scalar(out=st2[:, 0:4], in0=st2[:, 0:4], scalar1=1.0 / 128, scalar2=None, op0=ALU.mult), [r_st2], [r_st2])
                V("dve", lambda e: e.tensor_tensor(out=st2[:, 8:12], in0=st2[:, 0:4], in1=st2[:, 0:4], op=ALU.mult), [r_st2], [r_st2])
                V("dve", lambda e: e.scalar_tensor_tensor(out=st2[:, 4:8], in0=st2[:, 4:8], scalar=1.0 / 128, in1=st2[:, 8:12], op0=ALU.mult, op1=ALU.subtract), [r_st2], [r_st2])
                V("act", lambda e: e.activation(out=st2[:, 4:8], in_=st2[:, 4:8], func=AF.Ln, bias=EPS, scale=1.0), [r_st2], [r_st2])
                V("act", lambda e: e.activation(out=st2[:, 4:8], in_=st2[:, 4:8], func=AF.Exp, scale=-0.5), [r_st2], [r_st2])
                for h in range(4):
                    V("dve", lambda e, h=h: e.tensor_scalar(out=osb[:, h * 128:(h + 1) * 128], in0=osb[:, h * 128:(h + 1) * 128], scalar1=st2[:, h:h + 1],
                                                           scalar2=st2[:, 4 + h:5 + h], op0=ALU.subtract, op1=ALU.mult), [r_osb, r_st2], [r_osb])
                V("dve", lambda e: e.tensor_tensor(out=osb, in0=osb, in1=gng, op=ALU.mult), [r_osb, r_gng], [r_osb])
                V("dve", lambda e: e.tensor_tensor(out=osb, in0=osb, in1=gnb, op=ALU.add), [r_osb, r_gnb], [r_osb])
                V("dve", lambda e, c=c: e.tensor_tensor(out=retx[:, c, :], in0=osb, in1=gate, op=ALU.mult), [r_osb, r_gate], [r_retx[c]])
                if debug:
                    V("dve", lambda e: e.tensor_tensor(out=osq, in0=osb, in1=gate, op=ALU.mult), [r_osb, r_gate], [r_osq])
                    P.dma("sp", lambda e, c=c: e.dma_start(out=dbg["d_ret"][c * 128:(c + 1) * 128, :], in_=osq), r_dbg, [r_osq], no_waw=True, semres=r_osq)

            P.barrier()
            if stop == 3: raise _Stop()
            A.release(m_persist)

            attnx = A.alloc([TPC, 512], BF16); r_attnx = [Res("attnx%d" % i) for i in range(TPC)]
            m_att = A.mark()
            KT = A.alloc([NS * 128], BF16); r_KT = Res("KT")
            V1 = A.alloc([NS, 130], BF16); r_V1 = Res("V1")
            P.dma("sp", lambda e: e.dma_start(out=KT, in_=KT_d[:, :]), r_KT, [r_KTd])
            SCH = 10
            for s0 in range(0, NS, SCH):
                s1 = min(NS, s0 + SCH)
                P.dma("act", lambda e, s0=s0, s1=s1: e.dma_start(out=V1[:, s0:s1, :], in_=V_d[s0:s1].rearrange("s p c -> p s c")), r_V1, [r_Vd], no_waw=True)
            if debug:
                dtmp = A.alloc([NS * 130], F32); r_dtmp = Res("dtmp")
                V("dve", lambda e: e.tensor_copy(out=dtmp[:, 0:NS * 128], in_=KT), [r_KT], [r_dtmp])
                P.dma("sp", lambda e: e.dma_start(out=dbg["d_kt"], in_=dtmp[:, 0:NS * 128]), r_dbg, [r_dtmp], no_waw=True, semres=r_dtmp)
                V("dve", lambda e: e.tensor_copy(out=dtmp, in_=V1.rearrange("p a b -> p (a b)")), [r_V1], [r_dtmp])
                P.dma("sp", lambda e: e.dma_start(out=dbg["d_v1"], in_=dtmp), r_dbg, [r_dtmp], no_waw=True, semres=r_dtmp)
                V("dve", lambda e: e.tensor_copy(out=dtmp[:, 0:TPC * 512], in_=QT.rearrange("p a b -> p (a b)")), [r_QT], [r_dtmp])
                P.dma("sp", lambda e: e.dma_start(out=dbg["d_qt"], in_=dtmp[:, 0:TPC * 512]), r_dbg, [r_dtmp], no_waw=True, semres=r_dtmp)
            NPT = 3
            PT = [A.alloc([1024], BF16) for _ in range(NPT)]; r_PT = [Res("PT%d" % i) for i in range(NPT)]
            OT = A.alloc([512], F32); r_OT = Res("OT")
            rcp = A.alloc([8], F32); r_rcp = Res("rcp")
            QP = [[A.alloc([512], BF16) for _ in range(2)] for _ in range(2)]
            r_QP = [[Res("QP%d%d" % (i, j)) for j in range(2)] for i in range(2)]
            for i in range(2):
                for j in range(2):
                    V("pool", lambda e, i=i, j=j: e.memset(QP[i][j], 0.0), [], [r_QP[i][j]])
            groups = [(qt, kvh) for qt in range(TPC) for kvh in range(2)]
            NPAIR = NS // 2
            assert NS % 2 == 0
            iters = [(gi, kp) for gi in range(len(groups)) for kp in range(NPAIR)]

            def prep_q(gi):
                qt, kvh = groups[gi]
                pr = kvh * 64
                qp = QP[kvh][qt % 2]; r_qp = r_QP[kvh][qt % 2]
                V("pool", lambda e: e.tensor_copy(out=qp[pr:pr + 64, :], in_=QT[pr:pr + 64, qt, :]), [r_QT], [r_qp])

            def emit_S(i):
                gi, kp = iters[i]
                qt, kvh = groups[gi]
                qp = QP[kvh][qt % 2]; r_qp = r_QP[kvh][qt % 2]
                b0 = 2 * (i % 2)
                pb = i % NPT
                for u in range(2):
                    kt = 2 * kp + u
                    P.op("pe", lambda e, u=u, kt=kt: e.matmul(banks[b0 + u], KT[:, kt * 128:(kt + 1) * 128], qp, start=True, stop=True),
                         [r_KT, r_qp], [rb[b0 + u]], inc=(u == 1), pe_acc=(u == 1))
                V("act", lambda e: e.activation(out=PT[pb], in_=pbig[:, b0 * 512:(b0 + 2) * 512], func=AF.Exp, scale=0.125, bias=negC),
                  [rb[b0], rb[b0 + 1], r_negC], [r_PT[pb]])

            def emit_PV(i):
                gi, kp = iters[i]
                qt, kvh = groups[gi]
                ob = 4 + gi % 2
                pb = i % NPT
                for u in range(2):
                    kt = 2 * kp + u
                    P.op("pe", lambda e, u=u, kt=kt: e.matmul(banks[ob][0:65, :], V1[:, kt, kvh * 65:(kvh + 1) * 65], PT[pb][:, u * 512:(u + 1) * 512],
                                                              start=(kt == 0), stop=(kt == NS - 1)),
                         [r_V1, r_PT[pb]], [rb[ob]], inc=(kt == NS - 1), pe_acc=(kt > 0))

            def post_a(gi):
                ob = 4 + gi % 2
                V("dve", lambda e: e.tensor_copy(out=OT[0:65, :], in_=banks[ob][0:65, :]), [rb[ob]], [r_OT])

            def post_b(gi):
                qt, kvh = groups[gi]
                for g in range(4):
                    V("pe", lambda e, g=g: e.transpose(out=banks[6][:, g * 66:g * 66 + 65], in_=OT[0:65, g * 128:(g + 1) * 128], identity=ident_f[0:65, 0:65]),
                      [r_OT, r_idf], [rb[6]], inc=(g == 3), pe_acc=(g > 0))
                o4 = banks[6][:, 0:264].rearrange("p (g c) -> p g c", c=66)
                V("dve", lambda e: e.reciprocal(out=rcp[:, 0:4], in_=o4[:, :, 64]), [rb[6]], [r_rcp])
                V("dve", lambda e: e.tensor_tensor(out=attnx[:, qt, kvh * 256:(kvh + 1) * 256].rearrange("p (g d) -> p g d", d=64), in0=o4[:, :, 0:64],
                                                   in1=rcp[:, 0:4].unsqueeze(2).to_broadcast([128, 4, 64]), op=ALU.mult), [rb[6], r_rcp], [r_attnx[qt]])

            prep_q(0)
            if len(groups) > 1:
                prep_q(1)
            nI = len(iters)
            pending_b = {}
            for i in range(nI + 1):
                if i < nI:
                    gi, kt = iters[i]
                    if kt == 0 and gi + 2 < len(groups) and gi >= 0:
                        pass
                    emit_S(i)
                if i >= 1:
                    emit_PV(i - 1)
                    gj, ktj = iters[i - 1]
                    if ktj == NPAIR - 1:
                        post_a(gj)
                        pending_b[i + 3] = gj
                        if gj + 2 < len(groups):
                            prep_q(gj + 2)
                if i in pending_b:
                    post_b(pending_b.pop(i))
            for k in sorted(pending_b):
                post_b(pending_b[k])
            if debug:
                for qt in range(TPC):
                    V("dve", lambda e, qt=qt: e.tensor_copy(out=OT, in_=attnx[:, qt, :]), [r_attnx[qt]], [r_OT])
                    P.dma("sp", lambda e, qt=qt: e.dma_start(out=dbg["d_attn"][qt * 128:(qt + 1) * 128, :], in_=OT), r_dbg, [r_OT], no_waw=True, semres=r_OT)
            P.barrier()
            if stop == 4: raise _Stop()
            A.release(m_att)

            w_out_sb = A.alloc([8, D], BF16); r_wout = Res("wout")
            w_out_v = w_out.rearrange("(j p) n -> p j n", p=128)
            for c0 in range(0, D, 256):
                P.dma("pool", lambda e, c0=c0: e.dma_start(out=w_out_sb[:, :, c0:c0 + 256], in_=w_out_v[:, :, c0:c0 + 256]), r_wout, no_waw=True)
            wrt = A.alloc([8, 36], F32); r_wrt = Res("wrt")
            P.dma("sp", lambda e: e.dma_start(out=wrt, in_=w_rt.rearrange("(j p) n -> p j n", p=128)), r_wrt)
            brt, r_brt = bload(b_rt, 36, "brt")
            mixT = A.alloc([8, 128], BF16); r_mixT = Res("mixT")
            xr = [A.alloc([D], F32) for _ in range(2)]; r_xr = [Res("xr0"), Res("xr1")]
            xn = A.alloc([D], F32); r_xn = Res("xn")
            h2 = A.alloc([D], F32); r_h2 = Res("h2")
            h2b = A.alloc([D], BF16); r_h2b = Res("h2b")
            h2Tf = A.alloc([8, 128], F32); r_h2Tf = Res("h2Tf")
            junk2 = A.alloc([D], BF16); r_junk2 = Res("junk2")
            lgt = A.alloc([36], F32); r_lgt = Res("lgt")
            msk = A.alloc([32], F32); r_msk = Res("msk")
            m8 = A.alloc([8], F32); r_m8 = Res("m8")
            sm3 = A.alloc([16], F32); r_sm3 = Res("sm3")
            oh = A.alloc([64], F32); r_oh = Res("oh")
            xown = xp.rearrange("(s p) n -> s p n", p=128)
            for c in range(TPC):
                b = c % 2
                P.dma("sp", lambda e, c=c, b=b: e.dma_start(out=xr[b], in_=xown[c]), r_xr[b])
                pt0 = bank_bf(0)
                for j in range(8):
                    src = attnx[:, c, j * 128:(j + 1) * 128] if j < 4 else retx[:, c, (j - 4) * 128:(j - 3) * 128]
                    rs_ = r_attnx[c] if j < 4 else r_retx[c]
                    V("pe", lambda e, j=j, src=src: e.transpose(out=pt0[:, j * 128:(j + 1) * 128], in_=src, identity=ident_b), [rs_, r_idb], [rb[0]], inc=(j == 7), pe_acc=(j > 0))
                V("act", lambda e: e.activation(out=mixT.rearrange("p a b -> p (a b)"), in_=pt0, func=AF.Copy), [rb[0]], [r_mixT])
                for hf in range(2):
                    mm_group(banks[1 + hf], rb[1 + hf], [(mixT[:, j, :], w_out_sb[:, j, hf * 512:(hf + 1) * 512]) for j in range(8)], [r_mixT, r_wout])
                for hf in range(2):
                    sl = slice(hf * 512, (hf + 1) * 512)
                    V("dve", lambda e, hf=hf, sl=sl: e.tensor_tensor(out=xn[:, sl], in0=banks[1 + hf], in1=GT1[:, sl], op=ALU.mult), [rb[1 + hf], r_GT1], [r_xn])
                V("dve", lambda e, b=b: e.tensor_tensor(out=xn, in0=xn, in1=xr[b], op=ALU.add), [r_xn, r_xr[b]], [r_xn])
                P.dma("sp", lambda e, c=c: e.dma_start(out=XN_d[c * 128:(c + 1) * 128, :], in_=xn), r_XNd, [r_xn], no_waw=True, semres=r_xn)
                if debug:
                    P.dma("sp", lambda e, c=c: e.dma_start(out=dbg["d_xnew"][c * 128:(c + 1) * 128, :], in_=xn), r_dbg, [r_xn], no_waw=True, semres=r_xn)
                V("act", lambda e: e.activation(out=junk2, in_=xn, func=AF.Square, accum_out=sm3[:, 0:1]), [r_xn], [r_junk2, r_sm3])
                V("act", lambda e: e.activation(out=sm3[:, 1:2], in_=sm3[:, 0:1], func=AF.Ln, scale=1.0 / D, bias=EPS), [r_sm3], [r_sm3])
                V("act", lambda e: e.activation(out=sm3[:, 1:2], in_=sm3[:, 1:2], func=AF.Exp, scale=-0.5), [r_sm3], [r_sm3])
                V("dve", lambda e: e.scalar_tensor_tensor(out=h2, in0=xn, scalar=sm3[:, 1:2], in1=G2, op0=ALU.mult, op1=ALU.mult), [r_xn, r_sm3, r_G2], [r_h2])
                V("dve", lambda e: e.tensor_tensor(out=h2, in0=h2, in1=SH2, op=ALU.add), [r_h2, r_SH2], [r_h2])
                V("pool", lambda e: e.tensor_copy(out=h2b, in_=h2), [r_h2], [r_h2b])
                pt3 = bank_bf(3)
                for j in range(8):
                    V("pe", lambda e, j=j: e.transpose(out=pt3[:, j * 128:(j + 1) * 128], in_=h2b[:, j * 128:(j + 1) * 128], identity=ident_b), [r_h2b, r_idb], [rb[3]], inc=(j == 7), pe_acc=(j > 0))
                V("act", lambda e, c=c: e.activation(out=h2T[:, :, c * 128:(c + 1) * 128], in_=pt3.rearrange("p (j t) -> p j t", j=8), func=AF.Copy), [rb[3]], [r_h2T[c]])
                for j in range(8):
                    bk = 4 + j // 4
                    V("pe", lambda e, j=j, bk=bk: e.transpose(out=banks[bk][:, (j % 4) * 128:(j % 4 + 1) * 128], in_=h2[:, j * 128:(j + 1) * 128], identity=ident_f), [r_h2, r_idf], [rb[bk]],
                      inc=(j % 4 == 3), pe_acc=(j % 4 > 0))
                V("dve", lambda e: e.tensor_copy(out=h2Tf[:, 0:4, :].rearrange("p a b -> p (a b)"), in_=banks[4]), [rb[4]], [r_h2Tf])
                V("dve", lambda e: e.tensor_copy(out=h2Tf[:, 4:8, :].rearrange("p a b -> p (a b)"), in_=banks[5]), [rb[5]], [r_h2Tf])
                mm_group(banks[6][:, 0:36], rb[6], [(h2Tf[:, j, :], wrt[:, j, :]) for j in range(8)], [r_h2Tf, r_wrt])
                V("dve", lambda e: e.tensor_tensor(out=lgt, in0=banks[6][:, 0:36], in1=brt, op=ALU.add), [rb[6], r_brt], [r_lgt])
                V("dve", lambda e: e.tensor_reduce(out=sm3[:, 2:3], in_=lgt[:, 0:4], axis=AX.X, op=ALU.max), [r_lgt], [r_sm3])
                V("dve", lambda e: e.tensor_scalar(out=oh[:, 0:4], in0=lgt[:, 0:4], scalar1=sm3[:, 2:3], scalar2=None, op0=ALU.is_equal), [r_lgt, r_sm3], [r_oh])
                V("dve", lambda e: e.tensor_scalar(out=sm3[:, 3:4], in0=sm3[:, 2:3], scalar1=-1.0, scalar2=None, op0=ALU.mult), [r_sm3], [r_sm3])
                V("act", lambda e: e.activation(out=oh[:, 8:12], in_=lgt[:, 0:4], func=AF.Exp, bias=sm3[:, 3:4], scale=1.0, accum_out=sm3[:, 4:5]), [r_lgt, r_sm3], [r_oh, r_sm3])
                V("dve", lambda e: e.reciprocal(out=sm3[:, 5:6], in_=sm3[:, 4:5]), [r_sm3], [r_sm3])
                V("dve", lambda e: e.tensor_scalar(out=oh[:, 4:8], in0=oh[:, 0:4], scalar1=-1.0, scalar2=1e30, op0=ALU.add, op1=ALU.mult), [r_oh], [r_oh])
                V("dve", lambda e: e.tensor_tensor(out=msk.rearrange("p (g j) -> p g j", j=8), in0=lgt[:, 4:36].rearrange("p (g j) -> p g j", j=8),
                                                   in1=oh[:, 4:8].unsqueeze(2).to_broadcast([128, 4, 8]), op=ALU.add), [r_lgt, r_oh], [r_msk])
                V("dve", lambda e: e.max(out=m8, in_=msk), [r_msk], [r_m8])
                V("dve", lambda e: e.tensor_tensor(out=sm3[:, 6:7], in0=m8[:, 1:2], in1=m8[:, 0:1], op=ALU.subtract), [r_m8], [r_sm3])
                V("act", lambda e: e.activation(out=sm3[:, 6:7], in_=sm3[:, 6:7], func=AF.Exp), [r_sm3], [r_sm3])
                V("dve", lambda e: e.tensor_scalar(out=sm3[:, 6:7], in0=sm3[:, 6:7], scalar1=1.0, scalar2=None, op0=ALU.add), [r_sm3], [r_sm3])
                V("dve", lambda e: e.reciprocal(out=sm3[:, 7:8], in_=sm3[:, 6:7]), [r_sm3], [r_sm3])
                V("dve", lambda e: e.tensor_tensor(out=sm3[:, 8:9], in0=sm3[:, 7:8], in1=sm3[:, 5:6], op=ALU.mult), [r_sm3], [r_sm3])
                V("dve", lambda e: e.tensor_tensor(out=sm3[:, 9:10], in0=sm3[:, 5:6], in1=sm3[:, 8:9], op=ALU.subtract), [r_sm3], [r_sm3])
                V("dve", lambda e: e.tensor_scalar(out=oh[:, 0:32], in0=msk, scalar1=m8[:, 0:1], scalar2=sm3[:, 8:9], op0=ALU.is_equal, op1=ALU.mult), [r_msk, r_m8, r_sm3], [r_oh])
                V("dve", lambda e: e.tensor_scalar(out=oh[:, 32:64], in0=msk, scalar1=m8[:, 1:2], scalar2=sm3[:, 9:10], op0=ALU.is_equal, op1=ALU.mult), [r_msk, r_m8, r_sm3], [r_oh])
                V("dve", lambda e, c=c: e.tensor_tensor(out=Wtok[:, c, :], in0=oh[:, 0:32], in1=oh[:, 32:64], op=ALU.add), [r_oh], [r_Wtok[c]])
                if debug:
                    P.dma("sp", lambda e, c=c: e.dma_start(out=dbg["d_wtok"][c * 128:(c + 1) * 128, :], in_=Wtok[:, c, :]), r_dbg, [r_Wtok[c]], no_waw=True, semres=r_Wtok[c])
            P.barrier()
            if stop == 5: raise _Stop()
            A.release(m_moe)
            FG, r_FG = bload(final_g, D, "FG")

            yacc = A.alloc([TPC, D], F32); r_yacc = [Res("yacc%d" % i) for i in range(TPC)]
            for c in range(TPC):
                V("pool", lambda e, c=c: e.memset(yacc[:, c, :], 0.0), [], [r_yacc[c]])
            NWB = 2
            wgs = [A.alloc([8, 512], BF16) for _ in range(NWB)]
            wus = [A.alloc([8, 512], BF16) for _ in range(NWB)]
            wds = [A.alloc([4, D], BF16) for _ in range(NWB)]
            r_wgs = [Res("wgs%d" % i) for i in range(NWB)]; r_wus = [Res("wus%d" % i) for i in range(NWB)]; r_wds = [Res("wds%d" % i) for i in range(NWB)]
            sg = [A.alloc([512], F32) for _ in range(2)]; r_sg = [Res("sg0"), Res("sg1")]
            TG = min(4, TPC)
            NG = TPC // TG
            h1T = [A.alloc([4, TG * 128], BF16) for _ in range(2)]; r_h1T = [Res("h1T0"), Res("h1T1")]
            gi = 0
            for ex in range(32):
                wbf = ex % NWB
                for c0 in range(0, 512, 256):
                    P.dma("pool", lambda e, ex=ex, wbf=wbf, c0=c0: e.dma_start(out=wgs[wbf][:, :, c0:c0 + 256], in_=wg[ex].rearrange("(j p) n -> p j n", p=128)[:, :, c0:c0 + 256]), r_wgs[wbf], no_waw=(c0 > 0))
                for c0 in range(0, 512, 256):
                    P.dma("pool", lambda e, ex=ex, wbf=wbf, c0=c0: e.dma_start(out=wus[wbf][:, :, c0:c0 + 256], in_=wu[ex].rearrange("(j p) n -> p j n", p=128)[:, :, c0:c0 + 256]), r_wus[wbf], no_waw=(c0 > 0))
                for c0 in range(0, D, 256):
                    P.dma("pool", lambda e, ex=ex, wbf=wbf, c0=c0: e.dma_start(out=wds[wbf][:, :, c0:c0 + 256], in_=wd[ex].rearrange("(j p) n -> p j n", p=128)[:, :, c0:c0 + 256]), r_wds[wbf], no_waw=(c0 > 0))
                for tg in range(NG):
                    hb = gi % 2
                    gi += 1
                    N = TG * 128
                    tsl = slice(tg * N, (tg + 1) * N)
                    r_h2g = [r_h2T[tg * TG + i] for i in range(TG)]
                    for ec in range(4):
                        gb_ = (ec % 2) * 2
                        mm_group(banks[gb_][:, 0:N], rb[gb_], [(wgs[wbf][:, j, ec * 128:(ec + 1) * 128], h2T[:, j, tsl]) for j in range(8)], r_h2g + [r_wgs[wbf]])
                        mm_group(banks[gb_ + 1][:, 0:N], rb[gb_ + 1], [(wus[wbf][:, j, ec * 128:(ec + 1) * 128], h2T[:, j, tsl]) for j in range(8)], r_h2g + [r_wus[wbf]])
                        sb2 = ec % 2
                        V("act", lambda e, gb_=gb_, sb2=sb2, N=N: e.activation(out=sg[sb2][:, 0:N], in_=banks[gb_][:, 0:N], func=AF.Silu), [rb[gb_]], [r_sg[sb2]])
                        V("dve", lambda e, gb_=gb_, sb2=sb2, hb=hb, ec=ec, N=N: e.tensor_tensor(out=h1T[hb][:, ec, :], in0=banks[gb_ + 1][:, 0:N], in1=sg[sb2][:, 0:N], op=ALU.mult),
                          [rb[gb_ + 1], r_sg[sb2]], [r_h1T[hb]])
                    for i in range(TG):
                        c = tg * TG + i
                        for hf in range(2):
                            ob = 4 + (i * 2 + hf) % 4
                            mm_group(banks[ob], rb[ob], [(h1T[hb][:, ec, i * 128:(i + 1) * 128], wds[wbf][:, ec, hf * 512:(hf + 1) * 512]) for ec in range(4)], [r_h1T[hb], r_wds[wbf]])
                            V("dve", lambda e, ob=ob, c=c, hf=hf, ex=ex: e.scalar_tensor_tensor(out=yacc[:, c, hf * 512:(hf + 1) * 512], in0=banks[ob], scalar=Wtok[:, c, ex:ex + 1],
                                                                                       in1=yacc[:, c, hf * 512:(hf + 1) * 512], op0=ALU.mult, op1=ALU.add),
                              [rb[ob], r_Wtok[c], r_yacc[c]], [r_yacc[c]])
            xq = [A.alloc([D], F32) for _ in range(2)]; r_xq = [Res("xq0"), Res("xq1")]
            junk3 = A.alloc([D], BF16); r_junk3 = Res("junk3")
            sm4 = A.alloc([4], F32); r_sm4 = Res("sm4")
            for c in range(TPC):
                b = c % 2
                P.dma("sp", lambda e, c=c, b=b: e.dma_start(out=xq[b], in_=XN_d[c * 128:(c + 1) * 128, :]), r_xq[b], [r_XNd])
                V("dve", lambda e, c=c: e.tensor_tensor(out=yacc[:, c, :], in0=yacc[:, c, :], in1=GT2, op=ALU.mult), [r_yacc[c], r_GT2], [r_yacc[c]])
                V("dve", lambda e, c=c, b=b: e.tensor_tensor(out=xq[b], in0=xq[b], in1=yacc[:, c, :], op=ALU.add), [r_xq[b], r_yacc[c]], [r_xq[b]])
                V("act", lambda e, b=b: e.activation(out=junk3, in_=xq[b], func=AF.Square, accum_out=sm4[:, 0:1]), [r_xq[b]], [r_junk3, r_sm4])
                V("act", lambda e: e.activation(out=sm4[:, 1:2], in_=sm4[:, 0:1], func=AF.Ln, scale=1.0 / D, bias=EPS), [r_sm4], [r_sm4])
                V("act", lambda e: e.activation(out=sm4[:, 1:2], in_=sm4[:, 1:2], func=AF.Exp, scale=-0.5), [r_sm4], [r_sm4])
                V("dve", lambda e, b=b: e.scalar_tensor_tensor(out=xq[b], in0=xq[b], scalar=sm4[:, 1:2], in1=FG, op0=ALU.mult, op1=ALU.mult), [r_xq[b], r_sm4, r_FG], [r_xq[b]])
                P.dma("sp", lambda e, c=c, b=b: e.dma_start(out=out[c * 128:(c + 1) * 128, :], in_=xq[b]), r_out, [r_xq[b]], no_waw=True, semres=r_xq[b])
        try:
            body()
        except _Stop:
            P.barrier()
        fin = [r_out] + ([r_dbg] if debug else [])
        P.wait_all("sp", fin)
        P.wait_all("pool", fin)
        P.emit(st)
    return nc


def _rope_tables(L):
    GRID_W = 64
    rows = L // GRID_W
    row = np.repeat(np.arange(rows), GRID_W).astype(np.float32)
    col = np.tile(np.arange(GRID_W), rows).astype(np.float32)
    freqs = (np.float32(10000.0) ** (-np.arange(0, 32, 2, dtype=np.float32) / np.float32(32))).astype(np.float32)
    ang = np.concatenate([row[:, None] * freqs, col[:, None] * freqs], axis=-1).astype(np.float32)
    return np.cos(ang).astype(np.float32), np.sin(ang).astype(np.float32)


def make_in_maps(inputs, NT):
    TPC = NT // NCORES
    NS = NT + 2
    L = NT * 128
    f = lambda a: np.ascontiguousarray(np.asarray(a, dtype=np.float32))
    x = f(inputs["x"]).reshape(L, D)
    ctx = f(inputs["ctx"]).reshape(256, D)
    cos, sin = _rope_tables(L)
    cvecT = np.zeros((128, 16), np.float32)
    cvecT[:, 0:8] = f(inputs["c"]).reshape(8, 128).T
    cvecT[:, 8:16] = f(inputs["c_ctx"]).reshape(8, 128).T
    p = np.arange(128, dtype=np.float32)
    dpos = np.maximum(p[None, :] - p[:, None], 0.0)
    dneg = np.maximum(p[:, None] - p[None, :], 0.0)
    dmat = np.concatenate([dpos, dneg], axis=1).astype(np.float32)
    pcol = np.stack([p + 1.0, 128.0 - p], axis=1).astype(np.float32)
    shared = {
        "cvecT": cvecT,
        "w_ada": f(inputs["w_ada"]).reshape(D, 6 * D), "b_ada": f(inputs["b_ada"]).reshape(1, 6 * D),
        "norm1_g": f(inputs["norm1_g"]).reshape(1, D), "norm2_g": f(inputs["norm2_g"]).reshape(1, D),
        "final_g": f(inputs["final_norm_g"]).reshape(1, D),
        "w_in": f(inputs["w_in"]).reshape(D, 2304),
        "qn": f(inputs["attn_q_norm"]).reshape(1, 64), "kn": f(inputs["attn_k_norm"]).reshape(1, 64),
        "dec": np.concatenate([f(inputs["ret_decay_fwd"]).reshape(1, 4), f(inputs["ret_decay_bwd"]).reshape(1, 4)], axis=1),
        "gn_g": f(inputs["ret_gn_g"]).reshape(1, 512), "gn_b": f(inputs["ret_gn_b"]).reshape(1, 512),
        "w_out": f(inputs["w_out"]).reshape(D, D),
        "w_rt": np.ascontiguousarray(np.concatenate([f(inputs["moe_w_grp"]).reshape(D, 4), f(inputs["moe_w_exp"]).reshape(D, 32)], axis=1)),
        "b_rt": np.concatenate([f(inputs["moe_b_grp"]).reshape(1, 4), f(inputs["moe_b_exp"]).reshape(1, 32)], axis=1),
        "wg": f(inputs["moe_w_gate"]).reshape(32, D, 512), "wu": f(inputs["moe_w_up"]).reshape(32, D, 512),
        "wd": f(inputs["moe_w_down"]).reshape(32, 512, D),
        "dmat_t": dmat, "pcol_t": pcol,
    }
    maps = []
    for r in range(NCORES):
        own = list(range(r * TPC, (r + 1) * TPC))
        others = [t for t in range(NT) if t not in own]
        order = own + others
        rows = np.concatenate([np.arange(t * 128, (t + 1) * 128) for t in order])
        xpm = np.concatenate([x[rows], ctx], axis=0)
        cosp = np.concatenate([cos[rows], np.ones((256, 32), np.float32)], axis=0)
        sinp = np.concatenate([sin[rows], np.zeros((256, 32), np.float32)], axis=0)
        s0 = r * TPC * 128
        e0 = s0 + TPC * 128
        distf = np.zeros((128, NS), np.float32); distb = np.zeros((128, NS), np.float32)
        maskf = np.zeros((128, NS), np.float32); maskb = np.zeros((128, NS), np.float32)
        for s, t in enumerate(order):
            m = t * 128 + p
            if s < TPC:
                distf[:, s] = 127.0 - p; distb[:, s] = p; maskf[:, s] = 1.0; maskb[:, s] = 1.0
            elif t * 128 < s0:
                distf[:, s] = s0 - 1 - m; maskf[:, s] = 1.0
            else:
                distb[:, s] = m - e0; maskb[:, s] = 1.0
        for j in range(2):
            mc = j * 128 + p
            distf[:, NT + j] = s0 + 255 - mc; maskf[:, NT + j] = 1.0
            distb[:, NT + j] = L + mc - e0; maskb[:, NT + j] = 1.0
        d = dict(shared)
        d["xp"] = np.ascontiguousarray(xpm)
        d["cos_t"] = np.ascontiguousarray(cosp)
        d["sin_t"] = np.ascontiguousarray(sinp)
        d["dist_t"] = np.ascontiguousarray(np.concatenate([distf, distb, maskf, maskb], axis=1))
        maps.append(d)
    return maps


_NC_CACHE = {}


def run(inputs, NT, debug=False, stop=99, cores=None):
    key = (NT, debug, stop)
    if key not in _NC_CACHE:
        _NC_CACHE[key] = build_program(NT, debug, stop)
    nc = _NC_CACHE[key]
    maps = make_in_maps(inputs, NT)
    if stop < 6:
        for m in maps:
            for k in ("wg", "wu", "wd"):
                m[k] = m[k][0:1]
    if cores is not None:
        maps = [maps[c] for c in cores]
        return run_bass_kernel_spmd(nc, maps, core_ids=list(range(len(cores))))
    res = run_bass_kernel_spmd(nc, maps, core_ids=list(range(NCORES)))
    return res


def kernel(**inputs):
    NT = 128
    res = run(inputs, NT)
    outp = np.concatenate([np.asarray(res.results[r]["out"]) for r in range(NCORES)], axis=0)
    return outp.reshape(1, NT * 128, D).astype(np.float32)
```
